# Optimizing a Trainium2 kernel written in Bass

```python
import jax, jax.numpy as jnp
from jax import lax
import numpy as np

D_MODEL = 2048
BATCH = 2
SEQ = 4096
DEPTH = 1

CHUNK = 64
D_CONV = 1024
CONV_WIDTH = 31
N_HEADS = 16
HEAD_DIM = 64
D_ATTN = N_HEADS * HEAD_DIM
IDX_HEADS = 16
IDX_DIM = 64
TOPK_MAX = 256
Q_BLOCK = 128
N_BRANCH = 2
D_FF = -(-8 * D_MODEL // (3 * 256)) * 256
EPS = 1e-6

SZ_GLU = 2 * D_CONV
SZ_IQ = IDX_HEADS * IDX_DIM
SZ_GATE = N_BRANCH * D_MODEL
OFF_1 = SZ_GLU
OFF_2 = OFF_1 + D_ATTN
OFF_3 = OFF_2 + D_ATTN
OFF_4 = OFF_3 + D_ATTN
OFF_5 = OFF_4 + SZ_IQ
OFF_6 = OFF_5 + IDX_DIM
OFF_7 = OFF_6 + IDX_HEADS
D_IN = OFF_7 + SZ_GATE

kernel_name = "hybrid_conformer_dsa_block"


def rms_norm(x, g):
    xf = x.astype(jnp.float32)
    y = xf * lax.rsqrt(jnp.mean(xf * xf, axis=-1, keepdims=True) + EPS)
    return (y * g.astype(jnp.float32)).astype(x.dtype)


def layer_norm(x, g, b):
    xf = x.astype(jnp.float32)
    mu = jnp.mean(xf, axis=-1, keepdims=True)
    var = jnp.mean(jnp.square(xf - mu), axis=-1, keepdims=True)
    y = (xf - mu) * lax.rsqrt(var + EPS)
    return (y * g.astype(jnp.float32) + b.astype(jnp.float32)).astype(x.dtype)


def conv_branch(u_glu, w_dw, b_dw, g_ln, b_ln, w_out):
    a, gt = jnp.split(u_glu, 2, axis=-1)
    u = a * jax.nn.sigmoid(gt)
    u = lax.conv_general_dilated(
        u, w_dw[:, None, :], window_strides=(1,),
        padding=[(CONV_WIDTH - 1, 0)],
        dimension_numbers=("NWC", "WIO", "NWC"),
        feature_group_count=D_CONV) + b_dw
    u = jax.nn.silu(layer_norm(u, g_ln, b_ln))
    return u @ w_out


def sparse_attn_branch(q, k, v, q_idx, k_idx, w_idx, w_out):
    B, S = q.shape[0], q.shape[1]
    topk = min(TOPK_MAX, S // 4)
    nblk = S // Q_BLOCK
    qb_all = q.reshape(B, nblk, Q_BLOCK, N_HEADS, HEAD_DIM).transpose(1, 0, 2, 3, 4)
    qi_all = q_idx.reshape(B, nblk, Q_BLOCK, IDX_HEADS, IDX_DIM).transpose(1, 0, 2, 3, 4)
    wi_all = w_idx.reshape(B, nblk, Q_BLOCK, IDX_HEADS).transpose(1, 0, 2, 3)
    k4 = k.reshape(B, S, N_HEADS, HEAD_DIM)
    v4 = v.reshape(B, S, N_HEADS, HEAD_DIM)
    k_idx_f = k_idx.astype(jnp.float32)
    key_chunk = jnp.arange(S) // CHUNK
    idx_scale = (IDX_DIM ** -0.5) * (IDX_HEADS ** -0.5)
    attn_scale = HEAD_DIM ** -0.5
    gather = jax.vmap(lambda t, i: t[i])

    def block(args):
        blk, qb, qib, wb = args
        pos = blk * Q_BLOCK + jnp.arange(Q_BLOCK)
        q_chunk = pos // CHUNK
        admissible = key_chunk[None, :] <= q_chunk[:, None]
        rel = jax.nn.relu(jnp.einsum("bqhd,bsd->bqhs", qib.astype(jnp.float32), k_idx_f))
        score = jnp.einsum("bqhs,bqh->bqs", rel, wb.astype(jnp.float32)) * idx_scale
        score = jnp.where(admissible[None], score, -jnp.inf)
        _, sel = lax.top_k(score, topk)
        valid = key_chunk[sel] <= q_chunk[None, :, None]
        k_sel = gather(k4, sel)
        v_sel = gather(v4, sel)
        logits = jnp.einsum("bqhd,bqkhd->bqhk", qb, k_sel).astype(jnp.float32) * attn_scale
        logits = jnp.where(valid[:, :, None, :], logits, -jnp.inf)
        p = jax.nn.softmax(logits, axis=-1).astype(v4.dtype)
        return jnp.einsum("bqhk,bqkhd->bqhd", p, v_sel)

    out = lax.map(block, (jnp.arange(nblk), qb_all, qi_all, wi_all))
    out = out.transpose(1, 0, 2, 3, 4).reshape(B, S, D_ATTN)
    return out @ w_out


def setup_inputs(seed: int = 0) -> dict:
    key = jax.random.key(seed)
    ks = jax.random.split(key, 20)
    f32 = jnp.float32

    def nrm(k, shape, fan_in, gain=1.0):
        return jax.random.normal(k, shape, f32) * (gain * fan_in ** -0.5)

    def ones_noise(k, shape):
        return 1.0 + 0.05 * jax.random.normal(k, shape, f32)

    L = DEPTH
    return {
        "x": jax.random.normal(ks[0], (BATCH, SEQ, D_MODEL), f32),
        "c": jax.random.normal(ks[1], (BATCH, D_MODEL), f32),
        "w_ada": nrm(ks[2], (L, D_MODEL, 6 * D_MODEL), D_MODEL, 0.5),
        "b_ada": 0.02 * jax.random.normal(ks[3], (L, 6 * D_MODEL), f32),
        "g_pre_mix": ones_noise(ks[4], (L, D_MODEL)),
        "w_in": nrm(ks[5], (L, D_MODEL, D_IN), D_MODEL),
        "w_dw": nrm(ks[6], (L, CONV_WIDTH, D_CONV), CONV_WIDTH),
        "b_dw": 0.02 * jax.random.normal(ks[7], (L, D_CONV), f32),
        "g_conv_ln": ones_noise(ks[8], (L, D_CONV)),
        "b_conv_ln": 0.02 * jax.random.normal(ks[9], (L, D_CONV), f32),
        "w_conv_out": nrm(ks[10], (L, D_CONV, D_MODEL), D_CONV),
        "w_attn_out": nrm(ks[11], (L, D_ATTN, D_MODEL), D_ATTN),
        "w_o": nrm(ks[12], (L, D_MODEL, D_MODEL), D_MODEL),
        "g_post_mix": ones_noise(ks[13], (L, D_MODEL)),
        "g_pre_ffn": ones_noise(ks[14], (L, D_MODEL)),
        "w_gate": nrm(ks[15], (L, D_MODEL, D_FF), D_MODEL),
        "w_up": nrm(ks[16], (L, D_MODEL, D_FF), D_MODEL),
        "w_down": nrm(ks[17], (L, D_FF, D_MODEL), D_FF),
        "g_post_ffn": ones_noise(ks[18], (L, D_MODEL)),
    }


def reference(x, c, w_ada, b_ada, g_pre_mix, w_in, w_dw, b_dw, g_conv_ln, b_conv_ln,
              w_conv_out, w_attn_out, w_o, g_post_mix, g_pre_ffn, w_gate, w_up, w_down,
              g_post_ffn):
    c_act = jax.nn.silu(c)
    for l in range(DEPTH):
        mod = c_act @ w_ada[l] + b_ada[l]
        sh1, sc1, gt1, sh2, sc2, gt2 = jnp.split(mod[:, None, :], 6, axis=-1)

        h = rms_norm(x, g_pre_mix[l]) * (1.0 + sc1) + sh1
        proj = h @ w_in[l]
        u_glu, q, k, v, qi, ki, wi, gates = jnp.split(
            proj, [OFF_1, OFF_2, OFF_3, OFF_4, OFF_5, OFF_6, OFF_7], axis=-1)
        y_conv = conv_branch(u_glu, w_dw[l], b_dw[l], g_conv_ln[l], b_conv_ln[l], w_conv_out[l])
        y_attn = sparse_attn_branch(q, k, v, qi, ki, wi, w_attn_out[l])
        g = jax.nn.sigmoid(gates.astype(jnp.float32)).astype(x.dtype)
        g_conv, g_attn = jnp.split(g, 2, axis=-1)
        mixed = (g_conv * y_conv + g_attn * y_attn) @ w_o[l]
        x = x + gt1 * rms_norm(mixed, g_post_mix[l])

        h = rms_norm(x, g_pre_ffn[l]) * (1.0 + sc2) + sh2
        f = (jax.nn.silu(h @ w_gate[l]) * (h @ w_up[l])) @ w_down[l]
        x = x + gt2 * rms_norm(f, g_post_ffn[l])
    return x
```

```python
import numpy as np
from contextlib import ExitStack
import concourse.bass as bass
import concourse.mybir as mybir
from concourse.bass_utils import run_bass_kernel_spmd

F32 = mybir.dt.float32
BF16 = mybir.dt.bfloat16
AF = mybir.ActivationFunctionType
ALU = mybir.AluOpType
AX = mybir.AxisListType

D = 2048
KC = 16
NOWN = 1024
NCTX = 3072
NK = 4096
DFF = 5632
EPS = 1e-6
BIG = 4096.0
NITER = 17
SBQ = 2
O_GLU_A, O_GLU_G, O_Q, O_K, O_V, O_QI, O_KI, O_WI, O_GC, O_GA = (
    0, 1024, 2048, 3072, 4096, 5120, 6144, 6208, 6224, 8272)
IDX_SCALE = (64 ** -0.5) * (16 ** -0.5)
import os
BIS = os.environ.get('BIS', '')


class Buf:
    __slots__ = ("w", "r", "name")

    def __init__(self, name=""):
        self.w = {}
        self.r = {}
        self.name = name


class Eng:
    def __init__(self, name, eng, sem):
        self.name = name
        self.eng = eng
        self.sem = sem
        self.cnt = 0
        self.seen = {}


class Sched:
    def __init__(self, nc, stack, n_slots=40):
        self.nc = nc
        mk = lambda n: stack.enter_context(nc.semaphore(n))
        self.PE = Eng("pe", nc.tensor, mk("s_pe"))
        self.ACT = Eng("act", nc.scalar, mk("s_act"))
        self.DVE = Eng("dve", nc.vector, mk("s_dve"))
        self.POOL = Eng("pool", nc.gpsimd, mk("s_pool"))
        self.SP = Eng("sp", nc.sync, mk("s_sp"))
        self.engs = [self.PE, self.ACT, self.DVE, self.POOL, self.SP]
        self.slots = [[f"dma{i}", mk(f"s_dma{i}"), 0] for i in range(n_slots)]
        self.slot_i = 0
        self.prog = {}

    def _deps(self, reads, writes):
        d = {}

        def add(dic):
            for k, (sem, v) in dic.items():
                if k not in d or d[k][1] < v:
                    d[k] = (sem, v)
        for b in reads:
            add(b.w)
        for b in writes:
            add(b.w)
            add(b.r)
        return d

    def _wait(self, E, d):
        for k, (sem, v) in d.items():
            if E is self.PE and k == "pe":
                continue
            if E.seen.get(k, 0) >= v:
                continue
            E.eng.wait_ge(sem, v)
            E.seen[k] = v
            self.prog.setdefault(E.name, []).append(("w", k, v))

    def _mark(self, k, sem, v, reads, writes):
        for b in reads:
            if k not in b.r or b.r[k][1] < v:
                b.r[k] = (sem, v)
        for b in writes:
            b.w[k] = (sem, v)
            b.r = {}

    def op(self, E, fn, reads=(), writes=()):
        self._wait(E, self._deps(reads, writes))
        inst = fn(E.eng)
        E.cnt += 1
        inst.then_inc(E.sem, 1)
        self.prog.setdefault(E.name, []).append(("i", E.name, 1))
        self._mark(E.name, E.sem, E.cnt, reads, writes)

    def dma(self, Q, fn, reads=(), writes=()):
        d = self._deps(reads, writes)
        slot = self.slots[self.slot_i % len(self.slots)]
        self.slot_i += 1
        if slot[2] > 0:
            d[slot[0]] = (slot[1], slot[2])
        self._wait(Q, d)
        inst = fn(Q.eng)
        slot[2] += 16
        inst.then_inc(slot[1], 16)
        self.prog.setdefault(Q.name, []).append(("i", slot[0], 16))
        self._mark(slot[0], slot[1], slot[2], reads, writes)

    def barrier(self):
        evs = {}
        for E in self.engs:
            if E.cnt > 0:
                evs[E.name] = (E.sem, E.cnt)
        for s in self.slots:
            if s[2] > 0:
                evs[s[0]] = (s[1], s[2])
        for E in self.engs:
            self._wait(E, evs)


class Arena:
    def __init__(self, nc, stack, nbytes):
        self.t = stack.enter_context(nc.sbuf_tensor("arena", [128, nbytes // 4], F32))
        self.free = [(0, nbytes)]

    def alloc(self, nbytes):
        nbytes = (nbytes + 63) // 64 * 64
        for i, (o, n) in enumerate(self.free):
            if n >= nbytes:
                if n == nbytes:
                    self.free.pop(i)
                else:
                    self.free[i] = (o + nbytes, n - nbytes)
                return o, nbytes
        raise MemoryError(f"arena out of space for {nbytes}: free={self.free}")

    def release(self, o, n):
        fl = sorted(self.free + [(o, n)])
        out = []
        for a, b in fl:
            if out and out[-1][0] + out[-1][1] == a:
                out[-1] = (out[-1][0], out[-1][1] + b)
            else:
                out.append((a, b))
        self.free = out


class Scope:
    def __init__(self, arena):
        self.a = arena
        self.items = []

    def __enter__(self):
        return self

    def __exit__(self, *exc):
        self.close()
        return False

    def close(self):
        for o, n in self.items:
            self.a.release(o, n)
        self.items = []

    def alloc(self, shape, dt):
        esz = 4 if dt == F32 else 2
        n = 1
        for d_ in shape[1:]:
            n *= d_
        o, nb = self.a.alloc(n * esz)
        self.items.append((o, nb))
        v = self.a.t[0:shape[0], o // 4:(o + nb) // 4]
        if dt != F32:
            v = v.bitcast(dt)
        v = v[:, 0:n]
        if len(shape) == 3:
            v = v.rearrange("p (a b) -> p a b", a=shape[1])
        elif len(shape) == 4:
            v = v.rearrange("p (a b c) -> p a b c", a=shape[1], b=shape[2])
        return v


class _Stop(Exception):
    pass


def build(debug=(), limit=99):
    nc = bass.Bass("TRN2", target_bir_lowering=False)
    stack = ExitStack()

    def din(name, shape, dt=F32):
        return nc.dram_tensor(name, shape, dt, kind="ExternalInput").ap()

    xctx = din("xctx", [NK, D])
    cT_d = din("cT", [128, 16])
    w_ada = din("w_ada", [D, 6 * D])
    b_adaT = din("b_adaT", [128, 96])
    gpmT = din("g_pre_mixT", [128, 16])
    w_in = din("w_in", [D, 10320])
    w_dwT = din("w_dwT", [128, 8 * 31])
    b_dwT = din("b_dwT", [128, 8])
    g_lnT = din("g_lnT", [128, 8])
    b_lnT = din("b_lnT", [128, 8])
    w_co = din("w_conv_out", [1024, D])
    w_ao = din("w_attn_out", [1024, D])
    w_o = din("w_o", [D, D])
    gpostmT = din("g_post_mixT", [128, 16])
    gpfT = din("g_pre_ffnT", [128, 16])
    w_gate = din("w_gate", [D, DFF])
    w_up = din("w_up", [D, DFF])
    w_down = din("w_down", [DFF, D])
    gpostfT = din("g_post_ffnT", [128, 16])
    ident_d = din("ident", [128, 128])
    kneg_d = din("kneg", [1, 512])
    pen_d = din("pen", [128, 6])
    diag_d = din("diagc", [1, 256])
    pow2_d = din("pow2", [128, NITER + 1])
    halo_d = din("halo_valid", [128, 1])

    out = nc.dram_tensor("out", [NOWN, D], F32, kind="ExternalOutput").ap()
    kT_d = nc.dram_tensor("kT_scr", [8, 128, NK], BF16, kind="Internal").ap()
    v_d = nc.dram_tensor("v_scr", [NK, 8 * 160], BF16, kind="Internal").ap()
    gsc_d = nc.dram_tensor("g_scr", [2, D], F32, kind="Internal").ap()

    dbg_outs = {}

    S = Sched(nc, stack)
    PE, ACT, DVE, POOL, SP = S.PE, S.ACT, S.DVE, S.POOL, S.SP
    try:

        arena = Arena(nc, stack, 212480)
        root = Scope(arena)

        def sb(st, name, shape, dt):
            if st is stack:
                st = root
            return st.alloc(shape, dt)

        def ps(st, name, shape, dt):
            return st.enter_context(nc.psum_tensor("ps_" + name, shape, dt))

        PB = [ps(stack, f"pb{i}", [128, 512], F32) for i in range(6)]
        PBb = [Buf(f"pb{i}") for i in range(6)]
        PT = [ps(stack, f"pt{i}", [128, 1024], BF16) for i in range(2)]
        PTb = [Buf(f"pt{i}") for i in range(2)]

        identf = sb(stack, "identf", [128, 128], F32)
        identb = sb(stack, "identb", [128, 128], BF16)
        onesf = sb(stack, "onesf", [128, 128], F32)
        onesb = sb(stack, "onesb", [128, 128], BF16)
        epsc = sb(stack, "epsc", [128, 1], F32)
        modT = sb(stack, "modT", [128, 96], F32)
        s1 = sb(stack, "s1", [128, 16], F32)
        s2 = sb(stack, "s2", [128, 16], F32)
        smallv = sb(stack, "smallv", [128, 16 * 8], F32)
        kneg_b = sb(stack, "kneg_b", [1, 512], BF16)
        ones512 = sb(stack, "ones512", [1, 512], BF16)
        diag_b = sb(stack, "diag_b", [1, 256], BF16)
        pen = sb(stack, "pen", [128, 6], F32)
        pow2 = sb(stack, "pow2", [128, NITER + 1], F32)
        halov = sb(stack, "halov", [128, 1], F32)
        wdw = sb(stack, "wdw", [128, 8 * 31], F32)
        convv = sb(stack, "convv", [128, 24], F32)
        sc_kiT, sc_hown, sc_halo = Scope(arena), Scope(arena), Scope(arena)
        kiT = sb(sc_kiT, "kiT", [128, NK], BF16)
        hT_own = sb(sc_hown, "hT_own", [128, 16, NOWN], BF16)
        hT_halo = sb(sc_halo, "hT_halo", [128, 16, 32], BF16)
        B_const = Buf("const")
        B_mod = Buf("mod")
        B_kiT = Buf("kiT")
        B_hown = [Buf("hown0"), Buf("hown1")]
        B_halo = Buf("halo")

        def tap(name, src_ap, shape, dt, reads):
            if name not in debug:
                return
            t = nc.dram_tensor("dbg_" + name, shape, dt, kind="ExternalOutput").ap()
            dbg_outs[name] = t
            S.dma(SP, lambda e: e.dma_start(out=t, in_=src_ap), reads=reads, writes=[Buf()])

        ld = lambda dst, src: S.dma(SP, lambda e: e.dma_start(out=dst, in_=src), writes=[B_const])
        ld(identf[:], ident_d)
        ld(pen[:], pen_d)
        ld(pow2[:], pow2_d)
        ld(halov[:], halo_d)
        ld(wdw[:], w_dwT)
        ld(convv[:, 0:8], b_dwT)
        ld(convv[:, 8:16], g_lnT)
        ld(convv[:, 16:24], b_lnT)
        ld(smallv[:, 0:16], gpmT)
        ld(smallv[:, 16:32], gpostmT)
        ld(smallv[:, 32:48], gpfT)
        ld(smallv[:, 48:64], gpostfT)
        S.dma(POOL, lambda e: e.dma_start(out=kneg_b[:], in_=kneg_d), writes=[B_const])
        S.op(DVE, lambda e: e.memset(ones512[:], 1.0), writes=[B_const])
        S.dma(POOL, lambda e: e.dma_start(out=diag_b[:], in_=diag_d), writes=[B_const])
        S.op(DVE, lambda e: e.tensor_copy(out=identb[:], in_=identf[:]), reads=[B_const], writes=[B_const])
        S.op(DVE, lambda e: e.memset(onesf[:], 1.0), writes=[B_const])
        S.op(DVE, lambda e: e.memset(onesb[:], 1.0), writes=[B_const])
        S.op(DVE, lambda e: e.memset(epsc[:], EPS), writes=[B_const])

        def rstd_from_ssq(dst, ssq, tmp, rb, wb):
            S.op(ACT, lambda e: e.activation(out=tmp, in_=ssq, func=AF.Sqrt, bias=epsc[:, 0:1], scale=1.0 / D),
                 reads=rb + [B_const], writes=wb)
            S.op(DVE, lambda e: e.reciprocal(out=dst, in_=tmp), reads=wb, writes=wb)

        wv_in = w_in.rearrange("(c p) n -> p c n", p=128)

        cb = sb(stack, "cb", [128, 16], BF16)
        badd = sb(stack, "badd", [128, 96], F32)
        Bc = Buf()
        with Scope(arena) as st:
            cT = sb(st, "cT", [128, 16], F32)
            wa = [sb(st, f"wa{i}", [128, 16, 512], BF16) for i in range(2)]
            Bwa = [Buf(), Buf()]
            S.dma(SP, lambda e: e.dma_start(out=cT[:], in_=cT_d), writes=[Bc])
            S.dma(SP, lambda e: e.dma_start(out=badd[:], in_=b_adaT), writes=[Bc])
            S.op(ACT, lambda e: e.activation(out=cb[:], in_=cT[:], func=AF.Silu), reads=[Bc], writes=[Bc])
            wv_ada = w_ada.rearrange("(c p) n -> p c n", p=128)
            for blk in range(8):
                w_t = wa[blk % 2]
                for q4 in range(4):
                    S.dma(POOL, lambda e, w_t=w_t, q4=q4, blk=blk: e.dma_start(
                        out=w_t[:, q4 * 4:(q4 + 1) * 4, :], in_=wv_ada[:, q4 * 4:(q4 + 1) * 4, blk * 512:(blk + 1) * 512]),
                        writes=[Bwa[blk % 2]])
                for cc in range(4):
                    m = blk * 4 + cc
                    for kc in range(KC):
                        S.op(PE, lambda e, w_t=w_t, cc=cc, kc=kc, m=m: e.matmul(
                            PB[0][:, m:m + 1], lhsT=w_t[:, kc, cc * 128:(cc + 1) * 128], rhs=cb[:, kc:kc + 1],
                            start=(kc == 0), stop=(kc == KC - 1)),
                            reads=[Bwa[blk % 2], Bc], writes=[PBb[0]])
            S.op(DVE, lambda e: e.tensor_tensor(out=modT[:, 0:32], in0=PB[0][:, 0:32], in1=badd[:, 0:32], op=ALU.add),
                 reads=[PBb[0], Bc], writes=[B_mod])
            S.op(DVE, lambda e: e.scalar_tensor_tensor(out=s1[:], in0=modT[:, 16:32], scalar=1.0, in1=smallv[:, 0:16],
                                                       op0=ALU.add, op1=ALU.mult), reads=[B_mod, B_const], writes=[B_mod])
            S.barrier()
            if limit == 1:
                raise _Stop()

        def norm_transpose(st_bufs, src_tiles, nt, sc_t, bias_ap_fn, dst_fn, dst_bufs, src_bufs):
            xn, Bxn, ssq, rs, tmp, Bst = st_bufs
            for t in range(nt):
                S.op(ACT, lambda e, t=t: e.activation(out=xn[:, t, :], in_=src_tiles[t], func=AF.Square,
                                                      accum_out=ssq[:, t:t + 1]),
                     reads=[src_bufs[t]], writes=[Bxn[t], Bst[t]])
                rstd_from_ssq(rs[:, t:t + 1], ssq[:, t:t + 1], tmp[:, t:t + 1], [Bst[t]], [Bst[t]])
                S.op(ACT, lambda e, t=t: e.activation(out=xn[:, t, :], in_=src_tiles[t], func=AF.Copy,
                                                      scale=rs[:, t:t + 1]),
                     reads=[src_bufs[t], Bst[t]], writes=[Bxn[t]])
            for c in range(KC):
                pt = PT[c % 2]
                for t in range(nt):
                    S.op(PE, lambda e, t=t, c=c, pt=pt: e.transpose(out=pt[:, t * 128:(t + 1) * 128],
                                                                   in_=xn[:, t, c * 128:(c + 1) * 128], identity=identb[:]),
                         reads=[Bxn[t], B_const], writes=[PTb[c % 2]])
                if c % 2 == 0:
                    S.op(ACT, lambda e, c=c, pt=pt: e.activation(out=dst_fn(c), in_=pt[:, 0:nt * 128], func=AF.Identity,
                                                                 scale=sc_t[:, c:c + 1], bias=bias_ap_fn(c)),
                         reads=[PTb[c % 2], B_mod], writes=dst_bufs)
                else:
                    S.op(DVE, lambda e, c=c, pt=pt: e.tensor_scalar(out=dst_fn(c), in0=pt[:, 0:nt * 128],
                                                                    scalar1=sc_t[:, c:c + 1], scalar2=bias_ap_fn(c),
                                                                    op0=ALU.mult, op1=ALU.add),
                         reads=[PTb[c % 2], B_mod], writes=dst_bufs)

        with Scope(arena) as st:
            Wkv = sb(st, "Wkv", [128, 16, 2048], BF16)
            Wki = sb(st, "Wki", [128, 16, 128], BF16)
            Bw = Buf()
            for kc4 in range(8):
                S.dma(POOL, lambda e, kc4=kc4: e.dma_start(out=Wkv[:, kc4 * 2:kc4 * 2 + 2, :],
                                                           in_=wv_in[:, kc4 * 2:kc4 * 2 + 2, O_K:O_K + 2048]), writes=[Bw])
            for hlf in range(2):
                S.dma(POOL, lambda e, hlf=hlf: e.dma_start(out=Wki[:, :, hlf * 64:(hlf + 1) * 64],
                                                           in_=wv_in[:, :, O_KI:O_KI + 64]), writes=[Bw])
            xt = [sb(st, f"xt{i}", [128, D], F32) for i in range(2)]
            Bxt = [Buf(), Buf()]
            xn = sb(st, "xn", [128, 4, D], BF16)
            Bxn = [Buf() for _ in range(4)]
            ssq = sb(st, "ssq", [128, 4], F32)
            rs = sb(st, "rs", [128, 4], F32)
            tmp = sb(st, "tmpv", [128, 4], F32)
            Bst = [Buf() for _ in range(4)]
            hT_tmp = sb(st, "hT_tmp", [128, 16, 512], BF16)
            B_htmp = Buf()
            kT_g = sb(st, "kT_g", [128, 8, 512], BF16)
            B_kTg = Buf()
            v_g = sb(st, "v_g", [128, 4, 8, 160], BF16)
            B_vg = Buf()
            S.op(POOL, lambda e: e.memset(v_g[:], 0.0), writes=[B_vg])
            for t_ in range(4):
                S.op(POOL, lambda e, t_=t_: e.memset(v_g[:, t_, :, 64:65], 1.0), writes=[B_vg])
            B_kTd = Buf("kTd")
            B_vd = Buf("vd")
            was = [sb(st, f"was{i}", [128, 16, 128], BF16) for i in range(2)]
            Bwas = [Buf(), Buf()]

            def mod_rest(m):
                w_t, Bwt = was[m % 2], Bwas[m % 2]
                S.dma(POOL, lambda e: e.dma_start(out=w_t[:], in_=wv_ada[:, :, m * 128:(m + 1) * 128]), writes=[Bwt])
                for kc in range(KC):
                    S.op(PE, lambda e, kc=kc: e.matmul(PB[5][:, m:m + 1], lhsT=w_t[:, kc, :], rhs=cb[:, kc:kc + 1],
                                                       start=(kc == 0), stop=(kc == KC - 1)),
                         reads=[Bwt, Bc], writes=[PBb[5]])
            def norm_group(g):
                for t in range(4):
                    xb = xt[t % 2]
                    r0 = g * 512 + t * 128
                    S.dma(SP, lambda e, xb=xb, r0=r0: e.dma_start(out=xb[:], in_=xctx[r0:r0 + 128, :]),
                          writes=[Bxt[t % 2]])
                    S.op(ACT, lambda e, t=t, xb=xb: e.activation(out=xn[:, t, :], in_=xb[:], func=AF.Square,
                                                                 accum_out=ssq[:, t:t + 1]),
                         reads=[Bxt[t % 2]], writes=[Bxn[t], Bst[t]])
                    rstd_from_ssq(rs[:, t:t + 1], ssq[:, t:t + 1], tmp[:, t:t + 1], [Bst[t]], [Bst[t]])
                    S.op(ACT, lambda e, t=t, xb=xb: e.activation(out=xn[:, t, :], in_=xb[:], func=AF.Copy,
                                                                 scale=rs[:, t:t + 1]),
                         reads=[Bxt[t % 2], Bst[t]], writes=[Bxn[t]])
            norm_group(0)
            for g in range(8):
                own = g >= 6
                if own:
                    hsl = lambda c, g=g: hT_own[:, c, (g - 6) * 512:(g - 5) * 512]
                    hb = B_hown[g - 6]
                else:
                    hsl = lambda c: hT_tmp[:, c, :]
                    hb = B_htmp
                for c in range(KC):
                    pt = PT[c % 2]
                    for t in range(4):
                        S.op(PE, lambda e, t=t, c=c, pt=pt: e.transpose(out=pt[:, t * 128:(t + 1) * 128],
                                                                       in_=xn[:, t, c * 128:(c + 1) * 128],
                                                                       identity=identb[:]),
                             reads=[Bxn[t], B_const], writes=[PTb[c % 2]])
                    if c % 2 == 0:
                        S.op(ACT, lambda e, c=c, pt=pt, hsl=hsl: e.activation(out=hsl(c), in_=pt[:, 0:512], func=AF.Identity,
                                                                             scale=s1[:, c:c + 1], bias=modT[:, c:c + 1]),
                             reads=[PTb[c % 2], B_mod], writes=[hb])
                    else:
                        S.op(DVE, lambda e, c=c, pt=pt, hsl=hsl: e.tensor_scalar(out=hsl(c), in0=pt[:, 0:512],
                                                                                scalar1=s1[:, c:c + 1], scalar2=modT[:, c:c + 1],
                                                                                op0=ALU.mult, op1=ALU.add),
                             reads=[PTb[c % 2], B_mod], writes=[hb])
                if g + 1 < 8:
                    norm_group(g + 1)
                if g == 5:
                    S.op(DVE, lambda e: e.tensor_copy(out=hT_halo[:], in_=hT_tmp[:, :, 480:512]),
                         reads=[B_htmp], writes=[B_halo])
                for cc in range(8):
                    pb = cc % 2
                    for kc in range(KC):
                        S.op(PE, lambda e, cc=cc, kc=kc, pb=pb, hsl=hsl: e.matmul(
                            PB[pb][:, :], lhsT=Wkv[:, kc, cc * 128:(cc + 1) * 128], rhs=hsl(kc),
                            start=(kc == 0), stop=(kc == KC - 1)), reads=[Bw, hb], writes=[PBb[pb]])
                    if cc % 2 == 0:
                        S.op(ACT, lambda e, cc=cc, pb=pb: e.activation(out=kT_g[:, cc, :], in_=PB[pb][:, :], func=AF.Copy),
                             reads=[PBb[pb]], writes=[B_kTg])
                    else:
                        S.op(DVE, lambda e, cc=cc, pb=pb: e.tensor_copy(out=kT_g[:, cc, :], in_=PB[pb][:, :]),
                             reads=[PBb[pb]], writes=[B_kTg])
                S.dma(SP, lambda e, g=g: e.dma_start(out=kT_d[:, :, g * 512:(g + 1) * 512].rearrange("c p n -> p c n"),
                                                     in_=kT_g[:]), reads=[B_kTg], writes=[B_kTd])
                for kc in range(KC):
                    S.op(PE, lambda e, kc=kc, hsl=hsl: e.matmul(PB[2][:, :], lhsT=Wki[:, kc, :], rhs=hsl(kc),
                                                                start=(kc == 0), stop=(kc == KC - 1)),
                         reads=[Bw, hb], writes=[PBb[2]])
                S.op(ACT, lambda e, g=g: e.activation(out=kiT[:, g * 512:(g + 1) * 512], in_=PB[2][:, :], func=AF.Copy),
                     reads=[PBb[2]], writes=[B_kiT])
                for t in range(4):
                    for nb in range(2):
                        pb = 3 + (t * 2 + nb) % 2
                        for kc in range(KC):
                            S.op(PE, lambda e, t=t, nb=nb, kc=kc, pb=pb, hsl=hsl: e.matmul(
                                PB[pb][:, :], lhsT=hsl(kc)[:, t * 128:(t + 1) * 128],
                                rhs=Wkv[:, kc, 1024 + nb * 512:1024 + (nb + 1) * 512],
                                start=(kc == 0), stop=(kc == KC - 1)), reads=[Bw, hb], writes=[PBb[pb]])
                        srcv = PB[pb][:, :].rearrange("p (r e d) -> p r e d", e=2, d=64)
                        dst_e = v_g[:, t, nb * 4:(nb + 1) * 4, 0:64]
                        dst_o = v_g[:, t, nb * 4:(nb + 1) * 4, 96:160]
                        if pb == 3:
                            S.op(ACT, lambda e, dst_e=dst_e, srcv=srcv: e.activation(out=dst_e, in_=srcv[:, :, 0, :], func=AF.Copy),
                                 reads=[PBb[pb]], writes=[B_vg])
                            S.op(ACT, lambda e, dst_o=dst_o, srcv=srcv: e.activation(out=dst_o, in_=srcv[:, :, 1, :], func=AF.Copy),
                                 reads=[PBb[pb]], writes=[B_vg])
                        else:
                            S.op(DVE, lambda e, dst_e=dst_e, srcv=srcv: e.tensor_copy(out=dst_e, in_=srcv[:, :, 0, :]),
                                 reads=[PBb[pb]], writes=[B_vg])
                            S.op(DVE, lambda e, dst_o=dst_o, srcv=srcv: e.tensor_copy(out=dst_o, in_=srcv[:, :, 1, :]),
                                 reads=[PBb[pb]], writes=[B_vg])
                S.dma(SP, lambda e, g=g: e.dma_start(
                    out=v_d[g * 512:(g + 1) * 512, :].rearrange("(t p) n -> p t n", p=128),
                    in_=v_g[:].rearrange("p t h d -> p t (h d)")), reads=[B_vg], writes=[B_vd])
                for m_ in range(32 + g * 8, 32 + (g + 1) * 8):
                    mod_rest(m_)
            S.op(DVE, lambda e: e.tensor_tensor(out=modT[:, 32:96], in0=PB[5][:, 32:96], in1=badd[:, 32:96], op=ALU.add),
                 reads=[PBb[5], Bc, B_mod], writes=[B_mod])
            S.op(DVE, lambda e: e.scalar_tensor_tensor(out=s2[:], in0=modT[:, 64:80], scalar=1.0, in1=smallv[:, 32:48],
                                                       op0=ALU.add, op1=ALU.mult), reads=[B_mod, B_const], writes=[B_mod])
            S.op(DVE, lambda e: e.tensor_tensor(out=smallv[:, 64:80], in0=modT[:, 32:48], in1=smallv[:, 16:32], op=ALU.mult),
                 reads=[B_mod, B_const], writes=[B_mod])
            S.op(DVE, lambda e: e.tensor_tensor(out=smallv[:, 80:96], in0=modT[:, 80:96], in1=smallv[:, 48:64], op=ALU.mult),
                 reads=[B_mod, B_const], writes=[B_mod])
            B_gsc = Buf("gsc")
            for i in range(2):
                S.dma(SP, lambda e, i=i: e.dma_start(out=gsc_d[i].rearrange("(c p) -> p c", p=128),
                                                     in_=smallv[:, 64 + 16 * i:80 + 16 * i], allow_slow_non_contiguous=True),
                      reads=[B_mod], writes=[B_gsc])
            tap("modT", modT[:], [128, 96], F32, [B_mod])
            tap("hT_own", hT_own[:], [128, 16, NOWN], BF16, B_hown)
            tap("kiT", kiT[:], [128, NK], BF16, [B_kiT])
            S.barrier()
            if limit == 2:
                raise _Stop()

        def proj_fm(st, wsrc3, col0, nchunks, segs, consume, Bw2, wblk, seg_reads):
            nblk = (nchunks + 3) // 4
            for blk in range(nblk):
                w_t = wblk[blk % 2]
                ncol = min(4, nchunks - blk * 4) * 128
                for q4 in range(2):
                    S.dma(POOL, lambda e, w_t=w_t, q4=q4, blk=blk, ncol=ncol: e.dma_start(
                        out=w_t[:, q4 * 8:(q4 + 1) * 8, 0:ncol],
                        in_=wsrc3[:, q4 * 8:(q4 + 1) * 8, col0 + blk * 512:col0 + blk * 512 + ncol]),
                        writes=[Bw2[blk % 2]])
                for cc in range(ncol // 128):
                    ch = blk * 4 + cc
                    for si, (rhs_fn, wd) in enumerate(segs):
                        pbi = (ch * len(segs) + si) % 2
                        for kc in range(KC):
                            S.op(PE, lambda e, w_t=w_t, cc=cc, kc=kc, pbi=pbi, rhs_fn=rhs_fn, wd=wd: e.matmul(
                                PB[pbi][:, 0:wd], lhsT=w_t[:, kc, cc * 128:(cc + 1) * 128], rhs=rhs_fn(kc),
                                start=(kc == 0), stop=(kc == KC - 1)),
                                reads=[Bw2[blk % 2]] + seg_reads, writes=[PBb[pbi]])
                        consume(ch, si, PB[pbi], PBb[pbi])

        own_segs = [(lambda kc: hT_own[:, kc, 0:512], 512), (lambda kc: hT_own[:, kc, 512:1024], 512)]

        att_stack = Scope(arena)
        attT = sb(att_stack, "attT", [128, 8, NOWN], BF16)
        B_att = Buf("attT")
        with Scope(arena) as st:
            qT = sb(st, "qT", [128, 8, NOWN], BF16)
            qiT = sb(st, "qiT", [128, 8, NOWN], BF16)
            B_q = Buf()
            B_qi = Buf()
            wsc = sb(st, "wsc", [128, 8, 16], F32)
            wabs = sb(st, "wabs", [128, 8, 16], F32)
            wsgn = sb(st, "wsgn", [128, 8, 16], F32)
            B_wi = Buf()
            st2 = Scope(arena)
            wblk = [sb(st2, f"wblk{i}", [128, 16, 512], BF16) for i in range(2)]
            Bw2 = [Buf(), Buf()]
            Wwi = sb(st2, "Wwi", [128, 16, 16], BF16)
            B_wwi = Buf()

            def cons_q(dst, Bd):
                def f(ch, si, pb, pbb):
                    eng = ACT if (ch + si) % 2 == 0 else DVE
                    if eng is ACT:
                        S.op(ACT, lambda e: e.activation(out=dst[:, ch, si * 512:(si + 1) * 512], in_=pb[:, :], func=AF.Copy),
                             reads=[pbb], writes=[Bd])
                    else:
                        S.op(DVE, lambda e: e.tensor_copy(out=dst[:, ch, si * 512:(si + 1) * 512], in_=pb[:, :]),
                             reads=[pbb], writes=[Bd])
                return f
            proj_fm(st, wv_in, O_Q, 8, own_segs, cons_q(qT, B_q), Bw2, wblk, B_hown)
            proj_fm(st, wv_in, O_QI, 8, own_segs, cons_q(qiT, B_qi), Bw2, wblk, B_hown)
            S.dma(POOL, lambda e: e.dma_start(out=Wwi[:], in_=wv_in[:, :, O_WI:O_WI + 16]), writes=[B_wwi])
            for t in range(8):
                for kc in range(KC):
                    S.op(PE, lambda e, t=t, kc=kc: e.matmul(PB[2][:, 0:16], lhsT=hT_own[:, kc, t * 128:(t + 1) * 128],
                                                            rhs=Wwi[:, kc, :], start=(kc == 0), stop=(kc == KC - 1)),
                         reads=[B_wwi] + B_hown, writes=[PBb[2]])
                S.op(ACT, lambda e, t=t: e.activation(out=wsc[:, t, :], in_=PB[2][:, 0:16], func=AF.Copy, scale=IDX_SCALE),
                     reads=[PBb[2]], writes=[B_wi])
            S.op(ACT, lambda e: e.activation(out=wabs[:], in_=wsc[:], func=AF.Abs), reads=[B_wi], writes=[B_wi])
            S.op(ACT, lambda e: e.activation(out=wsgn[:], in_=wsc[:], func=AF.Sign), reads=[B_wi], writes=[B_wi])
            tap("qT", qT[:], [128, 8, NOWN], BF16, [B_q])
            tap("wsc", wsc[:], [128, 8, 16], F32, [B_wi])
            S.barrier()
            if limit == 3:
                raise _Stop()
            st2.close()

            scb2 = [sb(st, f"scb{i}", [128, NK], F32) for i in range(2)]
            B_sc2 = [Buf(), Buf()]
            mk = sb(st, "mk", [128, NK], BF16)
            B_mk = Buf()
            NQ = SBQ * 128
            maskT = sb(st, "maskT", [128, 32, NQ], BF16)
            B_maskT = Buf()
            dg2 = [sb(st, f"dg{i}", [128, 16, 128], BF16) for i in range(2)]
            B_dg2 = [Buf(), Buf()]
            NRR = 6
            rr = [sb(st, f"rr{i}", [128, 512], BF16) for i in range(NRR)]
            B_rr = [Buf() for _ in range(NRR)]
            kTc = sb(st, "kTc", [128, NK], BF16)
            B_kTc = [Buf(), Buf()]
            vc = sb(st, "vc", [128, 32, 160], BF16)
            B_vc = [Buf(), Buf()]
            NET = 4
            et = [sb(st, f"et{i}", [128, 2, NQ], BF16) for i in range(NET)]
            B_et = [Buf() for _ in range(NET)]
            ptl = [sb(st, f"ptl{i}", [128, 2, NQ], BF16) for i in range(NET)]
            B_ptl = [Buf() for _ in range(NET)]
            qpad = sb(st, "qpad", [128, 8, 2, NQ], BF16)
            B_qpad = Buf()
            S.op(POOL, lambda e: e.memset(qpad[:], 0.0), writes=[B_qpad])
            sv = sb(st, "sv", [128, 64], F32)
            B_sv = Buf()
            wtab = sb(st, "wtab", [128, NITER + 1], F32)
            rd = sb(st, "rd", [128, NQ], F32)
            B_rd = Buf()
            rbc = sb(st, "rbc", [128, NQ], F32)
            B_rbc = Buf()
            thr_dbg = sb(st, "thr_dbg", [128, 8], F32)
            B_thr = Buf()
            qk_slot = [PB[2][:, 0:2 * NQ], PB[3][:, 0:2 * NQ], PB[4][:, 0:2 * NQ],
                       PT[0][:, :].bitcast(F32)[:, 0:2 * NQ], PT[1][:, :].bitcast(F32)[:, 0:2 * NQ]]
            B_qk = [PBb[2], PBb[3], PBb[4], PTb[0], PTb[1]]
            NQK = len(qk_slot)

            def run_pipeline(nsteps, stages, lags):
                for tick in range(nsteps + max(lags)):
                    for f, lg in zip(stages, lags):
                        i = tick - lg
                        if 0 <= i < nsteps:
                            f(i)

            def indexer(qb):
                scb, B_sc = scb2[qb % 2], B_sc2[qb % 2]
                dg, B_dg = dg2[qb % 2], B_dg2[qb % 2]
                nkt = 24 + qb + 1
                N = nkt * 128
                q0 = qb * 128
                for h in range(16):
                    S.op(DVE if BIS == "dgdve" else POOL, lambda e, h=h: e.tensor_scalar(out=dg[:, h, :], in0=identb[:],
                                                              scalar1=wsgn[:, qb, h:h + 1], scalar2=None, op0=ALU.mult),
                         reads=[B_const, B_wi], writes=[B_dg])
                ngrp = (N + 511) // 512
                steps = [(n, h) for n in range(ngrp) for h in range(16)]

                def stA(i):
                    n, h = steps[i]
                    wdt = min(512, N - n * 512)
                    hb = (h % 2) * 64
                    pbi = i % 4
                    S.op(PE, lambda e: e.matmul(PB[pbi][:, 0:wdt], lhsT=qiT[hb:hb + 64, h // 2, q0:q0 + 128],
                                                rhs=kiT[hb:hb + 64, n * 512:n * 512 + wdt], start=True, stop=True),
                         reads=[B_qi, B_kiT], writes=[PBb[pbi]])

                def stB(i):
                    n, h = steps[i]
                    wdt = min(512, N - n * 512)
                    pbi = i % 4
                    r_t, B_r = rr[i % NRR], B_rr[i % NRR]
                    S.op(ACT, lambda e: e.activation(out=r_t[:, 0:wdt], in_=PB[pbi][:, 0:wdt], func=AF.Relu,
                                                     scale=wabs[:, qb, h:h + 1]),
                         reads=[PBb[pbi], B_wi], writes=[B_r])

                def stC(i):
                    n, h = steps[i]
                    wdt = min(512, N - n * 512)
                    r_t, B_r = rr[i % NRR], B_rr[i % NRR]
                    psc, pscb = PB[4 + n % 2], PBb[4 + n % 2]
                    S.op(PE, lambda e: e.matmul(psc[:, 0:wdt], lhsT=dg[:, h, :], rhs=r_t[:, 0:wdt], start=(h == 0), stop=False),
                         reads=[B_dg, B_r], writes=[pscb])
                    if h == 15:
                        last = (n == ngrp - 1)
                        sgm = min(n // 2, 3)
                        S.op(PE, lambda e: e.matmul(psc[:, 0:wdt], lhsT=kneg_b[0:1, sgm * 128:(sgm + 1) * 128],
                                                    rhs=ones512[0:1, 0:wdt], start=False, stop=(not last)),
                             reads=[B_const], writes=[pscb])
                        if last:
                            S.op(PE, lambda e: e.matmul(psc[:, wdt - 128:wdt], lhsT=diag_b[0:1, 0:128], rhs=diag_b[0:1, 128:256],
                                                        start=False, stop=True), reads=[B_const], writes=[pscb])
                        S.op(ACT, lambda e: e.activation(out=scb[:, n * 512:n * 512 + wdt], in_=psc[:, 0:wdt], func=AF.Copy),
                             reads=[pscb], writes=[B_sc])
                run_pipeline(len(steps), [stA, stB, stC], [0, 0, 3])

            def search(qb):
                scb, B_sc = scb2[qb % 2], B_sc2[qb % 2]
                nkt = 24 + qb + 1
                N = nkt * 128
                S.op(DVE, lambda e: e.tensor_reduce(out=sv[:, 0:1], in_=scb[:, 0:N], axis=AX.X, op=ALU.max),
                     reads=[B_sc], writes=[B_sv])
                S.op(DVE, lambda e: e.tensor_reduce(out=sv[:, 8:11], in_=scb[:, 0:NCTX].rearrange("p (s n) -> p s n", s=3),
                                                    axis=AX.X, op=ALU.min), reads=[B_sc], writes=[B_sv])
                if qb > 0:
                    S.op(DVE, lambda e: e.tensor_reduce(out=sv[:, 11:12], in_=scb[:, NCTX:NCTX + qb * 128],
                                                        axis=AX.X, op=ALU.min), reads=[B_sc], writes=[B_sv])
                else:
                    S.op(DVE, lambda e: e.memset(sv[:, 11:12], 3.0e38), writes=[B_sv])
                S.op(DVE, lambda e: e.tensor_reduce(out=sv[:, 12:14],
                                                    in_=scb[:, N - 128:N].rearrange("p (s n) -> p s n", s=2),
                                                    axis=AX.X, op=ALU.min), reads=[B_sc], writes=[B_sv])
                S.op(DVE, lambda e: e.tensor_tensor(out=sv[:, 16:22], in0=sv[:, 8:14], in1=pen[:], op=ALU.add),
                     reads=[B_sv, B_const], writes=[B_sv])
                S.op(DVE, lambda e: e.tensor_reduce(out=sv[:, 1:2], in_=sv[:, 16:22], axis=AX.X, op=ALU.min),
                     reads=[B_sv], writes=[B_sv])
                S.op(DVE, lambda e: e.tensor_tensor(out=sv[:, 2:3], in0=sv[:, 0:1], in1=sv[:, 1:2], op=ALU.subtract),
                     reads=[B_sv], writes=[B_sv])
                S.op(DVE, lambda e: e.tensor_scalar(out=sv[:, 2:3], in0=sv[:, 2:3], scalar1=1.002, scalar2=1e-6,
                                                    op0=ALU.mult, op1=ALU.add), reads=[B_sv], writes=[B_sv])
                S.op(DVE, lambda e: e.tensor_scalar(out=wtab[:], in0=pow2[:], scalar1=sv[:, 2:3], scalar2=None,
                                                    op0=ALU.mult), reads=[B_sv, B_const], writes=[B_sv])
                S.op(DVE, lambda e: e.tensor_tensor(out=sv[:, 1:2], in0=sv[:, 0:1], in1=sv[:, 2:3], op=ALU.subtract),
                     reads=[B_sv], writes=[B_sv])
                S.op(DVE, lambda e: e.tensor_tensor(out=sv[:, 3:4], in0=sv[:, 1:2], in1=wtab[:, 0:1], op=ALU.add),
                     reads=[B_sv], writes=[B_sv])
                for k in range(NITER):
                    S.op(DVE, lambda e: e.tensor_scalar(out=mk[:, 0:N], in0=scb[:, 0:N], scalar1=sv[:, 3:4],
                                                        scalar2=0.0, op0=ALU.is_ge, op1=ALU.add, accum_out=sv[:, 4:5]),
                         reads=[B_sc, B_sv], writes=[B_mk, B_sv])
                    S.op(DVE, lambda e: e.tensor_scalar(out=sv[:, 5:6], in0=sv[:, 4:5], scalar1=255.5, scalar2=0.5,
                                                        op0=ALU.is_ge, op1=ALU.subtract), reads=[B_sv], writes=[B_sv])
                    S.op(DVE, lambda e, k=k: e.scalar_tensor_tensor(out=sv[:, 3:4], in0=sv[:, 5:6], scalar=wtab[:, k:k + 1],
                                                                    in1=sv[:, 3:4], op0=ALU.mult, op1=ALU.add),
                         reads=[B_sv], writes=[B_sv])
                S.op(DVE, lambda e: e.tensor_tensor(out=sv[:, 6:7], in0=sv[:, 3:4], in1=wtab[:, NITER:NITER + 1],
                                                    op=ALU.subtract), reads=[B_sv], writes=[B_sv])
                S.op(DVE, lambda e: e.tensor_copy(out=thr_dbg[:, qb:qb + 1], in_=sv[:, 6:7]),
                     reads=[B_sv], writes=[B_thr])
                S.op(DVE, lambda e: e.tensor_scalar(out=mk[:, 0:N], in0=scb[:, 0:N], scalar1=sv[:, 6:7],
                                                    scalar2=None, op0=ALU.is_ge), reads=[B_sc, B_sv], writes=[B_mk])
                if qb == 7:
                    tap("scb", scb[:], [128, NK], F32, [B_sc])

            def mask_transposes(qb, i):
                nkt = 24 + qb + 1
                for kt0 in range(0, nkt, 8):
                    nk8 = min(8, nkt - kt0)
                    pt = PT[(kt0 // 8) % 2]
                    ptb = PTb[(kt0 // 8) % 2]
                    for a_ in range(nk8):
                        S.op(PE, lambda e, a_=a_: e.transpose(
                            out=pt[:, a_ * 128:(a_ + 1) * 128], in_=mk[:, (kt0 + a_) * 128:(kt0 + a_ + 1) * 128],
                            identity=identb[:]), reads=[B_mk, B_const], writes=[ptb])
                    S.op(ACT, lambda e: e.activation(
                        out=maskT[:, kt0:kt0 + nk8, i * 128:(i + 1) * 128],
                        in_=pt[:, 0:nk8 * 128].rearrange("p (a q) -> p a q", q=128), func=AF.Copy),
                        reads=[ptb], writes=[B_maskT])

            def attention(s):
                nkt_s = 24 + (s + 1) * SBQ
                qs0 = s * NQ
                S.op(POOL, lambda e: e.tensor_copy(out=qpad[0:64, :, 0, :], in_=qT[0:64, :, qs0:qs0 + NQ]),
                     reads=[B_q], writes=[B_qpad])
                S.op(POOL, lambda e: e.tensor_copy(out=qpad[64:128, :, 1, :], in_=qT[64:128, :, qs0:qs0 + NQ]),
                     reads=[B_q], writes=[B_qpad])
                steps = [(hc, kt) for hc in range(8) for kt in range(nkt_s)]
                kv_view = v_d.rearrange("(kt p) n -> p kt n", p=128)

                def load_kv(hc, half):
                    k0, k1 = (0, 16) if half == 0 else (16, 32)
                    S.dma(SP, lambda e: e.dma_start(out=kTc[:, k0 * 128:k1 * 128], in_=kT_d[hc][:, k0 * 128:k1 * 128]),
                          reads=[B_kTd], writes=[B_kTc[half]])
                    S.dma(SP, lambda e: e.dma_start(out=vc[:, k0:k1, :], in_=kv_view[:, k0:k1, hc * 160:(hc + 1) * 160]),
                          reads=[B_vd], writes=[B_vc[half]])

                def stA(i):
                    hc, kt = steps[i]
                    sl = i % NQK
                    S.op(PE, lambda e: e.matmul(qk_slot[sl], lhsT=kTc[:, kt * 128:(kt + 1) * 128],
                                                rhs=qpad[:, hc, :, :], start=True, stop=True),
                         reads=[B_kTc[kt // 16], B_qpad], writes=[B_qk[sl]])

                def stB(i):
                    sl = i % NQK
                    S.op(ACT, lambda e: e.activation(out=et[i % NET][:, :, :], in_=qk_slot[sl].rearrange("p (a q) -> p a q", a=2),
                                                     func=AF.Exp, scale=0.125),
                         reads=[B_qk[sl]], writes=[B_et[i % NET]])

                def stC(i):
                    hc, kt = steps[i]
                    S.op(DVE, lambda e: e.tensor_tensor(out=ptl[i % NET][:, :, :], in0=et[i % NET][:, :, :],
                                                        in1=maskT[:, kt:kt + 1, :].broadcast_to((128, 2, NQ)), op=ALU.mult),
                         reads=[B_et[i % NET], B_maskT], writes=[B_ptl[i % NET]])

                def stD(i):
                    hc, kt = steps[i]
                    for hh in range(2):
                        S.op(PE, lambda e, hh=hh: e.matmul(PB[hh][0:(65 if hh == 0 else 128), 0:NQ],
                                                           lhsT=(vc[:, kt, 0:65] if hh == 0 else vc[:, kt, 32:160]),
                                                           rhs=ptl[i % NET][:, hh, :], start=(kt == 0), stop=(kt == nkt_s - 1)),
                             reads=[B_vc[kt // 16], B_ptl[i % NET]], writes=[PBb[hh]])
                    if kt == 15 and hc + 1 < 8:
                        load_kv(hc + 1, 0)
                    if kt == nkt_s - 1:
                        if hc + 1 < 8:
                            load_kv(hc + 1, 1)
                        normalize(hc, 0)
                        normalize(hc, 1)

                def normalize(hc, hh):
                    hb = hh * 64
                    po, pob = PB[hh], PBb[hh]
                    dp = 64 if hh == 0 else 32
                    S.op(DVE, lambda e: e.reciprocal(out=rd[dp:dp + 1, :], in_=po[dp:dp + 1, 0:NQ]),
                         reads=[pob], writes=[B_rd])
                    S.op(PE, lambda e: e.matmul(PB[5][:, 0:NQ], lhsT=onesf[dp:dp + 1, 0:128], rhs=rd[dp:dp + 1, :],
                                                start=True, stop=True), reads=[B_rd, B_const], writes=[PBb[5]])
                    S.op(ACT, lambda e: e.activation(out=rbc[:, :], in_=PB[5][:, 0:NQ], func=AF.Copy),
                         reads=[PBb[5]], writes=[B_rbc])
                    S.op(DVE, lambda e: e.tensor_tensor(out=attT[hb:hb + 64, hc, qs0:qs0 + NQ], in0=po[hb:hb + 64, 0:NQ],
                                                        in1=rbc[hb:hb + 64, :], op=ALU.mult),
                         reads=[pob, B_rbc], writes=[B_att])
                load_kv(0, 0)
                load_kv(0, 1)
                run_pipeline(len(steps), [stA, stB, stC, stD], [0, 0, 0, 3])

            nsb = 8 // SBQ
            indexer(0)
            for s in range(nsb):
                S.op(POOL, lambda e: e.memset(maskT[:], 0.0), writes=[B_maskT])
                for i in range(SBQ):
                    qb = s * SBQ + i
                    search(qb)
                    if qb + 1 < 8:
                        indexer(qb + 1)
                    mask_transposes(qb, i)
                attention(s)
            tap("thr", thr_dbg[:], [128, 8], F32, [B_thr])
            tap("attT", attT[:], [128, 8, NOWN], BF16, [B_att])
            S.barrier()
            if limit == 4:
                raise _Stop()

        uact = sb(att_stack, "uact", [128, 8, NOWN], BF16)
        B_uact = Buf("uact")
        with Scope(arena) as st:
            uT = sb(st, "uT", [128, 8, 32 + NOWN], BF16)
            B_uT = Buf()
            st2 = Scope(arena)
            sg = sb(st2, "sg", [128, 8, 32 + NOWN], BF16)
            B_sg = Buf()
            wblk = [sb(st2, f"wblkb{i}", [128, 16, 512], BF16) for i in range(2)]
            Bw2 = [Buf(), Buf()]
            glu_segs = [(lambda kc: hT_halo[:, kc, :], 32)] + own_segs
            seg_off = [0, 32, 32 + 512]

            def cons_sg(ch, si, pb, pbb):
                wd = glu_segs[si][1]
                S.op(ACT, lambda e: e.activation(out=sg[:, ch, seg_off[si]:seg_off[si] + wd], in_=pb[:, 0:wd], func=AF.Sigmoid),
                     reads=[pbb], writes=[B_sg])

            def cons_u(ch, si, pb, pbb):
                wd = glu_segs[si][1]
                S.op(DVE, lambda e: e.tensor_tensor(out=uT[:, ch, seg_off[si]:seg_off[si] + wd], in0=pb[:, 0:wd],
                                                    in1=sg[:, ch, seg_off[si]:seg_off[si] + wd], op=ALU.mult),
                     reads=[pbb, B_sg], writes=[B_uT])
            proj_fm(st, wv_in, O_GLU_G, 8, glu_segs, cons_sg, Bw2, wblk, B_hown + [B_halo])
            proj_fm(st, wv_in, O_GLU_A, 8, glu_segs, cons_u, Bw2, wblk, B_hown + [B_halo])
            S.op(DVE, lambda e: e.tensor_scalar(out=uT[:, :, 0:32], in0=uT[:, :, 0:32], scalar1=halov[:, 0:1], scalar2=None,
                                                op0=ALU.mult), reads=[B_uT, B_const], writes=[B_uT])
            tap("uT", uT[:], [128, 8, 32 + NOWN], BF16, [B_uT])
            S.barrier()
            if limit == 5:
                raise _Stop()
            st2.close()
            yb = sb(st, "yb", [128, 8, NOWN], F32)
            ysq = sb(st, "ysq", [128, 8, NOWN], F32)
            B_y = Buf()
            dgc = [sb(st, f"dgc{i}", [128, 31, 128], BF16) for i in range(2)]
            B_dgc = [Buf(), Buf()]
            mu = sb(st, "mu", [128, 512], F32)
            var = sb(st, "var", [128, 512], F32)
            rstd = sb(st, "rstd", [128, 512], F32)
            B_ln = Buf()
            zt = sb(st, "zt", [128, 512], F32)
            B_z = Buf()
            for ch in range(8):
                dg_t, Bdg = dgc[ch % 2], B_dgc[ch % 2]
                for tp in range(31):
                    engd = DVE if tp % 2 == 0 else POOL
                    S.op(engd, lambda e, tp=tp: e.tensor_scalar(out=dg_t[:, tp, :], in0=identb[:],
                                                                scalar1=wdw[:, ch * 31 + tp:ch * 31 + tp + 1], scalar2=None,
                                                                op0=ALU.mult), reads=[B_const], writes=[Bdg])
                for g in range(2):
                    off = g * 512 + 2
                    pbi = 2 + (ch * 2 + g) % 2
                    for tp in range(31):
                        S.op(PE, lambda e, tp=tp: e.matmul(PB[pbi][:, :], lhsT=dg_t[:, tp, :], rhs=uT[:, ch, off + tp:off + tp + 512],
                                                           start=(tp == 0), stop=(tp == 30)),
                             reads=[Bdg, B_uT], writes=[PBb[pbi]])
                    S.op(ACT, lambda e: e.activation(out=yb[:, ch, g * 512:(g + 1) * 512], in_=PB[pbi][:, :], func=AF.Identity,
                                                     bias=convv[:, ch:ch + 1], scale=1.0),
                         reads=[PBb[pbi], B_const], writes=[B_y])
                    S.op(ACT, lambda e: e.activation(out=ysq[:, ch, g * 512:(g + 1) * 512], in_=PB[pbi][:, :], func=AF.Square,
                                                     bias=convv[:, ch:ch + 1], scale=1.0),
                         reads=[PBb[pbi], B_const], writes=[B_y])
            for g in range(2):
                gs = slice(g * 512, (g + 1) * 512)
                for ch in range(8):
                    S.op(PE, lambda e, ch=ch: e.matmul(PB[0][:, :], lhsT=onesf[:], rhs=yb[:, ch, gs], start=(ch == 0),
                                                       stop=(ch == 7)), reads=[B_y, B_const], writes=[PBb[0]])
                for ch in range(8):
                    S.op(PE, lambda e, ch=ch: e.matmul(PB[1][:, :], lhsT=onesf[:], rhs=ysq[:, ch, gs], start=(ch == 0),
                                                       stop=(ch == 7)), reads=[B_y, B_const], writes=[PBb[1]])
                S.op(ACT, lambda e: e.activation(out=mu[:], in_=PB[0][:, :], func=AF.Copy, scale=1.0 / 1024),
                     reads=[PBb[0]], writes=[B_ln])
                S.op(DVE, lambda e: e.tensor_tensor(out=var[:], in0=mu[:], in1=mu[:], op=ALU.mult), reads=[B_ln], writes=[B_ln])
                S.op(DVE, lambda e: e.scalar_tensor_tensor(out=var[:], in0=PB[1][:, :], scalar=1.0 / 1024, in1=var[:],
                                                           op0=ALU.mult, op1=ALU.subtract), reads=[PBb[1], B_ln], writes=[B_ln])
                S.op(ACT, lambda e: e.activation(out=rstd[:], in_=var[:], func=AF.Sqrt, bias=epsc[:, 0:1], scale=1.0),
                     reads=[B_ln, B_const], writes=[B_ln])
                S.op(DVE, lambda e: e.reciprocal(out=rstd[:], in_=rstd[:]), reads=[B_ln], writes=[B_ln])
                for ch in range(8):
                    S.op(DVE, lambda e, ch=ch: e.tensor_tensor(out=zt[:], in0=yb[:, ch, gs], in1=mu[:], op=ALU.subtract),
                         reads=[B_y, B_ln], writes=[B_z])
                    S.op(DVE, lambda e: e.tensor_tensor(out=zt[:], in0=zt[:], in1=rstd[:], op=ALU.mult),
                         reads=[B_ln], writes=[B_z])
                    S.op(ACT, lambda e, ch=ch: e.activation(out=uact[:, ch, gs], in_=zt[:], func=AF.Silu,
                                                            scale=convv[:, 8 + ch:9 + ch], bias=convv[:, 16 + ch:17 + ch]),
                         reads=[B_z, B_const], writes=[B_uact])
            tap("uact", uact[:], [128, 8, NOWN], BF16, [B_uact])
            S.barrier()
            if limit == 6:
                raise _Stop()

        wv_co = w_co.rearrange("(c p) n -> p c n", p=128)
        wv_ao = w_ao.rearrange("(c p) n -> p c n", p=128)
        wv_o = w_o.rearrange("(c p) n -> p c n", p=128)
        wv_g = w_gate.rearrange("(c p) n -> p c n", p=128)
        wv_u = w_up.rearrange("(c p) n -> p c n", p=128)
        wv_d = w_down.rearrange("(c p) n -> p c n", p=128)
        sc_kiT.close()
        sc_halo.close()
        wslot = [0, 0, 0]

        def wload(slots, Bs, which, dst_view_fn, src_ap, nsplit=2):
            i = wslot[which] % 2
            wslot[which] += 1
            w_t = slots[i]
            n0 = src_ap.shape[1]
            step = (n0 + nsplit - 1) // nsplit
            for a in range(0, n0, step):
                b = min(n0, a + step)
                S.dma(POOL, lambda e, a=a, b=b, w_t=w_t: e.dma_start(out=dst_view_fn(w_t)[:, a:b, :], in_=src_ap[:, a:b, :]),
                      writes=[Bs[i]])
            return w_t, Bs[i]

        sc_mg = Scope(arena)
        mergedT = sb(sc_mg, "mergedT", [128, 16, NOWN], BF16)
        B_mg = Buf()
        with Scope(arena) as st:
            Wa = [sb(st, f"Wa{i}", [128, 16, 512], BF16) for i in range(2)]
            B_Wa = [Buf(), Buf()]
            Wb = [sb(st, f"Wb{i}", [128, 16, 512], BF16) for i in range(2)]
            B_Wb = [Buf(), Buf()]
            tA = sb(st, "tA", [128, 512], F32)
            tB = sb(st, "tB", [128, 512], BF16)
            tC = sb(st, "tC", [128, 512], BF16)
            B_t = [Buf(), Buf(), Buf()]
            for nb in range(4):
                cs = slice(nb * 512, (nb + 1) * 512)
                Wgc, Bgc = wload(Wa, B_Wa, 0, lambda w: w[:, :, :], wv_in[:, :, O_GC + nb * 512:O_GC + (nb + 1) * 512])
                Wga, Bga = wload(Wa, B_Wa, 0, lambda w: w[:, :, :], wv_in[:, :, O_GA + nb * 512:O_GA + (nb + 1) * 512])
                Wco, Bco = wload(Wb, B_Wb, 1, lambda w: w[:, 0:8, :], wv_co[:, :, cs])
                Wao, Bao = wload(Wb, B_Wb, 1, lambda w: w[:, 0:8, :], wv_ao[:, :, cs])
                for g in range(2):
                    gs = slice(g * 512, (g + 1) * 512)
                    for cc in range(4):
                        f = nb * 4 + cc
                        fs = slice(cc * 128, (cc + 1) * 128)
                        for kc in range(KC):
                            S.op(PE, lambda e, kc=kc, fs=fs, Wgc=Wgc, gs=gs: e.matmul(PB[0][:, :], lhsT=Wgc[:, kc, fs], rhs=hT_own[:, kc, gs],
                                                                                   start=(kc == 0), stop=(kc == KC - 1)),
                                 reads=[Bgc] + B_hown, writes=[PBb[0]])
                        for kc in range(KC):
                            S.op(PE, lambda e, kc=kc, fs=fs, Wga=Wga, gs=gs: e.matmul(PB[1][:, :], lhsT=Wga[:, kc, fs], rhs=hT_own[:, kc, gs],
                                                                                   start=(kc == 0), stop=(kc == KC - 1)),
                                 reads=[Bga] + B_hown, writes=[PBb[1]])
                        for kc in range(8):
                            S.op(PE, lambda e, kc=kc, fs=fs, Wco=Wco, gs=gs: e.matmul(PB[2][:, :], lhsT=Wco[:, kc, fs], rhs=uact[:, kc, gs],
                                                                                   start=(kc == 0), stop=(kc == 7)),
                                 reads=[Bco, B_uact], writes=[PBb[2]])
                        for kc in range(8):
                            S.op(PE, lambda e, kc=kc, fs=fs, Wao=Wao, gs=gs: e.matmul(PB[3][:, :], lhsT=Wao[:, kc, fs], rhs=attT[:, kc, gs],
                                                                                   start=(kc == 0), stop=(kc == 7)),
                                 reads=[Bao, B_att], writes=[PBb[3]])
                        S.op(ACT, lambda e: e.activation(out=tB[:], in_=PB[0][:, :], func=AF.Sigmoid), reads=[PBb[0]], writes=[B_t[1]])
                        S.op(ACT, lambda e: e.activation(out=tC[:], in_=PB[1][:, :], func=AF.Sigmoid), reads=[PBb[1]], writes=[B_t[2]])
                        S.op(DVE, lambda e: e.tensor_tensor(out=tA[:], in0=PB[2][:, :], in1=tB[:], op=ALU.mult),
                             reads=[PBb[2], B_t[1]], writes=[B_t[0]])
                        S.op(DVE, lambda e: e.tensor_tensor(out=tB[:], in0=PB[3][:, :], in1=tC[:], op=ALU.mult),
                             reads=[PBb[3], B_t[2]], writes=[B_t[1]])
                        S.op(DVE, lambda e, f=f, gs=gs: e.tensor_tensor(out=mergedT[:, f, gs], in0=tA[:], in1=tB[:], op=ALU.add),
                             reads=[B_t[0], B_t[1]], writes=[B_mg])
            tap("mergedT", mergedT[:], [128, 16, NOWN], BF16, [B_mg])
            S.barrier()
            if limit == 7:
                raise _Stop()
        att_stack.close()
        sc_hown.close()

        sc_h2 = Scope(arena)
        h2T = sb(sc_h2, "h2T", [128, 16, NOWN], BF16)
        B_h2 = Buf()
        B_out = Buf("out")
        with Scope(arena) as st:
            mx = sb(st, "mx", [128, 8, D], F32)
            B_mx = [Buf() for _ in range(8)]
            ssp = sb(st, "ssp", [128, 8, 4], F32)
            B_ss = [Buf() for _ in range(8)]
            tC = sb(st, "tC2", [128, 512], BF16)
            B_tc = Buf()
            with Scope(arena) as stw:
                Wa = [sb(stw, f"Wo{i}", [128, 16, 512], BF16) for i in range(2)]
                B_Wa = [Buf(), Buf()]
                for nb in range(4):
                    Wo, Bo = wload(Wa, B_Wa, 0, lambda w: w[:, :, :], wv_o[:, :, nb * 512:(nb + 1) * 512])
                    for t in range(8):
                        pbi = 2 + (t % 4)
                        tok = slice(t * 128, (t + 1) * 128)
                        for kc in range(KC):
                            S.op(PE, lambda e, kc=kc: e.matmul(PB[pbi][:, :], lhsT=mergedT[:, kc, tok], rhs=Wo[:, kc, :],
                                                               start=(kc == 0), stop=(kc == KC - 1)),
                                 reads=[Bo, B_mg], writes=[PBb[pbi]])
                        S.op(ACT, lambda e: e.activation(out=mx[:, t, nb * 512:(nb + 1) * 512], in_=PB[pbi][:, :], func=AF.Copy),
                             reads=[PBb[pbi]], writes=[B_mx[t]])
                        S.op(ACT, lambda e: e.activation(out=tC[:], in_=PB[pbi][:, :], func=AF.Square,
                                                         accum_out=ssp[:, t, nb:nb + 1]),
                             reads=[PBb[pbi]], writes=[B_tc, B_ss[t]])
                S.barrier()
                if limit == 8:
                    raise _Stop()
            with Scope(arena) as stf:
                G1 = sb(stf, "G1", [128, D], F32)
                B_G = Buf()
                S.dma(SP, lambda e: e.dma_start(out=G1[:], in_=gsc_d[0].partition_broadcast(128)), reads=[B_gsc], writes=[B_G])
                ss1_ = sb(stf, "ss1", [128, 8, 16], F32)
                xt = [sb(stf, f"xtb{i}", [128, D], F32) for i in range(2)]
                B_xt = [Buf(), Buf()]
                x1t = [sb(stf, f"x1t{i}", [128, D], F32) for i in range(2)]
                B_x1 = [Buf(), Buf()]
                xn2 = [sb(stf, f"xn2{i}", [128, D], BF16) for i in range(2)]
                B_xn2 = [Buf(), Buf()]
                for t in range(8):
                    ss1 = ss1_[:, t, :]
                    xb = xt[t % 2]
                    x1 = x1t[t % 2]
                    xn = xn2[t % 2]
                    Bxn = B_xn2[t % 2]
                    r0 = NCTX + t * 128
                    o0 = t * 128
                    S.dma(SP, lambda e: e.dma_start(out=xb[:], in_=xctx[r0:r0 + 128, :]), writes=[B_xt[t % 2]])
                    S.op(DVE, lambda e: e.tensor_reduce(out=ss1[:, 0:1], in_=ssp[:, t, :], axis=AX.X, op=ALU.add),
                         reads=[B_ss[t]], writes=[B_ss[t]])
                    rstd_from_ssq(ss1[:, 2:3], ss1[:, 0:1], ss1[:, 1:2], [B_ss[t]], [B_ss[t]])
                    S.op(DVE, lambda e: e.scalar_tensor_tensor(out=mx[:, t, :], in0=mx[:, t, :], scalar=ss1[:, 2:3], in1=G1[:],
                                                               op0=ALU.mult, op1=ALU.mult),
                         reads=[B_ss[t], B_G], writes=[B_mx[t]])
                    S.op(DVE, lambda e: e.tensor_tensor(out=x1[:], in0=mx[:, t, :], in1=xb[:], op=ALU.add),
                         reads=[B_mx[t], B_xt[t % 2]], writes=[B_x1[t % 2]])
                    S.dma(SP, lambda e: e.dma_start(out=out[o0:o0 + 128, :], in_=x1[:]), reads=[B_x1[t % 2]],
                          writes=[B_out])
                    S.op(ACT, lambda e: e.activation(out=xn[:], in_=x1[:], func=AF.Square, accum_out=ss1[:, 4:5]),
                         reads=[B_x1[t % 2]], writes=[Bxn, B_ss[t]])
                    rstd_from_ssq(ss1[:, 6:7], ss1[:, 4:5], ss1[:, 5:6], [B_ss[t]], [B_ss[t]])
                    S.op(ACT, lambda e: e.activation(out=xn[:], in_=x1[:], func=AF.Copy, scale=ss1[:, 6:7]),
                         reads=[B_x1[t % 2], B_ss[t]], writes=[Bxn])
                    for c4 in range(4):
                        pt = PT[c4 % 2]
                        for cc in range(4):
                            c = c4 * 4 + cc
                            S.op(PE, lambda e, c=c, cc=cc: e.transpose(out=pt[:, cc * 128:(cc + 1) * 128], in_=xn[:, c * 128:(c + 1) * 128],
                                                                     identity=identb[:]), reads=[Bxn, B_const], writes=[PTb[c4 % 2]])
                        for cc in range(4):
                            c = c4 * 4 + cc
                            if c4 % 2 == 0:
                                S.op(ACT, lambda e, c=c, cc=cc: e.activation(out=h2T[:, c, o0:o0 + 128], in_=pt[:, cc * 128:(cc + 1) * 128],
                                                                            func=AF.Identity, scale=s2[:, c:c + 1],
                                                                            bias=modT[:, 48 + c:49 + c]),
                                     reads=[PTb[c4 % 2], B_mod], writes=[B_h2])
                            else:
                                S.op(DVE, lambda e, c=c, cc=cc: e.tensor_scalar(out=h2T[:, c, o0:o0 + 128], in0=pt[:, cc * 128:(cc + 1) * 128],
                                                                               scalar1=s2[:, c:c + 1], scalar2=modT[:, 48 + c:49 + c],
                                                                               op0=ALU.mult, op1=ALU.add),
                                     reads=[PTb[c4 % 2], B_mod], writes=[B_h2])
                tap("h2T", h2T[:], [128, 16, NOWN], BF16, [B_h2])
                S.barrier()
                if limit == 9:
                    raise _Stop()
        sc_mg.close()

        with Scope(arena) as st:
            mx = sb(st, "mx3", [128, 8, D], F32)
            B_mx = [Buf() for _ in range(8)]
            with Scope(arena) as stw:
                Wa = [sb(stw, f"Wg{i}", [128, 16, 256], BF16) for i in range(2)]
                B_Wa = [Buf(), Buf()]
                Wb = [sb(stw, f"Wu{i}", [128, 16, 256], BF16) for i in range(2)]
                B_Wb = [Buf(), Buf()]
                Wd_t = [sb(stw, f"Wd{i}", [128, 11, 512], BF16) for i in range(2)]
                B_Wd = [Buf(), Buf()]
                tBs = [sb(stw, f"tB3{i}", [128, 512], BF16) for i in range(2)]
                B_tb = [Buf(), Buf()]
                aT = sb(stw, "aT", [128, 11, NOWN], BF16)
                B_aT = Buf()
                stepi = 0
                for Q in range(4):
                    for j2 in range(6):
                        nch = 2 if j2 < 5 else 1
                        c0 = (Q * 11 + j2 * 2) * 128
                        Wg, Bg = wload(Wa, B_Wa, 0, lambda w: w[:, :, 0:nch * 128], wv_g[:, :, c0:c0 + nch * 128])
                        Wu, Bu = wload(Wb, B_Wb, 1, lambda w: w[:, :, 0:nch * 128], wv_u[:, :, c0:c0 + nch * 128])
                        for cc in range(nch):
                            j = j2 * 2 + cc
                            fs = slice(cc * 128, (cc + 1) * 128)
                            for half in range(2):
                                gs = slice(half * 512, (half + 1) * 512)
                                pg, pu = (0, 1) if stepi % 2 == 0 else (4, 5)
                                tB, Btb = tBs[stepi % 2], B_tb[stepi % 2]
                                stepi += 1
                                for kc in range(KC):
                                    S.op(PE, lambda e, kc=kc: e.matmul(PB[pg][:, :], lhsT=Wg[:, kc, fs], rhs=h2T[:, kc, gs],
                                                                       start=(kc == 0), stop=(kc == KC - 1)),
                                         reads=[Bg, B_h2], writes=[PBb[pg]])
                                for kc in range(KC):
                                    S.op(PE, lambda e, kc=kc: e.matmul(PB[pu][:, :], lhsT=Wu[:, kc, fs], rhs=h2T[:, kc, gs],
                                                                       start=(kc == 0), stop=(kc == KC - 1)),
                                         reads=[Bu, B_h2], writes=[PBb[pu]])
                                S.op(ACT, lambda e: e.activation(out=tB[:], in_=PB[pg][:, :], func=AF.Silu), reads=[PBb[pg]], writes=[Btb])
                                S.op(DVE, lambda e: e.tensor_tensor(out=aT[:, j, gs], in0=PB[pu][:, :], in1=tB[:], op=ALU.mult),
                                     reads=[PBb[pu], Btb], writes=[B_aT])
                    for nb in range(4):
                        Wd, Bd = wload(Wd_t, B_Wd, 2, lambda w: w[:, :, :],
                                       wv_d[:, Q * 11:(Q + 1) * 11, nb * 512:(nb + 1) * 512])
                        for t in range(8):
                            pbi = 2 + (t % 2)
                            for j in range(11):
                                S.op(PE, lambda e, j=j: e.matmul(PB[pbi][:, :], lhsT=aT[:, j, t * 128:(t + 1) * 128], rhs=Wd[:, j, :],
                                                                 start=(j == 0), stop=(j == 10)), reads=[Bd, B_aT], writes=[PBb[pbi]])
                            dst = mx[:, t, nb * 512:(nb + 1) * 512]
                            if Q == 0:
                                S.op(ACT, lambda e: e.activation(out=dst, in_=PB[pbi][:, :], func=AF.Copy),
                                     reads=[PBb[pbi]], writes=[B_mx[t]])
                            else:
                                S.op(DVE, lambda e: e.tensor_tensor(out=dst, in0=PB[pbi][:, :], in1=dst, op=ALU.add),
                                     reads=[PBb[pbi]], writes=[B_mx[t]])
                S.barrier()
                if limit == 10:
                    raise _Stop()
            with Scope(arena) as stf:
                G2 = sb(stf, "G2", [128, D], F32)
                B_G = Buf()
                S.dma(SP, lambda e: e.dma_start(out=G2[:], in_=gsc_d[1].partition_broadcast(128)), reads=[B_gsc], writes=[B_G])
                ss1_ = sb(stf, "ss3", [128, 8, 16], F32)
                B_ss = [Buf() for _ in range(8)]
                x1t = [sb(stf, f"x1t3{i}", [128, D], F32) for i in range(2)]
                B_x1 = [Buf(), Buf()]
                xn2 = sb(stf, "xn3", [128, D], BF16)
                B_xn2 = Buf()
                for t in range(8):
                    ss1 = ss1_[:, t, :]
                    x1 = x1t[t % 2]
                    o0 = t * 128
                    S.dma(SP, lambda e: e.dma_start(out=x1[:], in_=out[o0:o0 + 128, :]), reads=[B_out],
                          writes=[B_x1[t % 2]])
                    S.op(ACT, lambda e: e.activation(out=xn2[:], in_=mx[:, t, :], func=AF.Square, accum_out=ss1[:, 8:9]),
                         reads=[B_mx[t]], writes=[B_xn2, B_ss[t]])
                    rstd_from_ssq(ss1[:, 10:11], ss1[:, 8:9], ss1[:, 9:10], [B_ss[t]], [B_ss[t]])
                    S.op(DVE, lambda e: e.scalar_tensor_tensor(out=mx[:, t, :], in0=mx[:, t, :], scalar=ss1[:, 10:11], in1=G2[:],
                                                               op0=ALU.mult, op1=ALU.mult),
                         reads=[B_ss[t], B_G], writes=[B_mx[t]])
                    S.op(DVE, lambda e: e.tensor_tensor(out=x1[:], in0=mx[:, t, :], in1=x1[:], op=ALU.add),
                         reads=[B_mx[t]], writes=[B_x1[t % 2]])
                    S.dma(SP, lambda e: e.dma_start(out=out[o0:o0 + 128, :], in_=x1[:]), reads=[B_x1[t % 2]],
                          writes=[B_out])
                S.barrier()
        sc_h2.close()
    except _Stop:
        S.barrier()
    stack.close()
    build.last_sched = S
    return nc, dbg_outs


def _fm(v, nch):
    return np.ascontiguousarray(np.asarray(v, np.float32).reshape(nch, 128).T)


def make_in_maps(x, c, w_ada, b_ada, g_pre_mix, w_in, w_dw, b_dw, g_conv_ln, b_conv_ln, w_conv_out, w_attn_out,
                 w_o, g_post_mix, g_pre_ffn, w_gate, w_up, w_down, g_post_ffn):
    x = np.asarray(x, np.float32)
    shared = {
        "w_ada": np.ascontiguousarray(np.asarray(w_ada, np.float32)[0]),
        "b_adaT": _fm(np.asarray(b_ada)[0], 96),
        "g_pre_mixT": _fm(np.asarray(g_pre_mix)[0], 16),
        "w_in": np.ascontiguousarray(np.asarray(w_in, np.float32)[0]),
        "w_dwT": np.ascontiguousarray(
            np.asarray(w_dw, np.float32)[0].T.reshape(8, 128, 31).transpose(1, 0, 2).reshape(128, 8 * 31)),
        "b_dwT": _fm(np.asarray(b_dw)[0], 8),
        "g_lnT": _fm(np.asarray(g_conv_ln)[0], 8),
        "b_lnT": _fm(np.asarray(b_conv_ln)[0], 8),
        "w_conv_out": np.ascontiguousarray(np.asarray(w_conv_out, np.float32)[0]),
        "w_attn_out": np.ascontiguousarray(np.asarray(w_attn_out, np.float32)[0]),
        "w_o": np.ascontiguousarray(np.asarray(w_o, np.float32)[0]),
        "g_post_mixT": _fm(np.asarray(g_post_mix)[0], 16),
        "g_pre_ffnT": _fm(np.asarray(g_pre_ffn)[0], 16),
        "w_gate": np.ascontiguousarray(np.asarray(w_gate, np.float32)[0]),
        "w_up": np.ascontiguousarray(np.asarray(w_up, np.float32)[0]),
        "w_down": np.ascontiguousarray(np.asarray(w_down, np.float32)[0]),
        "g_post_ffnT": _fm(np.asarray(g_post_ffn)[0], 16),
        "ident": np.eye(128, dtype=np.float32),
        "pow2": np.ascontiguousarray(np.tile((2.0 ** -(np.arange(NITER + 1) + 1.0)).astype(np.float32)[None, :], (128, 1))),
    }
    diagc = np.zeros((1, 256), np.float32)
    diagc[0, 0:64] = 1.0
    diagc[0, 128 + 64:256] = -BIG
    shared["diagc"] = diagc
    maps = []
    for core in range(8):
        b, j = core // 4, core % 4
        xc = np.zeros((NK, D), np.float32)
        nval = j * 1024
        xc[NCTX - nval:NCTX] = x[b, 0:nval]
        xc[NCTX:] = x[b, j * 1024:(j + 1) * 1024]
        kneg = np.zeros((1, 512), np.float32)
        for sgm in range(3):
            if sgm < 3 - j:
                kneg[0, sgm * 128:(sgm + 1) * 128] = -BIG
        pen = np.zeros((128, 6), np.float32)
        for sgm in range(3):
            if sgm < 3 - j:
                pen[:, sgm] = 2 * BIG
        pen[0:64, 5] = 2 * BIG
        m = dict(shared)
        m["xctx"] = xc
        m["cT"] = _fm(np.asarray(c, np.float32)[b], 16)
        m["kneg"] = kneg
        m["pen"] = pen
        m["halo_valid"] = np.full((128, 1), 1.0 if j > 0 else 0.0, np.float32)
        maps.append(m)
    return maps


_NC_CACHE = {}


def kernel(**inputs):
    if "nc" not in _NC_CACHE:
        _NC_CACHE["nc"] = build()[0]
    nc = _NC_CACHE["nc"]
    maps = make_in_maps(**inputs)
    res = run_bass_kernel_spmd(nc, maps, core_ids=list(range(8)))
    outp = np.zeros((2, 4096, D), np.float32)
    for core in range(8):
        b, j = core // 4, core % 4
        outp[b, j * 1024:(j + 1) * 1024] = res.results[core]["out"]
    return outp
```

```python
import numpy as np
from contextlib import ExitStack
import concourse.bass as bass
import concourse.mybir as mybir
from concourse.bass_utils import run_bass_kernel_spmd

F32 = mybir.dt.float32
BF16 = mybir.dt.bfloat16
AF = mybir.ActivationFunctionType
ALU = mybir.AluOpType
AX = mybir.AxisListType

D = 2048
KC = 16
NOWN = 1024
NCTX = 3072
NK = 4096
DFF = 5632
EPS = 1e-6
BIG = 4096.0
NITER = 17
SBQ = 2
O_GLU_A, O_GLU_G, O_Q, O_K, O_V, O_QI, O_KI, O_WI, O_GC, O_GA = (
    0, 1024, 2048, 3072, 4096, 5120, 6144, 6208, 6224, 8272)
IDX_SCALE = (64 ** -0.5) * (16 ** -0.5)
import os
BIS = os.environ.get('BIS', '')


class Buf:
    __slots__ = ("w", "r", "name")

    def __init__(self, name=""):
        self.w = {}
        self.r = {}
        self.name = name


class Eng:
    def __init__(self, name, eng, sem):
        self.name = name
        self.eng = eng
        self.sem = sem
        self.cnt = 0
        self.seen = {}


class Sched:
    def __init__(self, nc, stack, n_slots=40):
        self.nc = nc
        mk = lambda n: stack.enter_context(nc.semaphore(n))
        self.PE = Eng("pe", nc.tensor, mk("s_pe"))
        self.ACT = Eng("act", nc.scalar, mk("s_act"))
        self.DVE = Eng("dve", nc.vector, mk("s_dve"))
        self.POOL = Eng("pool", nc.gpsimd, mk("s_pool"))
        self.SP = Eng("sp", nc.sync, mk("s_sp"))
        self.engs = [self.PE, self.ACT, self.DVE, self.POOL, self.SP]
        self.slots = [[f"dma{i}", mk(f"s_dma{i}"), 0] for i in range(n_slots)]
        self.slot_i = 0
        self.prog = {}

    def _deps(self, reads, writes):
        d = {}

        def add(dic):
            for k, (sem, v) in dic.items():
                if k not in d or d[k][1] < v:
                    d[k] = (sem, v)
        for b in reads:
            add(b.w)
        for b in writes:
            add(b.w)
            add(b.r)
        return d

    def _wait(self, E, d):
        for k, (sem, v) in d.items():
            if E is self.PE and k == "pe":
                continue
            if E.seen.get(k, 0) >= v:
                continue
            E.eng.wait_ge(sem, v)
            E.seen[k] = v
            self.prog.setdefault(E.name, []).append(("w", k, v))

    def _mark(self, k, sem, v, reads, writes):
        for b in reads:
            if k not in b.r or b.r[k][1] < v:
                b.r[k] = (sem, v)
        for b in writes:
            b.w[k] = (sem, v)
            b.r = {}

    def op(self, E, fn, reads=(), writes=()):
        self._wait(E, self._deps(reads, writes))
        inst = fn(E.eng)
        E.cnt += 1
        inst.then_inc(E.sem, 1)
        self.prog.setdefault(E.name, []).append(("i", E.name, 1))
        self._mark(E.name, E.sem, E.cnt, reads, writes)

    def dma(self, Q, fn, reads=(), writes=()):
        d = self._deps(reads, writes)
        slot = self.slots[self.slot_i % len(self.slots)]
        self.slot_i += 1
        if slot[2] > 0:
            d[slot[0]] = (slot[1], slot[2])
        self._wait(Q, d)
        inst = fn(Q.eng)
        slot[2] += 16
        inst.then_inc(slot[1], 16)
        self.prog.setdefault(Q.name, []).append(("i", slot[0], 16))
        self._mark(slot[0], slot[1], slot[2], reads, writes)

    def barrier(self):
        evs = {}
        for E in self.engs:
            if E.cnt > 0:
                evs[E.name] = (E.sem, E.cnt)
        for s in self.slots:
            if s[2] > 0:
                evs[s[0]] = (s[1], s[2])
        for E in self.engs:
            self._wait(E, evs)


class Arena:
    def __init__(self, nc, stack, nbytes):
        self.t = stack.enter_context(nc.sbuf_tensor("arena", [128, nbytes // 4], F32))
        self.free = [(0, nbytes)]

    def alloc(self, nbytes):
        nbytes = (nbytes + 63) // 64 * 64
        for i, (o, n) in enumerate(self.free):
            if n >= nbytes:
                if n == nbytes:
                    self.free.pop(i)
                else:
                    self.free[i] = (o + nbytes, n - nbytes)
                return o, nbytes
        raise MemoryError(f"arena out of space for {nbytes}: free={self.free}")

    def release(self, o, n):
        fl = sorted(self.free + [(o, n)])
        out = []
        for a, b in fl:
            if out and out[-1][0] + out[-1][1] == a:
                out[-1] = (out[-1][0], out[-1][1] + b)
            else:
                out.append((a, b))
        self.free = out


class Scope:
    def __init__(self, arena):
        self.a = arena
        self.items = []

    def __enter__(self):
        return self

    def __exit__(self, *exc):
        self.close()
        return False

    def close(self):
        for o, n in self.items:
            self.a.release(o, n)
        self.items = []

    def alloc(self, shape, dt):
        esz = 4 if dt == F32 else 2
        n = 1
        for d_ in shape[1:]:
            n *= d_
        o, nb = self.a.alloc(n * esz)
        self.items.append((o, nb))
        v = self.a.t[0:shape[0], o // 4:(o + nb) // 4]
        if dt != F32:
            v = v.bitcast(dt)
        v = v[:, 0:n]
        if len(shape) == 3:
            v = v.rearrange("p (a b) -> p a b", a=shape[1])
        elif len(shape) == 4:
            v = v.rearrange("p (a b c) -> p a b c", a=shape[1], b=shape[2])
        return v


class _Stop(Exception):
    pass


def build(debug=(), limit=99):
    nc = bass.Bass("TRN2", target_bir_lowering=False)
    stack = ExitStack()

    def din(name, shape, dt=F32):
        return nc.dram_tensor(name, shape, dt, kind="ExternalInput").ap()

    xctx = din("xctx", [NK, D])
    cT_d = din("cT", [128, 16])
    w_ada = din("w_ada", [D, 6 * D])
    b_adaT = din("b_adaT", [128, 96])
    gpmT = din("g_pre_mixT", [128, 16])
    w_in = din("w_in", [D, 10320])
    w_dwT = din("w_dwT", [128, 8 * 31])
    b_dwT = din("b_dwT", [128, 8])
    g_lnT = din("g_lnT", [128, 8])
    b_lnT = din("b_lnT", [128, 8])
    w_co = din("w_conv_out", [1024, D])
    w_ao = din("w_attn_out", [1024, D])
    w_o = din("w_o", [D, D])
    gpostmT = din("g_post_mixT", [128, 16])
    gpfT = din("g_pre_ffnT", [128, 16])
    w_gate = din("w_gate", [D, DFF])
    w_up = din("w_up", [D, DFF])
    w_down = din("w_down", [DFF, D])
    gpostfT = din("g_post_ffnT", [128, 16])
    ident_d = din("ident", [128, 128])
    kneg_d = din("kneg", [1, 512])
    pen_d = din("pen", [128, 6])
    diag_d = din("diagc", [1, 256])
    pow2_d = din("pow2", [128, NITER + 1])
    halo_d = din("halo_valid", [128, 1])

    out = nc.dram_tensor("out", [NOWN, D], F32, kind="ExternalOutput").ap()
    kT_d = nc.dram_tensor("kT_scr", [8, 128, NK], BF16, kind="Internal").ap()
    v_d = nc.dram_tensor("v_scr", [NK, 8 * 160], BF16, kind="Internal").ap()
    gsc_d = nc.dram_tensor("g_scr", [2, D], F32, kind="Internal").ap()
    hown_d = nc.dram_tensor("hown_scr", [128, 16 * NOWN], BF16, kind="Internal").ap()

    dbg_outs = {}

    S = Sched(nc, stack)
    PE, ACT, DVE, POOL, SP = S.PE, S.ACT, S.DVE, S.POOL, S.SP
    try:

        arena = Arena(nc, stack, 212480)
        root = Scope(arena)

        def sb(st, name, shape, dt):
            if st is stack:
                st = root
            return st.alloc(shape, dt)

        def ps(st, name, shape, dt):
            return st.enter_context(nc.psum_tensor("ps_" + name, shape, dt))

        PB = [ps(stack, f"pb{i}", [128, 512], F32) for i in range(6)]
        PBb = [Buf(f"pb{i}") for i in range(6)]
        PT = [ps(stack, f"pt{i}", [128, 1024], BF16) for i in range(2)]
        PTb = [Buf(f"pt{i}") for i in range(2)]

        identf = sb(stack, "identf", [128, 128], F32)
        identb = sb(stack, "identb", [128, 128], BF16)
        onesf = sb(stack, "onesf", [128, 128], F32)
        onesb = sb(stack, "onesb", [128, 128], BF16)
        epsc = sb(stack, "epsc", [128, 1], F32)
        modT = sb(stack, "modT", [128, 96], F32)
        s1 = sb(stack, "s1", [128, 16], F32)
        s2 = sb(stack, "s2", [128, 16], F32)
        smallv = sb(stack, "smallv", [128, 16 * 8], F32)
        kneg_b = sb(stack, "kneg_b", [1, 512], BF16)
        ones512 = sb(stack, "ones512", [1, 512], BF16)
        diag_b = sb(stack, "diag_b", [1, 256], BF16)
        pen = sb(stack, "pen", [128, 6], F32)
        pow2 = sb(stack, "pow2", [128, NITER + 1], F32)
        halov = sb(stack, "halov", [128, 1], F32)
        wdw = sb(stack, "wdw", [128, 8 * 31], F32)
        convv = sb(stack, "convv", [128, 24], F32)
        sc_kiT, sc_hown, sc_halo = Scope(arena), Scope(arena), Scope(arena)
        kiT = sb(sc_kiT, "kiT", [128, NK], BF16)
        hT_own = sb(sc_hown, "hT_own", [128, 16, NOWN], BF16)
        hT_halo = sb(sc_halo, "hT_halo", [128, 16, 32], BF16)
        B_const = Buf("const")
        B_mod = Buf("mod")
        B_kiT = Buf("kiT")
        B_hown = [Buf("hown0"), Buf("hown1")]
        B_halo = Buf("halo")

        def tap(name, src_ap, shape, dt, reads):
            if name not in debug:
                return
            t = nc.dram_tensor("dbg_" + name, shape, dt, kind="ExternalOutput").ap()
            dbg_outs[name] = t
            S.dma(SP, lambda e: e.dma_start(out=t, in_=src_ap), reads=reads, writes=[Buf()])

        ld = lambda dst, src: S.dma(SP, lambda e: e.dma_start(out=dst, in_=src), writes=[B_const])
        ld(identf[:], ident_d)
        ld(pen[:], pen_d)
        ld(pow2[:], pow2_d)
        ld(halov[:], halo_d)
        ld(wdw[:], w_dwT)
        ld(convv[:, 0:8], b_dwT)
        ld(convv[:, 8:16], g_lnT)
        ld(convv[:, 16:24], b_lnT)
        ld(smallv[:, 0:16], gpmT)
        ld(smallv[:, 16:32], gpostmT)
        ld(smallv[:, 32:48], gpfT)
        ld(smallv[:, 48:64], gpostfT)
        S.dma(POOL, lambda e: e.dma_start(out=kneg_b[:], in_=kneg_d), writes=[B_const])
        S.op(DVE, lambda e: e.memset(ones512[:], 1.0), writes=[B_const])
        S.dma(POOL, lambda e: e.dma_start(out=diag_b[:], in_=diag_d), writes=[B_const])
        S.op(DVE, lambda e: e.tensor_copy(out=identb[:], in_=identf[:]), reads=[B_const], writes=[B_const])
        S.op(DVE, lambda e: e.memset(onesf[:], 1.0), writes=[B_const])
        S.op(DVE, lambda e: e.memset(onesb[:], 1.0), writes=[B_const])
        S.op(DVE, lambda e: e.memset(epsc[:], EPS), writes=[B_const])

        def rstd_from_ssq(dst, ssq, tmp, rb, wb):
            S.op(ACT, lambda e: e.activation(out=tmp, in_=ssq, func=AF.Sqrt, bias=epsc[:, 0:1], scale=1.0 / D),
                 reads=rb + [B_const], writes=wb)
            S.op(DVE, lambda e: e.reciprocal(out=dst, in_=tmp), reads=wb, writes=wb)

        wv_in = w_in.rearrange("(c p) n -> p c n", p=128)

        cb = sb(stack, "cb", [128, 16], BF16)
        badd = sb(stack, "badd", [128, 96], F32)
        Bc = Buf()
        with Scope(arena) as st:
            cT = sb(st, "cT", [128, 16], F32)
            wa = [sb(st, f"wa{i}", [128, 16, 512], BF16) for i in range(2)]
            Bwa = [Buf(), Buf()]
            S.dma(SP, lambda e: e.dma_start(out=cT[:], in_=cT_d), writes=[Bc])
            S.dma(SP, lambda e: e.dma_start(out=badd[:], in_=b_adaT), writes=[Bc])
            S.op(ACT, lambda e: e.activation(out=cb[:], in_=cT[:], func=AF.Silu), reads=[Bc], writes=[Bc])
            wv_ada = w_ada.rearrange("(c p) n -> p c n", p=128)
            for blk in range(8):
                w_t = wa[blk % 2]
                for q4 in range(4):
                    S.dma(POOL, lambda e, w_t=w_t, q4=q4, blk=blk: e.dma_start(
                        out=w_t[:, q4 * 4:(q4 + 1) * 4, :], in_=wv_ada[:, q4 * 4:(q4 + 1) * 4, blk * 512:(blk + 1) * 512]),
                        writes=[Bwa[blk % 2]])
                for cc in range(4):
                    m = blk * 4 + cc
                    for kc in range(KC):
                        S.op(PE, lambda e, w_t=w_t, cc=cc, kc=kc, m=m: e.matmul(
                            PB[0][:, m:m + 1], lhsT=w_t[:, kc, cc * 128:(cc + 1) * 128], rhs=cb[:, kc:kc + 1],
                            start=(kc == 0), stop=(kc == KC - 1)),
                            reads=[Bwa[blk % 2], Bc], writes=[PBb[0]])
            S.op(DVE, lambda e: e.tensor_tensor(out=modT[:, 0:32], in0=PB[0][:, 0:32], in1=badd[:, 0:32], op=ALU.add),
                 reads=[PBb[0], Bc], writes=[B_mod])
            S.op(DVE, lambda e: e.scalar_tensor_tensor(out=s1[:], in0=modT[:, 16:32], scalar=1.0, in1=smallv[:, 0:16],
                                                       op0=ALU.add, op1=ALU.mult), reads=[B_mod, B_const], writes=[B_mod])
            S.barrier()
            if limit == 1:
                raise _Stop()

        def norm_transpose(st_bufs, src_tiles, nt, sc_t, bias_ap_fn, dst_fn, dst_bufs, src_bufs):
            xn, Bxn, ssq, rs, tmp, Bst = st_bufs
            for t in range(nt):
                S.op(ACT, lambda e, t=t: e.activation(out=xn[:, t, :], in_=src_tiles[t], func=AF.Square,
                                                      accum_out=ssq[:, t:t + 1]),
                     reads=[src_bufs[t]], writes=[Bxn[t], Bst[t]])
                rstd_from_ssq(rs[:, t:t + 1], ssq[:, t:t + 1], tmp[:, t:t + 1], [Bst[t]], [Bst[t]])
                S.op(ACT, lambda e, t=t: e.activation(out=xn[:, t, :], in_=src_tiles[t], func=AF.Copy,
                                                      scale=rs[:, t:t + 1]),
                     reads=[src_bufs[t], Bst[t]], writes=[Bxn[t]])
            for c in range(KC):
                pt = PT[c % 2]
                for t in range(nt):
                    S.op(PE, lambda e, t=t, c=c, pt=pt: e.transpose(out=pt[:, t * 128:(t + 1) * 128],
                                                                   in_=xn[:, t, c * 128:(c + 1) * 128], identity=identb[:]),
                         reads=[Bxn[t], B_const], writes=[PTb[c % 2]])
                if c % 2 == 0:
                    S.op(ACT, lambda e, c=c, pt=pt: e.activation(out=dst_fn(c), in_=pt[:, 0:nt * 128], func=AF.Identity,
                                                                 scale=sc_t[:, c:c + 1], bias=bias_ap_fn(c)),
                         reads=[PTb[c % 2], B_mod], writes=dst_bufs)
                else:
                    S.op(DVE, lambda e, c=c, pt=pt: e.tensor_scalar(out=dst_fn(c), in0=pt[:, 0:nt * 128],
                                                                    scalar1=sc_t[:, c:c + 1], scalar2=bias_ap_fn(c),
                                                                    op0=ALU.mult, op1=ALU.add),
                         reads=[PTb[c % 2], B_mod], writes=dst_bufs)

        with Scope(arena) as st:
            Wkv = sb(st, "Wkv", [128, 16, 2048], BF16)
            Wki = sb(st, "Wki", [128, 16, 128], BF16)
            Bw = Buf()
            for kc4 in range(8):
                S.dma(POOL, lambda e, kc4=kc4: e.dma_start(out=Wkv[:, kc4 * 2:kc4 * 2 + 2, :],
                                                           in_=wv_in[:, kc4 * 2:kc4 * 2 + 2, O_K:O_K + 2048]), writes=[Bw])
            for hlf in range(2):
                S.dma(POOL, lambda e, hlf=hlf: e.dma_start(out=Wki[:, :, hlf * 64:(hlf + 1) * 64],
                                                           in_=wv_in[:, :, O_KI:O_KI + 64]), writes=[Bw])
            xt = [sb(st, f"xt{i}", [128, D], F32) for i in range(2)]
            Bxt = [Buf(), Buf()]
            xn = sb(st, "xn", [128, 4, D], BF16)
            Bxn = [Buf() for _ in range(4)]
            ssq = sb(st, "ssq", [128, 4], F32)
            rs = sb(st, "rs", [128, 4], F32)
            tmp = sb(st, "tmpv", [128, 4], F32)
            Bst = [Buf() for _ in range(4)]
            hT_tmp = sb(st, "hT_tmp", [128, 16, 512], BF16)
            B_htmp = Buf()
            kT_g = sb(st, "kT_g", [128, 8, 512], BF16)
            B_kTg = Buf()
            v_g = sb(st, "v_g", [128, 4, 8, 160], BF16)
            B_vg = Buf()
            S.op(POOL, lambda e: e.memset(v_g[:], 0.0), writes=[B_vg])
            for t_ in range(4):
                S.op(POOL, lambda e, t_=t_: e.memset(v_g[:, t_, :, 64:65], 1.0), writes=[B_vg])
            B_kTd = Buf("kTd")
            B_vd = Buf("vd")
            def norm_group(g):
                for t in range(4):
                    xb = xt[t % 2]
                    r0 = g * 512 + t * 128
                    S.dma(SP, lambda e, xb=xb, r0=r0: e.dma_start(out=xb[:], in_=xctx[r0:r0 + 128, :]),
                          writes=[Bxt[t % 2]])
                    S.op(ACT, lambda e, t=t, xb=xb: e.activation(out=xn[:, t, :], in_=xb[:], func=AF.Square,
                                                                 accum_out=ssq[:, t:t + 1]),
                         reads=[Bxt[t % 2]], writes=[Bxn[t], Bst[t]])
                    rstd_from_ssq(rs[:, t:t + 1], ssq[:, t:t + 1], tmp[:, t:t + 1], [Bst[t]], [Bst[t]])
                    S.op(ACT, lambda e, t=t, xb=xb: e.activation(out=xn[:, t, :], in_=xb[:], func=AF.Copy,
                                                                 scale=rs[:, t:t + 1]),
                         reads=[Bxt[t % 2], Bst[t]], writes=[Bxn[t]])
            norm_group(0)
            for g in range(8):
                own = g >= 6
                if own:
                    hsl = lambda c, g=g: hT_own[:, c, (g - 6) * 512:(g - 5) * 512]
                    hb = B_hown[g - 6]
                else:
                    hsl = lambda c: hT_tmp[:, c, :]
                    hb = B_htmp
                for c in range(KC):
                    pt = PT[c % 2]
                    for t in range(4):
                        S.op(PE, lambda e, t=t, c=c, pt=pt: e.transpose(out=pt[:, t * 128:(t + 1) * 128],
                                                                       in_=xn[:, t, c * 128:(c + 1) * 128],
                                                                       identity=identb[:]),
                             reads=[Bxn[t], B_const], writes=[PTb[c % 2]])
                    if c % 2 == 0:
                        S.op(ACT, lambda e, c=c, pt=pt, hsl=hsl: e.activation(out=hsl(c), in_=pt[:, 0:512], func=AF.Identity,
                                                                             scale=s1[:, c:c + 1], bias=modT[:, c:c + 1]),
                             reads=[PTb[c % 2], B_mod], writes=[hb])
                    else:
                        S.op(DVE, lambda e, c=c, pt=pt, hsl=hsl: e.tensor_scalar(out=hsl(c), in0=pt[:, 0:512],
                                                                                scalar1=s1[:, c:c + 1], scalar2=modT[:, c:c + 1],
                                                                                op0=ALU.mult, op1=ALU.add),
                             reads=[PTb[c % 2], B_mod], writes=[hb])
                if g + 1 < 8:
                    norm_group(g + 1)
                if g == 5:
                    S.op(DVE, lambda e: e.tensor_copy(out=hT_halo[:], in_=hT_tmp[:, :, 480:512]),
                         reads=[B_htmp], writes=[B_halo])
                for cc in range(8):
                    pb = cc % 2
                    for kc in range(KC):
                        S.op(PE, lambda e, cc=cc, kc=kc, pb=pb, hsl=hsl: e.matmul(
                            PB[pb][:, :], lhsT=Wkv[:, kc, cc * 128:(cc + 1) * 128], rhs=hsl(kc),
                            start=(kc == 0), stop=(kc == KC - 1)), reads=[Bw, hb], writes=[PBb[pb]])
                    if cc % 2 == 0:
                        S.op(ACT, lambda e, cc=cc, pb=pb: e.activation(out=kT_g[:, cc, :], in_=PB[pb][:, :], func=AF.Copy),
                             reads=[PBb[pb]], writes=[B_kTg])
                    else:
                        S.op(DVE, lambda e, cc=cc, pb=pb: e.tensor_copy(out=kT_g[:, cc, :], in_=PB[pb][:, :]),
                             reads=[PBb[pb]], writes=[B_kTg])
                S.dma(SP, lambda e, g=g: e.dma_start(out=kT_d[:, :, g * 512:(g + 1) * 512].rearrange("c p n -> p c n"),
                                                     in_=kT_g[:]), reads=[B_kTg], writes=[B_kTd])
                for kc in range(KC):
                    S.op(PE, lambda e, kc=kc, hsl=hsl: e.matmul(PB[2][:, :], lhsT=Wki[:, kc, :], rhs=hsl(kc),
                                                                start=(kc == 0), stop=(kc == KC - 1)),
                         reads=[Bw, hb], writes=[PBb[2]])
                S.op(ACT, lambda e, g=g: e.activation(out=kiT[:, g * 512:(g + 1) * 512], in_=PB[2][:, :], func=AF.Copy),
                     reads=[PBb[2]], writes=[B_kiT])
                for t in range(4):
                    for nb in range(2):
                        pb = 3 + (t * 2 + nb) % 2
                        for kc in range(KC):
                            S.op(PE, lambda e, t=t, nb=nb, kc=kc, pb=pb, hsl=hsl: e.matmul(
                                PB[pb][:, :], lhsT=hsl(kc)[:, t * 128:(t + 1) * 128],
                                rhs=Wkv[:, kc, 1024 + nb * 512:1024 + (nb + 1) * 512],
                                start=(kc == 0), stop=(kc == KC - 1)), reads=[Bw, hb], writes=[PBb[pb]])
                        srcv = PB[pb][:, :].rearrange("p (r e d) -> p r e d", e=2, d=64)
                        dst_e = v_g[:, t, nb * 4:(nb + 1) * 4, 0:64]
                        dst_o = v_g[:, t, nb * 4:(nb + 1) * 4, 96:160]
                        if pb == 3:
                            S.op(ACT, lambda e, dst_e=dst_e, srcv=srcv: e.activation(out=dst_e, in_=srcv[:, :, 0, :], func=AF.Copy),
                                 reads=[PBb[pb]], writes=[B_vg])
                            S.op(ACT, lambda e, dst_o=dst_o, srcv=srcv: e.activation(out=dst_o, in_=srcv[:, :, 1, :], func=AF.Copy),
                                 reads=[PBb[pb]], writes=[B_vg])
                        else:
                            S.op(DVE, lambda e, dst_e=dst_e, srcv=srcv: e.tensor_copy(out=dst_e, in_=srcv[:, :, 0, :]),
                                 reads=[PBb[pb]], writes=[B_vg])
                            S.op(DVE, lambda e, dst_o=dst_o, srcv=srcv: e.tensor_copy(out=dst_o, in_=srcv[:, :, 1, :]),
                                 reads=[PBb[pb]], writes=[B_vg])
                S.dma(SP, lambda e, g=g: e.dma_start(
                    out=v_d[g * 512:(g + 1) * 512, :].rearrange("(t p) n -> p t n", p=128),
                    in_=v_g[:].rearrange("p t h d -> p t (h d)")), reads=[B_vg], writes=[B_vd])
            tap("hT_own", hT_own[:], [128, 16, NOWN], BF16, B_hown)
            tap("kiT", kiT[:], [128, NK], BF16, [B_kiT])
            S.barrier()
            if limit == 2:
                raise _Stop()

        def proj_fm(st, wsrc3, col0, nchunks, segs, consume, Bw2, wblk, seg_reads):
            nblk = (nchunks + 3) // 4
            for blk in range(nblk):
                w_t = wblk[blk % 2]
                ncol = min(4, nchunks - blk * 4) * 128
                for q4 in range(2):
                    S.dma(POOL, lambda e, w_t=w_t, q4=q4, blk=blk, ncol=ncol: e.dma_start(
                        out=w_t[:, q4 * 8:(q4 + 1) * 8, 0:ncol],
                        in_=wsrc3[:, q4 * 8:(q4 + 1) * 8, col0 + blk * 512:col0 + blk * 512 + ncol]),
                        writes=[Bw2[blk % 2]])
                for cc in range(ncol // 128):
                    ch = blk * 4 + cc
                    for si, (rhs_fn, wd) in enumerate(segs):
                        pbi = (ch * len(segs) + si) % 2
                        for kc in range(KC):
                            S.op(PE, lambda e, w_t=w_t, cc=cc, kc=kc, pbi=pbi, rhs_fn=rhs_fn, wd=wd: e.matmul(
                                PB[pbi][:, 0:wd], lhsT=w_t[:, kc, cc * 128:(cc + 1) * 128], rhs=rhs_fn(kc),
                                start=(kc == 0), stop=(kc == KC - 1)),
                                reads=[Bw2[blk % 2]] + seg_reads, writes=[PBb[pbi]])
                        consume(ch, si, PB[pbi], PBb[pbi])

        own_segs = [(lambda kc: hT_own[:, kc, 0:512], 512), (lambda kc: hT_own[:, kc, 512:1024], 512)]

        att_stack = Scope(arena)
        attT = sb(att_stack, "attT", [128, 8, NOWN], BF16)
        B_att = Buf("attT")
        with Scope(arena) as st:
            qT = sb(st, "qT", [128, 8, NOWN], BF16)
            qiT = sb(st, "qiT", [128, 8, NOWN], BF16)
            B_q = Buf()
            B_qi = Buf()
            wsc = sb(st, "wsc", [128, 8, 16], F32)
            wabs = sb(st, "wabs", [128, 8, 16], F32)
            wsgn = sb(st, "wsgn", [128, 8, 16], F32)
            B_wi = Buf()
            st2 = Scope(arena)
            wblk = [sb(st2, f"wblk{i}", [128, 16, 512], BF16) for i in range(2)]
            Bw2 = [Buf(), Buf()]
            Wwi = sb(st2, "Wwi", [128, 16, 16], BF16)
            B_wwi = Buf()

            def cons_q(dst, Bd):
                def f(ch, si, pb, pbb):
                    eng = ACT if (ch + si) % 2 == 0 else DVE
                    if eng is ACT:
                        S.op(ACT, lambda e: e.activation(out=dst[:, ch, si * 512:(si + 1) * 512], in_=pb[:, :], func=AF.Copy),
                             reads=[pbb], writes=[Bd])
                    else:
                        S.op(DVE, lambda e: e.tensor_copy(out=dst[:, ch, si * 512:(si + 1) * 512], in_=pb[:, :]),
                             reads=[pbb], writes=[Bd])
                return f
            proj_fm(st, wv_in, O_Q, 8, own_segs, cons_q(qT, B_q), Bw2, wblk, B_hown)
            proj_fm(st, wv_in, O_QI, 8, own_segs, cons_q(qiT, B_qi), Bw2, wblk, B_hown)
            S.dma(POOL, lambda e: e.dma_start(out=Wwi[:], in_=wv_in[:, :, O_WI:O_WI + 16]), writes=[B_wwi])
            for t in range(8):
                for kc in range(KC):
                    S.op(PE, lambda e, t=t, kc=kc: e.matmul(PB[2][:, 0:16], lhsT=hT_own[:, kc, t * 128:(t + 1) * 128],
                                                            rhs=Wwi[:, kc, :], start=(kc == 0), stop=(kc == KC - 1)),
                         reads=[B_wwi] + B_hown, writes=[PBb[2]])
                S.op(ACT, lambda e, t=t: e.activation(out=wsc[:, t, :], in_=PB[2][:, 0:16], func=AF.Copy, scale=IDX_SCALE),
                     reads=[PBb[2]], writes=[B_wi])
            S.op(ACT, lambda e: e.activation(out=wabs[:], in_=wsc[:], func=AF.Abs), reads=[B_wi], writes=[B_wi])
            S.op(ACT, lambda e: e.activation(out=wsgn[:], in_=wsc[:], func=AF.Sign), reads=[B_wi], writes=[B_wi])
            tap("qT", qT[:], [128, 8, NOWN], BF16, [B_q])
            tap("wsc", wsc[:], [128, 8, 16], F32, [B_wi])
            B_hownd = Buf("hownd")
            S.dma(SP, lambda e: e.dma_start(out=hown_d, in_=hT_own[:].rearrange("p c n -> p (c n)")), reads=B_hown, writes=[B_hownd])
            S.barrier()
            if limit == 3:
                raise _Stop()
            st2.close()
            sc_hown.close()
            PTf = PT[1][:, :].bitcast(F32)
            was = [sb(st, f"was{i}", [128, 16, 128], BF16) for i in range(2)]
            Bwas = [Buf(), Buf()]

            def mod_rest(m):
                w_t, Bwt = was[m % 2], Bwas[m % 2]
                S.dma(POOL, lambda e: e.dma_start(out=w_t[:], in_=wv_ada[:, :, m * 128:(m + 1) * 128]), writes=[Bwt])
                for kc in range(KC):
                    S.op(PE, lambda e, kc=kc: e.matmul(PTf[:, m:m + 1], lhsT=w_t[:, kc, :], rhs=cb[:, kc:kc + 1],
                                                       start=(kc == 0), stop=(kc == KC - 1)),
                         reads=[Bwt, Bc], writes=[PTb[1]])

            scb2 = [sb(st, f"scb{i}", [128, NK], F32) for i in range(2)]
            B_sc2 = [Buf(), Buf()]
            mk = sb(st, "mk", [128, NK], BF16)
            B_mk = Buf()
            NQ = SBQ * 128
            maskT = sb(st, "maskT", [128, 32, NQ], BF16)
            B_maskT = Buf()
            dg2 = [sb(st, f"dg{i}", [128, 16, 128], BF16) for i in range(2)]
            B_dg2 = [Buf(), Buf()]
            NRR = 6
            rr = [sb(st, f"rr{i}", [128, 512], BF16) for i in range(NRR)]
            B_rr = [Buf() for _ in range(NRR)]
            kTc = sb(st, "kTc", [128, NK], BF16)
            B_kTc = [Buf(), Buf()]
            vc = sb(st, "vc", [128, 32, 160], BF16)
            B_vc = [Buf(), Buf()]
            NET = 4
            et = [sb(st, f"et{i}", [128, 2, NQ], BF16) for i in range(NET)]
            B_et = [Buf() for _ in range(NET)]
            ptl = [sb(st, f"ptl{i}", [128, 2, NQ], BF16) for i in range(NET)]
            B_ptl = [Buf() for _ in range(NET)]
            qpad = sb(st, "qpad", [128, 8, 2, NQ], BF16)
            B_qpad = Buf()
            S.op(POOL, lambda e: e.memset(qpad[:], 0.0), writes=[B_qpad])
            sv = sb(st, "sv", [128, 64], F32)
            B_sv = Buf()
            wtab = sb(st, "wtab", [128, NITER + 1], F32)
            rd = sb(st, "rd", [128, NQ], F32)
            B_rd = Buf()
            rbc = sb(st, "rbc", [128, NQ], F32)
            B_rbc = Buf()
            thr_dbg = sb(st, "thr_dbg", [128, 8], F32)
            B_thr = Buf()
            qk_slot = [PB[2][:, 0:2 * NQ], PB[3][:, 0:2 * NQ], PB[4][:, 0:2 * NQ],
                       PT[0][:, :].bitcast(F32)[:, 0:2 * NQ]]
            B_qk = [PBb[2], PBb[3], PBb[4], PTb[0]]
            NQK = len(qk_slot)

            def run_pipeline(nsteps, stages, lags):
                for tick in range(nsteps + max(lags)):
                    for f, lg in zip(stages, lags):
                        i = tick - lg
                        if 0 <= i < nsteps:
                            f(i)

            def indexer(qb):
                scb, B_sc = scb2[qb % 2], B_sc2[qb % 2]
                dg, B_dg = dg2[qb % 2], B_dg2[qb % 2]
                nkt = 24 + qb + 1
                N = nkt * 128
                q0 = qb * 128
                for h in range(16):
                    S.op(DVE if BIS == "dgdve" else POOL, lambda e, h=h: e.tensor_scalar(out=dg[:, h, :], in0=identb[:],
                                                              scalar1=wsgn[:, qb, h:h + 1], scalar2=None, op0=ALU.mult),
                         reads=[B_const, B_wi], writes=[B_dg])
                ngrp = (N + 511) // 512
                steps = [(n, h) for n in range(ngrp) for h in range(16)]

                def stA(i):
                    n, h = steps[i]
                    wdt = min(512, N - n * 512)
                    hb = (h % 2) * 64
                    pbi = i % 4
                    S.op(PE, lambda e: e.matmul(PB[pbi][:, 0:wdt], lhsT=qiT[hb:hb + 64, h // 2, q0:q0 + 128],
                                                rhs=kiT[hb:hb + 64, n * 512:n * 512 + wdt], start=True, stop=True),
                         reads=[B_qi, B_kiT], writes=[PBb[pbi]])

                def stB(i):
                    n, h = steps[i]
                    wdt = min(512, N - n * 512)
                    pbi = i % 4
                    r_t, B_r = rr[i % NRR], B_rr[i % NRR]
                    S.op(ACT, lambda e: e.activation(out=r_t[:, 0:wdt], in_=PB[pbi][:, 0:wdt], func=AF.Relu,
                                                     scale=wabs[:, qb, h:h + 1]),
                         reads=[PBb[pbi], B_wi], writes=[B_r])

                def stC(i):
                    n, h = steps[i]
                    wdt = min(512, N - n * 512)
                    r_t, B_r = rr[i % NRR], B_rr[i % NRR]
                    psc, pscb = PB[4 + n % 2], PBb[4 + n % 2]
                    S.op(PE, lambda e: e.matmul(psc[:, 0:wdt], lhsT=dg[:, h, :], rhs=r_t[:, 0:wdt], start=(h == 0), stop=False),
                         reads=[B_dg, B_r], writes=[pscb])
                    if h == 15:
                        last = (n == ngrp - 1)
                        sgm = min(n // 2, 3)
                        S.op(PE, lambda e: e.matmul(psc[:, 0:wdt], lhsT=kneg_b[0:1, sgm * 128:(sgm + 1) * 128],
                                                    rhs=ones512[0:1, 0:wdt], start=False, stop=(not last)),
                             reads=[B_const], writes=[pscb])
                        if last:
                            S.op(PE, lambda e: e.matmul(psc[:, wdt - 128:wdt], lhsT=diag_b[0:1, 0:128], rhs=diag_b[0:1, 128:256],
                                                        start=False, stop=True), reads=[B_const], writes=[pscb])
                        S.op(ACT, lambda e: e.activation(out=scb[:, n * 512:n * 512 + wdt], in_=psc[:, 0:wdt], func=AF.Copy),
                             reads=[pscb], writes=[B_sc])
                run_pipeline(len(steps), [stA, stB, stC], [0, 0, 3])

            def search(qb):
                scb, B_sc = scb2[qb % 2], B_sc2[qb % 2]
                nkt = 24 + qb + 1
                N = nkt * 128
                S.op(DVE, lambda e: e.tensor_reduce(out=sv[:, 0:1], in_=scb[:, 0:N], axis=AX.X, op=ALU.max),
                     reads=[B_sc], writes=[B_sv])
                S.op(DVE, lambda e: e.tensor_reduce(out=sv[:, 8:11], in_=scb[:, 0:NCTX].rearrange("p (s n) -> p s n", s=3),
                                                    axis=AX.X, op=ALU.min), reads=[B_sc], writes=[B_sv])
                if qb > 0:
                    S.op(DVE, lambda e: e.tensor_reduce(out=sv[:, 11:12], in_=scb[:, NCTX:NCTX + qb * 128],
                                                        axis=AX.X, op=ALU.min), reads=[B_sc], writes=[B_sv])
                else:
                    S.op(DVE, lambda e: e.memset(sv[:, 11:12], 3.0e38), writes=[B_sv])
                S.op(DVE, lambda e: e.tensor_reduce(out=sv[:, 12:14],
                                                    in_=scb[:, N - 128:N].rearrange("p (s n) -> p s n", s=2),
                                                    axis=AX.X, op=ALU.min), reads=[B_sc], writes=[B_sv])
                S.op(DVE, lambda e: e.tensor_tensor(out=sv[:, 16:22], in0=sv[:, 8:14], in1=pen[:], op=ALU.add),
                     reads=[B_sv, B_const], writes=[B_sv])
                S.op(DVE, lambda e: e.tensor_reduce(out=sv[:, 1:2], in_=sv[:, 16:22], axis=AX.X, op=ALU.min),
                     reads=[B_sv], writes=[B_sv])
                S.op(DVE, lambda e: e.tensor_tensor(out=sv[:, 2:3], in0=sv[:, 0:1], in1=sv[:, 1:2], op=ALU.subtract),
                     reads=[B_sv], writes=[B_sv])
                S.op(DVE, lambda e: e.tensor_scalar(out=sv[:, 2:3], in0=sv[:, 2:3], scalar1=1.002, scalar2=1e-6,
                                                    op0=ALU.mult, op1=ALU.add), reads=[B_sv], writes=[B_sv])
                S.op(DVE, lambda e: e.tensor_scalar(out=wtab[:], in0=pow2[:], scalar1=sv[:, 2:3], scalar2=None,
                                                    op0=ALU.mult), reads=[B_sv, B_const], writes=[B_sv])
                S.op(DVE, lambda e: e.tensor_tensor(out=sv[:, 1:2], in0=sv[:, 0:1], in1=sv[:, 2:3], op=ALU.subtract),
                     reads=[B_sv], writes=[B_sv])
                S.op(DVE, lambda e: e.tensor_tensor(out=sv[:, 3:4], in0=sv[:, 1:2], in1=wtab[:, 0:1], op=ALU.add),
                     reads=[B_sv], writes=[B_sv])
                for k in range(NITER):
                    S.op(DVE, lambda e: e.tensor_scalar(out=mk[:, 0:N], in0=scb[:, 0:N], scalar1=sv[:, 3:4],
                                                        scalar2=0.0, op0=ALU.is_ge, op1=ALU.add, accum_out=sv[:, 4:5]),
                         reads=[B_sc, B_sv], writes=[B_mk, B_sv])
                    S.op(DVE, lambda e: e.tensor_scalar(out=sv[:, 5:6], in0=sv[:, 4:5], scalar1=255.5, scalar2=0.5,
                                                        op0=ALU.is_ge, op1=ALU.subtract), reads=[B_sv], writes=[B_sv])
                    S.op(DVE, lambda e, k=k: e.scalar_tensor_tensor(out=sv[:, 3:4], in0=sv[:, 5:6], scalar=wtab[:, k:k + 1],
                                                                    in1=sv[:, 3:4], op0=ALU.mult, op1=ALU.add),
                         reads=[B_sv], writes=[B_sv])
                S.op(DVE, lambda e: e.tensor_tensor(out=sv[:, 6:7], in0=sv[:, 3:4], in1=wtab[:, NITER:NITER + 1],
                                                    op=ALU.subtract), reads=[B_sv], writes=[B_sv])
                S.op(DVE, lambda e: e.tensor_copy(out=thr_dbg[:, qb:qb + 1], in_=sv[:, 6:7]),
                     reads=[B_sv], writes=[B_thr])
                S.op(DVE, lambda e: e.tensor_scalar(out=mk[:, 0:N], in0=scb[:, 0:N], scalar1=sv[:, 6:7],
                                                    scalar2=None, op0=ALU.is_ge), reads=[B_sc, B_sv], writes=[B_mk])
                if qb == 7:
                    tap("scb", scb[:], [128, NK], F32, [B_sc])

            def mask_transposes(qb, i):
                nkt = 24 + qb + 1
                for kt0 in range(0, nkt, 8):
                    nk8 = min(8, nkt - kt0)
                    pt = PT[0]
                    ptb = PTb[0]
                    for a_ in range(nk8):
                        S.op(PE, lambda e, a_=a_: e.transpose(
                            out=pt[:, a_ * 128:(a_ + 1) * 128], in_=mk[:, (kt0 + a_) * 128:(kt0 + a_ + 1) * 128],
                            identity=identb[:]), reads=[B_mk, B_const], writes=[ptb])
                    S.op(ACT, lambda e: e.activation(
                        out=maskT[:, kt0:kt0 + nk8, i * 128:(i + 1) * 128],
                        in_=pt[:, 0:nk8 * 128].rearrange("p (a q) -> p a q", q=128), func=AF.Copy),
                        reads=[ptb], writes=[B_maskT])

            def attention(s):
                nkt_s = 24 + (s + 1) * SBQ
                qs0 = s * NQ
                S.op(POOL, lambda e: e.tensor_copy(out=qpad[0:64, :, 0, :], in_=qT[0:64, :, qs0:qs0 + NQ]),
                     reads=[B_q], writes=[B_qpad])
                S.op(POOL, lambda e: e.tensor_copy(out=qpad[64:128, :, 1, :], in_=qT[64:128, :, qs0:qs0 + NQ]),
                     reads=[B_q], writes=[B_qpad])
                steps = [(hc, kt) for hc in range(8) for kt in range(nkt_s)]
                kv_view = v_d.rearrange("(kt p) n -> p kt n", p=128)

                def load_kv(hc, half):
                    k0, k1 = (0, 16) if half == 0 else (16, 32)
                    S.dma(SP, lambda e: e.dma_start(out=kTc[:, k0 * 128:k1 * 128], in_=kT_d[hc][:, k0 * 128:k1 * 128]),
                          reads=[B_kTd], writes=[B_kTc[half]])
                    S.dma(SP, lambda e: e.dma_start(out=vc[:, k0:k1, :], in_=kv_view[:, k0:k1, hc * 160:(hc + 1) * 160]),
                          reads=[B_vd], writes=[B_vc[half]])

                def stA(i):
                    hc, kt = steps[i]
                    sl = i % NQK
                    S.op(PE, lambda e: e.matmul(qk_slot[sl], lhsT=kTc[:, kt * 128:(kt + 1) * 128],
                                                rhs=qpad[:, hc, :, :], start=True, stop=True),
                         reads=[B_kTc[kt // 16], B_qpad], writes=[B_qk[sl]])

                def stB(i):
                    sl = i % NQK
                    S.op(ACT, lambda e: e.activation(out=et[i % NET][:, :, :], in_=qk_slot[sl].rearrange("p (a q) -> p a q", a=2),
                                                     func=AF.Exp, scale=0.125),
                         reads=[B_qk[sl]], writes=[B_et[i % NET]])

                def stC(i):
                    hc, kt = steps[i]
                    S.op(DVE, lambda e: e.tensor_tensor(out=ptl[i % NET][:, :, :], in0=et[i % NET][:, :, :],
                                                        in1=maskT[:, kt:kt + 1, :].broadcast_to((128, 2, NQ)), op=ALU.mult),
                         reads=[B_et[i % NET], B_maskT], writes=[B_ptl[i % NET]])

                def stD(i):
                    hc, kt = steps[i]
                    for hh in range(2):
                        S.op(PE, lambda e, hh=hh: e.matmul(PB[hh][0:(65 if hh == 0 else 128), 0:NQ],
                                                           lhsT=(vc[:, kt, 0:65] if hh == 0 else vc[:, kt, 32:160]),
                                                           rhs=ptl[i % NET][:, hh, :], start=(kt == 0), stop=(kt == nkt_s - 1)),
                             reads=[B_vc[kt // 16], B_ptl[i % NET]], writes=[PBb[hh]])
                    if kt == 15 and hc + 1 < 8:
                        load_kv(hc + 1, 0)
                    if kt == nkt_s - 1:
                        if hc + 1 < 8:
                            load_kv(hc + 1, 1)
                        normalize(hc, 0)
                        normalize(hc, 1)

                def normalize(hc, hh):
                    hb = hh * 64
                    po, pob = PB[hh], PBb[hh]
                    dp = 64 if hh == 0 else 32
                    S.op(DVE, lambda e: e.reciprocal(out=rd[dp:dp + 1, :], in_=po[dp:dp + 1, 0:NQ]),
                         reads=[pob], writes=[B_rd])
                    S.op(PE, lambda e: e.matmul(PB[5][:, 0:NQ], lhsT=onesf[dp:dp + 1, 0:128], rhs=rd[dp:dp + 1, :],
                                                start=True, stop=True), reads=[B_rd, B_const], writes=[PBb[5]])
                    S.op(ACT, lambda e: e.activation(out=rbc[:, :], in_=PB[5][:, 0:NQ], func=AF.Copy),
                         reads=[PBb[5]], writes=[B_rbc])
                    S.op(DVE, lambda e: e.tensor_tensor(out=attT[hb:hb + 64, hc, qs0:qs0 + NQ], in0=po[hb:hb + 64, 0:NQ],
                                                        in1=rbc[hb:hb + 64, :], op=ALU.mult),
                         reads=[pob, B_rbc], writes=[B_att])
                load_kv(0, 0)
                load_kv(0, 1)
                run_pipeline(len(steps), [stA, stB, stC, stD], [0, 0, 0, 3])

            nsb = 8 // SBQ
            indexer(0)
            for s in range(nsb):
                S.op(POOL, lambda e: e.memset(maskT[:], 0.0), writes=[B_maskT])
                for i in range(SBQ):
                    qb = s * SBQ + i
                    search(qb)
                    if qb + 1 < 8:
                        indexer(qb + 1)
                    for m_ in range(32 + qb * 8, 32 + (qb + 1) * 8):
                        mod_rest(m_)
                    mask_transposes(qb, i)
                attention(s)
            S.op(DVE, lambda e: e.tensor_tensor(out=modT[:, 32:96], in0=PTf[:, 32:96], in1=badd[:, 32:96], op=ALU.add),
                 reads=[PTb[1], Bc, B_mod], writes=[B_mod])
            S.op(DVE, lambda e: e.scalar_tensor_tensor(out=s2[:], in0=modT[:, 64:80], scalar=1.0, in1=smallv[:, 32:48],
                                                       op0=ALU.add, op1=ALU.mult), reads=[B_mod, B_const], writes=[B_mod])
            S.op(DVE, lambda e: e.tensor_tensor(out=smallv[:, 64:80], in0=modT[:, 32:48], in1=smallv[:, 16:32], op=ALU.mult),
                 reads=[B_mod, B_const], writes=[B_mod])
            S.op(DVE, lambda e: e.tensor_tensor(out=smallv[:, 80:96], in0=modT[:, 80:96], in1=smallv[:, 48:64], op=ALU.mult),
                 reads=[B_mod, B_const], writes=[B_mod])
            B_gsc = Buf("gsc")
            for i in range(2):
                S.dma(SP, lambda e, i=i: e.dma_start(out=gsc_d[i].rearrange("(c p) -> p c", p=128),
                                                     in_=smallv[:, 64 + 16 * i:80 + 16 * i], allow_slow_non_contiguous=True),
                      reads=[B_mod], writes=[B_gsc])
            tap("modT", modT[:], [128, 96], F32, [B_mod])
            tap("thr", thr_dbg[:], [128, 8], F32, [B_thr])
            tap("attT", attT[:], [128, 8, NOWN], BF16, [B_att])
            S.barrier()
            if limit == 4:
                raise _Stop()

        uact = sb(att_stack, "uact", [128, 8, NOWN], BF16)
        B_uact = Buf("uact")
        with Scope(arena) as st:
            sc_hown = Scope(arena)
            hT_own = sb(sc_hown, "hT_own2", [128, 16, NOWN], BF16)
            S.dma(SP, lambda e: e.dma_start(out=hT_own[:].rearrange("p c n -> p (c n)"), in_=hown_d), reads=[B_hownd], writes=B_hown)
            uT = sb(st, "uT", [128, 8, 32 + NOWN], BF16)
            B_uT = Buf()
            st2 = Scope(arena)
            sg = sb(st2, "sg", [128, 8, 32 + NOWN], BF16)
            B_sg = Buf()
            wblk = [sb(st2, f"wblkb{i}", [128, 16, 512], BF16) for i in range(2)]
            Bw2 = [Buf(), Buf()]
            glu_segs = [(lambda kc: hT_halo[:, kc, :], 32)] + own_segs
            seg_off = [0, 32, 32 + 512]

            def cons_sg(ch, si, pb, pbb):
                wd = glu_segs[si][1]
                S.op(ACT, lambda e: e.activation(out=sg[:, ch, seg_off[si]:seg_off[si] + wd], in_=pb[:, 0:wd], func=AF.Sigmoid),
                     reads=[pbb], writes=[B_sg])

            def cons_u(ch, si, pb, pbb):
                wd = glu_segs[si][1]
                S.op(DVE, lambda e: e.tensor_tensor(out=uT[:, ch, seg_off[si]:seg_off[si] + wd], in0=pb[:, 0:wd],
                                                    in1=sg[:, ch, seg_off[si]:seg_off[si] + wd], op=ALU.mult),
                     reads=[pbb, B_sg], writes=[B_uT])
            proj_fm(st, wv_in, O_GLU_G, 8, glu_segs, cons_sg, Bw2, wblk, B_hown + [B_halo])
            proj_fm(st, wv_in, O_GLU_A, 8, glu_segs, cons_u, Bw2, wblk, B_hown + [B_halo])
            S.op(DVE, lambda e: e.tensor_scalar(out=uT[:, :, 0:32], in0=uT[:, :, 0:32], scalar1=halov[:, 0:1], scalar2=None,
                                                op0=ALU.mult), reads=[B_uT, B_const], writes=[B_uT])
            tap("uT", uT[:], [128, 8, 32 + NOWN], BF16, [B_uT])
            S.barrier()
            if limit == 5:
                raise _Stop()
            st2.close()
            yb = sb(st, "yb", [128, 8, NOWN], F32)
            ysq = sb(st, "ysq", [128, 8, NOWN], F32)
            B_y = Buf()
            dgc = [sb(st, f"dgc{i}", [128, 31, 128], BF16) for i in range(2)]
            B_dgc = [Buf(), Buf()]
            mu = sb(st, "mu", [128, 512], F32)
            var = sb(st, "var", [128, 512], F32)
            rstd = sb(st, "rstd", [128, 512], F32)
            B_ln = Buf()
            zt = sb(st, "zt", [128, 512], F32)
            B_z = Buf()
            for ch in range(8):
                dg_t, Bdg = dgc[ch % 2], B_dgc[ch % 2]
                for tp in range(31):
                    engd = DVE if tp % 2 == 0 else POOL
                    S.op(engd, lambda e, tp=tp: e.tensor_scalar(out=dg_t[:, tp, :], in0=identb[:],
                                                                scalar1=wdw[:, ch * 31 + tp:ch * 31 + tp + 1], scalar2=None,
                                                                op0=ALU.mult), reads=[B_const], writes=[Bdg])
                for g in range(2):
                    off = g * 512 + 2
                    pbi = 2 + (ch * 2 + g) % 2
                    for tp in range(31):
                        S.op(PE, lambda e, tp=tp: e.matmul(PB[pbi][:, :], lhsT=dg_t[:, tp, :], rhs=uT[:, ch, off + tp:off + tp + 512],
                                                           start=(tp == 0), stop=(tp == 30)),
                             reads=[Bdg, B_uT], writes=[PBb[pbi]])
                    S.op(ACT, lambda e: e.activation(out=yb[:, ch, g * 512:(g + 1) * 512], in_=PB[pbi][:, :], func=AF.Identity,
                                                     bias=convv[:, ch:ch + 1], scale=1.0),
                         reads=[PBb[pbi], B_const], writes=[B_y])
                    S.op(ACT, lambda e: e.activation(out=ysq[:, ch, g * 512:(g + 1) * 512], in_=PB[pbi][:, :], func=AF.Square,
                                                     bias=convv[:, ch:ch + 1], scale=1.0),
                         reads=[PBb[pbi], B_const], writes=[B_y])
            for g in range(2):
                gs = slice(g * 512, (g + 1) * 512)
                for ch in range(8):
                    S.op(PE, lambda e, ch=ch: e.matmul(PB[0][:, :], lhsT=onesf[:], rhs=yb[:, ch, gs], start=(ch == 0),
                                                       stop=(ch == 7)), reads=[B_y, B_const], writes=[PBb[0]])
                for ch in range(8):
                    S.op(PE, lambda e, ch=ch: e.matmul(PB[1][:, :], lhsT=onesf[:], rhs=ysq[:, ch, gs], start=(ch == 0),
                                                       stop=(ch == 7)), reads=[B_y, B_const], writes=[PBb[1]])
                S.op(ACT, lambda e: e.activation(out=mu[:], in_=PB[0][:, :], func=AF.Copy, scale=1.0 / 1024),
                     reads=[PBb[0]], writes=[B_ln])
                S.op(DVE, lambda e: e.tensor_tensor(out=var[:], in0=mu[:], in1=mu[:], op=ALU.mult), reads=[B_ln], writes=[B_ln])
                S.op(DVE, lambda e: e.scalar_tensor_tensor(out=var[:], in0=PB[1][:, :], scalar=1.0 / 1024, in1=var[:],
                                                           op0=ALU.mult, op1=ALU.subtract), reads=[PBb[1], B_ln], writes=[B_ln])
                S.op(ACT, lambda e: e.activation(out=rstd[:], in_=var[:], func=AF.Sqrt, bias=epsc[:, 0:1], scale=1.0),
                     reads=[B_ln, B_const], writes=[B_ln])
                S.op(DVE, lambda e: e.reciprocal(out=rstd[:], in_=rstd[:]), reads=[B_ln], writes=[B_ln])
                for ch in range(8):
                    S.op(DVE, lambda e, ch=ch: e.tensor_tensor(out=zt[:], in0=yb[:, ch, gs], in1=mu[:], op=ALU.subtract),
                         reads=[B_y, B_ln], writes=[B_z])
                    S.op(DVE, lambda e: e.tensor_tensor(out=zt[:], in0=zt[:], in1=rstd[:], op=ALU.mult),
                         reads=[B_ln], writes=[B_z])
                    S.op(ACT, lambda e, ch=ch: e.activation(out=uact[:, ch, gs], in_=zt[:], func=AF.Silu,
                                                            scale=convv[:, 8 + ch:9 + ch], bias=convv[:, 16 + ch:17 + ch]),
                         reads=[B_z, B_const], writes=[B_uact])
            tap("uact", uact[:], [128, 8, NOWN], BF16, [B_uact])
            S.barrier()
            if limit == 6:
                raise _Stop()

        wv_co = w_co.rearrange("(c p) n -> p c n", p=128)
        wv_ao = w_ao.rearrange("(c p) n -> p c n", p=128)
        wv_o = w_o.rearrange("(c p) n -> p c n", p=128)
        wv_g = w_gate.rearrange("(c p) n -> p c n", p=128)
        wv_u = w_up.rearrange("(c p) n -> p c n", p=128)
        wv_d = w_down.rearrange("(c p) n -> p c n", p=128)
        sc_kiT.close()
        sc_halo.close()
        wslot = [0, 0, 0]

        def wload(slots, Bs, which, dst_view_fn, src_ap, nsplit=2):
            i = wslot[which] % 2
            wslot[which] += 1
            w_t = slots[i]
            n0 = src_ap.shape[1]
            step = (n0 + nsplit - 1) // nsplit
            for a in range(0, n0, step):
                b = min(n0, a + step)
                S.dma(POOL, lambda e, a=a, b=b, w_t=w_t: e.dma_start(out=dst_view_fn(w_t)[:, a:b, :], in_=src_ap[:, a:b, :]),
                      writes=[Bs[i]])
            return w_t, Bs[i]

        sc_mg = Scope(arena)
        mergedT = sb(sc_mg, "mergedT", [128, 16, NOWN], BF16)
        B_mg = Buf()
        with Scope(arena) as st:
            Wa = [sb(st, f"Wa{i}", [128, 16, 512], BF16) for i in range(2)]
            B_Wa = [Buf(), Buf()]
            Wb = [sb(st, f"Wb{i}", [128, 16, 512], BF16) for i in range(2)]
            B_Wb = [Buf(), Buf()]
            tA = sb(st, "tA", [128, 512], F32)
            tB = sb(st, "tB", [128, 512], BF16)
            tC = sb(st, "tC", [128, 512], BF16)
            B_t = [Buf(), Buf(), Buf()]
            for nb in range(4):
                cs = slice(nb * 512, (nb + 1) * 512)
                Wgc, Bgc = wload(Wa, B_Wa, 0, lambda w: w[:, :, :], wv_in[:, :, O_GC + nb * 512:O_GC + (nb + 1) * 512])
                Wga, Bga = wload(Wa, B_Wa, 0, lambda w: w[:, :, :], wv_in[:, :, O_GA + nb * 512:O_GA + (nb + 1) * 512])
                Wco, Bco = wload(Wb, B_Wb, 1, lambda w: w[:, 0:8, :], wv_co[:, :, cs])
                Wao, Bao = wload(Wb, B_Wb, 1, lambda w: w[:, 0:8, :], wv_ao[:, :, cs])
                for g in range(2):
                    gs = slice(g * 512, (g + 1) * 512)
                    for cc in range(4):
                        f = nb * 4 + cc
                        fs = slice(cc * 128, (cc + 1) * 128)
                        for kc in range(KC):
                            S.op(PE, lambda e, kc=kc, fs=fs, Wgc=Wgc, gs=gs: e.matmul(PB[0][:, :], lhsT=Wgc[:, kc, fs], rhs=hT_own[:, kc, gs],
                                                                                   start=(kc == 0), stop=(kc == KC - 1)),
                                 reads=[Bgc] + B_hown, writes=[PBb[0]])
                        for kc in range(KC):
                            S.op(PE, lambda e, kc=kc, fs=fs, Wga=Wga, gs=gs: e.matmul(PB[1][:, :], lhsT=Wga[:, kc, fs], rhs=hT_own[:, kc, gs],
                                                                                   start=(kc == 0), stop=(kc == KC - 1)),
                                 reads=[Bga] + B_hown, writes=[PBb[1]])
                        for kc in range(8):
                            S.op(PE, lambda e, kc=kc, fs=fs, Wco=Wco, gs=gs: e.matmul(PB[2][:, :], lhsT=Wco[:, kc, fs], rhs=uact[:, kc, gs],
                                                                                   start=(kc == 0), stop=(kc == 7)),
                                 reads=[Bco, B_uact], writes=[PBb[2]])
                        for kc in range(8):
                            S.op(PE, lambda e, kc=kc, fs=fs, Wao=Wao, gs=gs: e.matmul(PB[3][:, :], lhsT=Wao[:, kc, fs], rhs=attT[:, kc, gs],
                                                                                   start=(kc == 0), stop=(kc == 7)),
                                 reads=[Bao, B_att], writes=[PBb[3]])
                        S.op(ACT, lambda e: e.activation(out=tB[:], in_=PB[0][:, :], func=AF.Sigmoid), reads=[PBb[0]], writes=[B_t[1]])
                        S.op(ACT, lambda e: e.activation(out=tC[:], in_=PB[1][:, :], func=AF.Sigmoid), reads=[PBb[1]], writes=[B_t[2]])
                        S.op(DVE, lambda e: e.tensor_tensor(out=tA[:], in0=PB[2][:, :], in1=tB[:], op=ALU.mult),
                             reads=[PBb[2], B_t[1]], writes=[B_t[0]])
                        S.op(DVE, lambda e: e.tensor_tensor(out=tB[:], in0=PB[3][:, :], in1=tC[:], op=ALU.mult),
                             reads=[PBb[3], B_t[2]], writes=[B_t[1]])
                        S.op(DVE, lambda e, f=f, gs=gs: e.tensor_tensor(out=mergedT[:, f, gs], in0=tA[:], in1=tB[:], op=ALU.add),
                             reads=[B_t[0], B_t[1]], writes=[B_mg])
            tap("mergedT", mergedT[:], [128, 16, NOWN], BF16, [B_mg])
            S.barrier()
            if limit == 7:
                raise _Stop()
        att_stack.close()
        sc_hown.close()

        sc_h2 = Scope(arena)
        h2T = sb(sc_h2, "h2T", [128, 16, NOWN], BF16)
        B_h2 = Buf()
        B_out = Buf("out")
        with Scope(arena) as st:
            mx = sb(st, "mx", [128, 8, D], F32)
            B_mx = [Buf() for _ in range(8)]
            ssp = sb(st, "ssp", [128, 8, 4], F32)
            B_ss = [Buf() for _ in range(8)]
            tC = sb(st, "tC2", [128, 512], BF16)
            B_tc = Buf()
            with Scope(arena) as stw:
                Wa = [sb(stw, f"Wo{i}", [128, 16, 512], BF16) for i in range(2)]
                B_Wa = [Buf(), Buf()]
                for nb in range(4):
                    Wo, Bo = wload(Wa, B_Wa, 0, lambda w: w[:, :, :], wv_o[:, :, nb * 512:(nb + 1) * 512])
                    for t in range(8):
                        pbi = 2 + (t % 4)
                        tok = slice(t * 128, (t + 1) * 128)
                        for kc in range(KC):
                            S.op(PE, lambda e, kc=kc: e.matmul(PB[pbi][:, :], lhsT=mergedT[:, kc, tok], rhs=Wo[:, kc, :],
                                                               start=(kc == 0), stop=(kc == KC - 1)),
                                 reads=[Bo, B_mg], writes=[PBb[pbi]])
                        S.op(ACT, lambda e: e.activation(out=mx[:, t, nb * 512:(nb + 1) * 512], in_=PB[pbi][:, :], func=AF.Copy),
                             reads=[PBb[pbi]], writes=[B_mx[t]])
                        S.op(ACT, lambda e: e.activation(out=tC[:], in_=PB[pbi][:, :], func=AF.Square,
                                                         accum_out=ssp[:, t, nb:nb + 1]),
                             reads=[PBb[pbi]], writes=[B_tc, B_ss[t]])
                S.barrier()
                if limit == 8:
                    raise _Stop()
            with Scope(arena) as stf:
                G1 = sb(stf, "G1", [128, D], F32)
                B_G = Buf()
                S.dma(SP, lambda e: e.dma_start(out=G1[:], in_=gsc_d[0].partition_broadcast(128)), reads=[B_gsc], writes=[B_G])
                ss1_ = sb(stf, "ss1", [128, 8, 16], F32)
                xt = [sb(stf, f"xtb{i}", [128, D], F32) for i in range(2)]
                B_xt = [Buf(), Buf()]
                x1t = [sb(stf, f"x1t{i}", [128, D], F32) for i in range(2)]
                B_x1 = [Buf(), Buf()]
                xn2 = [sb(stf, f"xn2{i}", [128, D], BF16) for i in range(2)]
                B_xn2 = [Buf(), Buf()]
                for t in range(8):
                    ss1 = ss1_[:, t, :]
                    xb = xt[t % 2]
                    x1 = x1t[t % 2]
                    xn = xn2[t % 2]
                    Bxn = B_xn2[t % 2]
                    r0 = NCTX + t * 128
                    o0 = t * 128
                    S.dma(SP, lambda e: e.dma_start(out=xb[:], in_=xctx[r0:r0 + 128, :]), writes=[B_xt[t % 2]])
                    S.op(DVE, lambda e: e.tensor_reduce(out=ss1[:, 0:1], in_=ssp[:, t, :], axis=AX.X, op=ALU.add),
                         reads=[B_ss[t]], writes=[B_ss[t]])
                    rstd_from_ssq(ss1[:, 2:3], ss1[:, 0:1], ss1[:, 1:2], [B_ss[t]], [B_ss[t]])
                    S.op(DVE, lambda e: e.scalar_tensor_tensor(out=mx[:, t, :], in0=mx[:, t, :], scalar=ss1[:, 2:3], in1=G1[:],
                                                               op0=ALU.mult, op1=ALU.mult),
                         reads=[B_ss[t], B_G], writes=[B_mx[t]])
                    S.op(DVE, lambda e: e.tensor_tensor(out=x1[:], in0=mx[:, t, :], in1=xb[:], op=ALU.add),
                         reads=[B_mx[t], B_xt[t % 2]], writes=[B_x1[t % 2]])
                    S.dma(SP, lambda e: e.dma_start(out=out[o0:o0 + 128, :], in_=x1[:]), reads=[B_x1[t % 2]],
                          writes=[B_out])
                    S.op(ACT, lambda e: e.activation(out=xn[:], in_=x1[:], func=AF.Square, accum_out=ss1[:, 4:5]),
                         reads=[B_x1[t % 2]], writes=[Bxn, B_ss[t]])
                    rstd_from_ssq(ss1[:, 6:7], ss1[:, 4:5], ss1[:, 5:6], [B_ss[t]], [B_ss[t]])
                    S.op(ACT, lambda e: e.activation(out=xn[:], in_=x1[:], func=AF.Copy, scale=ss1[:, 6:7]),
                         reads=[B_x1[t % 2], B_ss[t]], writes=[Bxn])
                    for c4 in range(4):
                        pt = PT[c4 % 2]
                        for cc in range(4):
                            c = c4 * 4 + cc
                            S.op(PE, lambda e, c=c, cc=cc: e.transpose(out=pt[:, cc * 128:(cc + 1) * 128], in_=xn[:, c * 128:(c + 1) * 128],
                                                                     identity=identb[:]), reads=[Bxn, B_const], writes=[PTb[c4 % 2]])
                        for cc in range(4):
                            c = c4 * 4 + cc
                            if c4 % 2 == 0:
                                S.op(ACT, lambda e, c=c, cc=cc: e.activation(out=h2T[:, c, o0:o0 + 128], in_=pt[:, cc * 128:(cc + 1) * 128],
                                                                            func=AF.Identity, scale=s2[:, c:c + 1],
                                                                            bias=modT[:, 48 + c:49 + c]),
                                     reads=[PTb[c4 % 2], B_mod], writes=[B_h2])
                            else:
                                S.op(DVE, lambda e, c=c, cc=cc: e.tensor_scalar(out=h2T[:, c, o0:o0 + 128], in0=pt[:, cc * 128:(cc + 1) * 128],
                                                                               scalar1=s2[:, c:c + 1], scalar2=modT[:, 48 + c:49 + c],
                                                                               op0=ALU.mult, op1=ALU.add),
                                     reads=[PTb[c4 % 2], B_mod], writes=[B_h2])
                tap("h2T", h2T[:], [128, 16, NOWN], BF16, [B_h2])
                S.barrier()
                if limit == 9:
                    raise _Stop()
        sc_mg.close()

        with Scope(arena) as st:
            mx = sb(st, "mx3", [128, 8, D], F32)
            B_mx = [Buf() for _ in range(8)]
            with Scope(arena) as stw:
                Wa = [sb(stw, f"Wg{i}", [128, 16, 256], BF16) for i in range(2)]
                B_Wa = [Buf(), Buf()]
                Wb = [sb(stw, f"Wu{i}", [128, 16, 256], BF16) for i in range(2)]
                B_Wb = [Buf(), Buf()]
                Wd_t = [sb(stw, f"Wd{i}", [128, 11, 512], BF16) for i in range(2)]
                B_Wd = [Buf(), Buf()]
                tBs = [sb(stw, f"tB3{i}", [128, 512], BF16) for i in range(2)]
                B_tb = [Buf(), Buf()]
                aT = sb(stw, "aT", [128, 11, NOWN], BF16)
                B_aT = Buf()
                stepi = 0
                for Q in range(4):
                    for j2 in range(6):
                        nch = 2 if j2 < 5 else 1
                        c0 = (Q * 11 + j2 * 2) * 128
                        Wg, Bg = wload(Wa, B_Wa, 0, lambda w: w[:, :, 0:nch * 128], wv_g[:, :, c0:c0 + nch * 128])
                        Wu, Bu = wload(Wb, B_Wb, 1, lambda w: w[:, :, 0:nch * 128], wv_u[:, :, c0:c0 + nch * 128])
                        for cc in range(nch):
                            j = j2 * 2 + cc
                            fs = slice(cc * 128, (cc + 1) * 128)
                            for half in range(2):
                                gs = slice(half * 512, (half + 1) * 512)
                                pg, pu = (0, 1) if stepi % 2 == 0 else (4, 5)
                                tB, Btb = tBs[stepi % 2], B_tb[stepi % 2]
                                stepi += 1
                                for kc in range(KC):
                                    S.op(PE, lambda e, kc=kc: e.matmul(PB[pg][:, :], lhsT=Wg[:, kc, fs], rhs=h2T[:, kc, gs],
                                                                       start=(kc == 0), stop=(kc == KC - 1)),
                                         reads=[Bg, B_h2], writes=[PBb[pg]])
                                for kc in range(KC):
                                    S.op(PE, lambda e, kc=kc: e.matmul(PB[pu][:, :], lhsT=Wu[:, kc, fs], rhs=h2T[:, kc, gs],
                                                                       start=(kc == 0), stop=(kc == KC - 1)),
                                         reads=[Bu, B_h2], writes=[PBb[pu]])
                                S.op(ACT, lambda e: e.activation(out=tB[:], in_=PB[pg][:, :], func=AF.Silu), reads=[PBb[pg]], writes=[Btb])
                                S.op(DVE, lambda e: e.tensor_tensor(out=aT[:, j, gs], in0=PB[pu][:, :], in1=tB[:], op=ALU.mult),
                                     reads=[PBb[pu], Btb], writes=[B_aT])
                    for nb in range(4):
                        Wd, Bd = wload(Wd_t, B_Wd, 2, lambda w: w[:, :, :],
                                       wv_d[:, Q * 11:(Q + 1) * 11, nb * 512:(nb + 1) * 512])
                        for t in range(8):
                            pbi = 2 + (t % 2)
                            for j in range(11):
                                S.op(PE, lambda e, j=j: e.matmul(PB[pbi][:, :], lhsT=aT[:, j, t * 128:(t + 1) * 128], rhs=Wd[:, j, :],
                                                                 start=(j == 0), stop=(j == 10)), reads=[Bd, B_aT], writes=[PBb[pbi]])
                            dst = mx[:, t, nb * 512:(nb + 1) * 512]
                            if Q == 0:
                                S.op(ACT, lambda e: e.activation(out=dst, in_=PB[pbi][:, :], func=AF.Copy),
                                     reads=[PBb[pbi]], writes=[B_mx[t]])
                            else:
                                S.op(DVE, lambda e: e.tensor_tensor(out=dst, in0=PB[pbi][:, :], in1=dst, op=ALU.add),
                                     reads=[PBb[pbi]], writes=[B_mx[t]])
                S.barrier()
                if limit == 10:
                    raise _Stop()
            with Scope(arena) as stf:
                G2 = sb(stf, "G2", [128, D], F32)
                B_G = Buf()
                S.dma(SP, lambda e: e.dma_start(out=G2[:], in_=gsc_d[1].partition_broadcast(128)), reads=[B_gsc], writes=[B_G])
                ss1_ = sb(stf, "ss3", [128, 8, 16], F32)
                B_ss = [Buf() for _ in range(8)]
                x1t = [sb(stf, f"x1t3{i}", [128, D], F32) for i in range(2)]
                B_x1 = [Buf(), Buf()]
                xn2 = sb(stf, "xn3", [128, D], BF16)
                B_xn2 = Buf()
                for t in range(8):
                    ss1 = ss1_[:, t, :]
                    x1 = x1t[t % 2]
                    o0 = t * 128
                    S.dma(SP, lambda e: e.dma_start(out=x1[:], in_=out[o0:o0 + 128, :]), reads=[B_out],
                          writes=[B_x1[t % 2]])
                    S.op(ACT, lambda e: e.activation(out=xn2[:], in_=mx[:, t, :], func=AF.Square, accum_out=ss1[:, 8:9]),
                         reads=[B_mx[t]], writes=[B_xn2, B_ss[t]])
                    rstd_from_ssq(ss1[:, 10:11], ss1[:, 8:9], ss1[:, 9:10], [B_ss[t]], [B_ss[t]])
                    S.op(DVE, lambda e: e.scalar_tensor_tensor(out=mx[:, t, :], in0=mx[:, t, :], scalar=ss1[:, 10:11], in1=G2[:],
                                                               op0=ALU.mult, op1=ALU.mult),
                         reads=[B_ss[t], B_G], writes=[B_mx[t]])
                    S.op(DVE, lambda e: e.tensor_tensor(out=x1[:], in0=mx[:, t, :], in1=x1[:], op=ALU.add),
                         reads=[B_mx[t]], writes=[B_x1[t % 2]])
                    S.dma(SP, lambda e: e.dma_start(out=out[o0:o0 + 128, :], in_=x1[:]), reads=[B_x1[t % 2]],
                          writes=[B_out])
                S.barrier()
        sc_h2.close()
    except _Stop:
        S.barrier()
    stack.close()
    build.last_sched = S
    return nc, dbg_outs


def _fm(v, nch):
    return np.ascontiguousarray(np.asarray(v, np.float32).reshape(nch, 128).T)


def make_in_maps(x, c, w_ada, b_ada, g_pre_mix, w_in, w_dw, b_dw, g_conv_ln, b_conv_ln, w_conv_out, w_attn_out,
                 w_o, g_post_mix, g_pre_ffn, w_gate, w_up, w_down, g_post_ffn):
    x = np.asarray(x, np.float32)
    shared = {
        "w_ada": np.ascontiguousarray(np.asarray(w_ada, np.float32)[0]),
        "b_adaT": _fm(np.asarray(b_ada)[0], 96),
        "g_pre_mixT": _fm(np.asarray(g_pre_mix)[0], 16),
        "w_in": np.ascontiguousarray(np.asarray(w_in, np.float32)[0]),
        "w_dwT": np.ascontiguousarray(
            np.asarray(w_dw, np.float32)[0].T.reshape(8, 128, 31).transpose(1, 0, 2).reshape(128, 8 * 31)),
        "b_dwT": _fm(np.asarray(b_dw)[0], 8),
        "g_lnT": _fm(np.asarray(g_conv_ln)[0], 8),
        "b_lnT": _fm(np.asarray(b_conv_ln)[0], 8),
        "w_conv_out": np.ascontiguousarray(np.asarray(w_conv_out, np.float32)[0]),
        "w_attn_out": np.ascontiguousarray(np.asarray(w_attn_out, np.float32)[0]),
        "w_o": np.ascontiguousarray(np.asarray(w_o, np.float32)[0]),
        "g_post_mixT": _fm(np.asarray(g_post_mix)[0], 16),
        "g_pre_ffnT": _fm(np.asarray(g_pre_ffn)[0], 16),
        "w_gate": np.ascontiguousarray(np.asarray(w_gate, np.float32)[0]),
        "w_up": np.ascontiguousarray(np.asarray(w_up, np.float32)[0]),
        "w_down": np.ascontiguousarray(np.asarray(w_down, np.float32)[0]),
        "g_post_ffnT": _fm(np.asarray(g_post_ffn)[0], 16),
        "ident": np.eye(128, dtype=np.float32),
        "pow2": np.ascontiguousarray(np.tile((2.0 ** -(np.arange(NITER + 1) + 1.0)).astype(np.float32)[None, :], (128, 1))),
    }
    diagc = np.zeros((1, 256), np.float32)
    diagc[0, 0:64] = 1.0
    diagc[0, 128 + 64:256] = -BIG
    shared["diagc"] = diagc
    maps = []
    for core in range(8):
        b, j = core // 4, core % 4
        xc = np.zeros((NK, D), np.float32)
        nval = j * 1024
        xc[NCTX - nval:NCTX] = x[b, 0:nval]
        xc[NCTX:] = x[b, j * 1024:(j + 1) * 1024]
        kneg = np.zeros((1, 512), np.float32)
        for sgm in range(3):
            if sgm < 3 - j:
                kneg[0, sgm * 128:(sgm + 1) * 128] = -BIG
        pen = np.zeros((128, 6), np.float32)
        for sgm in range(3):
            if sgm < 3 - j:
                pen[:, sgm] = 2 * BIG
        pen[0:64, 5] = 2 * BIG
        m = dict(shared)
        m["xctx"] = xc
        m["cT"] = _fm(np.asarray(c, np.float32)[b], 16)
        m["kneg"] = kneg
        m["pen"] = pen
        m["halo_valid"] = np.full((128, 1), 1.0 if j > 0 else 0.0, np.float32)
        maps.append(m)
    return maps


_NC_CACHE = {}


def kernel(**inputs):
    if "nc" not in _NC_CACHE:
        _NC_CACHE["nc"] = build()[0]
    nc = _NC_CACHE["nc"]
    maps = make_in_maps(**inputs)
    res = run_bass_kernel_spmd(nc, maps, core_ids=list(range(8)))
    outp = np.zeros((2, 4096, D), np.float32)
    for core in range(8):
        b, j = core // 4, core % 4
        outp[b, j * 1024:(j + 1) * 1024] = res.results[core]["out"]
    return outp
```

```python
import numpy as np
from contextlib import ExitStack
import concourse.bass as bass
import concourse.mybir as mybir
from concourse.bass_utils import run_bass_kernel_spmd

F32 = mybir.dt.float32
BF16 = mybir.dt.bfloat16
AF = mybir.ActivationFunctionType
ALU = mybir.AluOpType
AX = mybir.AxisListType

D = 2048
KC = 16
NOWN = 1024
NCTX = 3072
NK = 4096
DFF = 5632
EPS = 1e-6
BIG = 4096.0
NITER = 17
SBQ = 2
O_GLU_A, O_GLU_G, O_Q, O_K, O_V, O_QI, O_KI, O_WI, O_GC, O_GA = (
    0, 1024, 2048, 3072, 4096, 5120, 6144, 6208, 6224, 8272)
IDX_SCALE = (64 ** -0.5) * (16 ** -0.5)
import os
BIS = os.environ.get('BIS', '')


class Buf:
    __slots__ = ("w", "r", "name")

    def __init__(self, name=""):
        self.w = {}
        self.r = {}
        self.name = name


class Eng:
    def __init__(self, name, eng, sem):
        self.name = name
        self.eng = eng
        self.sem = sem
        self.cnt = 0
        self.seen = {}


class Sched:
    def __init__(self, nc, stack, n_slots=40):
        self.nc = nc
        mk = lambda n: stack.enter_context(nc.semaphore(n))
        self.PE = Eng("pe", nc.tensor, mk("s_pe"))
        self.ACT = Eng("act", nc.scalar, mk("s_act"))
        self.DVE = Eng("dve", nc.vector, mk("s_dve"))
        self.POOL = Eng("pool", nc.gpsimd, mk("s_pool"))
        self.SP = Eng("sp", nc.sync, mk("s_sp"))
        self.engs = [self.PE, self.ACT, self.DVE, self.POOL, self.SP]
        self.slots = [[f"dma{i}", mk(f"s_dma{i}"), 0] for i in range(n_slots)]
        self.slot_i = 0
        self.prog = {}

    def _deps(self, reads, writes):
        d = {}

        def add(dic):
            for k, (sem, v) in dic.items():
                if k not in d or d[k][1] < v:
                    d[k] = (sem, v)
        for b in reads:
            add(b.w)
        for b in writes:
            add(b.w)
            add(b.r)
        return d

    def _wait(self, E, d):
        for k, (sem, v) in d.items():
            if E is self.PE and k == "pe":
                continue
            if E.seen.get(k, 0) >= v:
                continue
            E.eng.wait_ge(sem, v)
            E.seen[k] = v
            self.prog.setdefault(E.name, []).append(("w", k, v))

    def _mark(self, k, sem, v, reads, writes):
        for b in reads:
            if k not in b.r or b.r[k][1] < v:
                b.r[k] = (sem, v)
        for b in writes:
            b.w[k] = (sem, v)
            b.r = {}

    def op(self, E, fn, reads=(), writes=()):
        self._wait(E, self._deps(reads, writes))
        inst = fn(E.eng)
        E.cnt += 1
        inst.then_inc(E.sem, 1)
        self.prog.setdefault(E.name, []).append(("i", E.name, 1))
        self._mark(E.name, E.sem, E.cnt, reads, writes)

    def dma(self, Q, fn, reads=(), writes=()):
        d = self._deps(reads, writes)
        slot = self.slots[self.slot_i % len(self.slots)]
        self.slot_i += 1
        if slot[2] > 0:
            d[slot[0]] = (slot[1], slot[2])
        self._wait(Q, d)
        inst = fn(Q.eng)
        slot[2] += 16
        inst.then_inc(slot[1], 16)
        self.prog.setdefault(Q.name, []).append(("i", slot[0], 16))
        self._mark(slot[0], slot[1], slot[2], reads, writes)

    def barrier(self):
        evs = {}
        for E in self.engs:
            if E.cnt > 0:
                evs[E.name] = (E.sem, E.cnt)
        for s in self.slots:
            if s[2] > 0:
                evs[s[0]] = (s[1], s[2])
        for E in self.engs:
            self._wait(E, evs)


class Arena:
    def __init__(self, nc, stack, nbytes):
        self.t = stack.enter_context(nc.sbuf_tensor("arena", [128, nbytes // 4], F32))
        self.free = [(0, nbytes)]

    def alloc(self, nbytes):
        nbytes = (nbytes + 63) // 64 * 64
        for i, (o, n) in enumerate(self.free):
            if n >= nbytes:
                if n == nbytes:
                    self.free.pop(i)
                else:
                    self.free[i] = (o + nbytes, n - nbytes)
                return o, nbytes
        raise MemoryError(f"arena out of space for {nbytes}: free={self.free}")

    def release(self, o, n):
        fl = sorted(self.free + [(o, n)])
        out = []
        for a, b in fl:
            if out and out[-1][0] + out[-1][1] == a:
                out[-1] = (out[-1][0], out[-1][1] + b)
            else:
                out.append((a, b))
        self.free = out


class Scope:
    def __init__(self, arena):
        self.a = arena
        self.items = []

    def __enter__(self):
        return self

    def __exit__(self, *exc):
        self.close()
        return False

    def close(self):
        for o, n in self.items:
            self.a.release(o, n)
        self.items = []

    def alloc(self, shape, dt):
        esz = 4 if dt == F32 else 2
        n = 1
        for d_ in shape[1:]:
            n *= d_
        o, nb = self.a.alloc(n * esz)
        self.items.append((o, nb))
        v = self.a.t[0:shape[0], o // 4:(o + nb) // 4]
        if dt != F32:
            v = v.bitcast(dt)
        v = v[:, 0:n]
        if len(shape) == 3:
            v = v.rearrange("p (a b) -> p a b", a=shape[1])
        elif len(shape) == 4:
            v = v.rearrange("p (a b c) -> p a b c", a=shape[1], b=shape[2])
        return v


class _Stop(Exception):
    pass


def build(debug=(), limit=99):
    nc = bass.Bass("TRN2", target_bir_lowering=False)
    stack = ExitStack()

    def din(name, shape, dt=F32):
        return nc.dram_tensor(name, shape, dt, kind="ExternalInput").ap()

    xctx = din("xctx", [NK, D])
    cT_d = din("cT", [128, 16])
    w_ada = din("w_ada", [D, 6 * D])
    b_adaT = din("b_adaT", [128, 96])
    gpmT = din("g_pre_mixT", [128, 16])
    w_in = din("w_in", [D, 10320])
    w_dwT = din("w_dwT", [128, 8 * 31])
    b_dwT = din("b_dwT", [128, 8])
    g_lnT = din("g_lnT", [128, 8])
    b_lnT = din("b_lnT", [128, 8])
    w_co = din("w_conv_out", [1024, D])
    w_ao = din("w_attn_out", [1024, D])
    w_o = din("w_o", [D, D])
    gpostmT = din("g_post_mixT", [128, 16])
    gpfT = din("g_pre_ffnT", [128, 16])
    w_gate = din("w_gate", [D, DFF])
    w_up = din("w_up", [D, DFF])
    w_down = din("w_down", [DFF, D])
    gpostfT = din("g_post_ffnT", [128, 16])
    ident_d = din("ident", [128, 128])
    kneg_d = din("kneg", [1, 512])
    pen_d = din("pen", [128, 6])
    diag_d = din("diagc", [1, 256])
    pow2_d = din("pow2", [128, NITER + 1])
    halo_d = din("halo_valid", [128, 1])

    out = nc.dram_tensor("out", [NOWN, D], F32, kind="ExternalOutput").ap()
    kT_d = nc.dram_tensor("kT_scr", [8, 128, NK], BF16, kind="Internal").ap()
    v_d = nc.dram_tensor("v_scr", [NK, 8 * 160], BF16, kind="Internal").ap()
    gsc_d = nc.dram_tensor("g_scr", [2, D], F32, kind="Internal").ap()
    hown_d = nc.dram_tensor("hown_scr", [128, 16 * NOWN], BF16, kind="Internal").ap()

    dbg_outs = {}

    S = Sched(nc, stack)
    PE, ACT, DVE, POOL, SP = S.PE, S.ACT, S.DVE, S.POOL, S.SP
    try:

        arena = Arena(nc, stack, 212480)
        root = Scope(arena)

        def sb(st, name, shape, dt):
            if st is stack:
                st = root
            return st.alloc(shape, dt)

        def ps(st, name, shape, dt):
            return st.enter_context(nc.psum_tensor("ps_" + name, shape, dt))

        PB = [ps(stack, f"pb{i}", [128, 512], F32) for i in range(6)]
        PBb = [Buf(f"pb{i}") for i in range(6)]
        PT = [ps(stack, f"pt{i}", [128, 1024], BF16) for i in range(2)]
        PTb = [Buf(f"pt{i}") for i in range(2)]

        identf = sb(stack, "identf", [128, 128], F32)
        identb = sb(stack, "identb", [128, 128], BF16)
        onesf = sb(stack, "onesf", [128, 128], F32)
        onesb = sb(stack, "onesb", [128, 128], BF16)
        epsc = sb(stack, "epsc", [128, 1], F32)
        modT = sb(stack, "modT", [128, 96], F32)
        s1 = sb(stack, "s1", [128, 16], F32)
        s2 = sb(stack, "s2", [128, 16], F32)
        smallv = sb(stack, "smallv", [128, 16 * 8], F32)
        kneg_b = sb(stack, "kneg_b", [1, 512], BF16)
        ones512 = sb(stack, "ones512", [1, 512], BF16)
        diag_b = sb(stack, "diag_b", [1, 256], BF16)
        pen = sb(stack, "pen", [128, 6], F32)
        pow2 = sb(stack, "pow2", [128, NITER + 1], F32)
        halov = sb(stack, "halov", [128, 1], F32)
        wdw = sb(stack, "wdw", [128, 8 * 31], F32)
        convv = sb(stack, "convv", [128, 24], F32)
        sc_kiT, sc_hown, sc_halo = Scope(arena), Scope(arena), Scope(arena)
        kiT = sb(sc_kiT, "kiT", [128, NK], BF16)
        hT_own = sb(sc_hown, "hT_own", [128, 16, NOWN], BF16)
        hT_halo = sb(sc_halo, "hT_halo", [128, 16, 32], BF16)
        B_const = Buf("const")
        B_mod = Buf("mod")
        B_kiT = Buf("kiT")
        B_hown = [Buf("hown0"), Buf("hown1")]
        B_halo = Buf("halo")

        def tap(name, src_ap, shape, dt, reads):
            if name not in debug:
                return
            t = nc.dram_tensor("dbg_" + name, shape, dt, kind="ExternalOutput").ap()
            dbg_outs[name] = t
            S.dma(SP, lambda e: e.dma_start(out=t, in_=src_ap), reads=reads, writes=[Buf()])

        ld = lambda dst, src: S.dma(SP, lambda e: e.dma_start(out=dst, in_=src), writes=[B_const])
        ld(identf[:], ident_d)
        ld(pen[:], pen_d)
        ld(pow2[:], pow2_d)
        ld(halov[:], halo_d)
        ld(wdw[:], w_dwT)
        ld(convv[:, 0:8], b_dwT)
        ld(convv[:, 8:16], g_lnT)
        ld(convv[:, 16:24], b_lnT)
        ld(smallv[:, 0:16], gpmT)
        ld(smallv[:, 16:32], gpostmT)
        ld(smallv[:, 32:48], gpfT)
        ld(smallv[:, 48:64], gpostfT)
        S.dma(POOL, lambda e: e.dma_start(out=kneg_b[:], in_=kneg_d), writes=[B_const])
        S.op(DVE, lambda e: e.memset(ones512[:], 1.0), writes=[B_const])
        S.dma(POOL, lambda e: e.dma_start(out=diag_b[:], in_=diag_d), writes=[B_const])
        S.op(DVE, lambda e: e.tensor_copy(out=identb[:], in_=identf[:]), reads=[B_const], writes=[B_const])
        S.op(DVE, lambda e: e.memset(onesf[:], 1.0), writes=[B_const])
        S.op(DVE, lambda e: e.memset(onesb[:], 1.0), writes=[B_const])
        S.op(DVE, lambda e: e.memset(epsc[:], EPS), writes=[B_const])

        def rstd_from_ssq(dst, ssq, tmp, rb, wb):
            S.op(ACT, lambda e: e.activation(out=tmp, in_=ssq, func=AF.Sqrt, bias=epsc[:, 0:1], scale=1.0 / D),
                 reads=rb + [B_const], writes=wb)
            S.op(DVE, lambda e: e.reciprocal(out=dst, in_=tmp), reads=wb, writes=wb)

        wv_in = w_in.rearrange("(c p) n -> p c n", p=128)

        cb = sb(stack, "cb", [128, 16], BF16)
        badd = sb(stack, "badd", [128, 96], F32)
        Bc = Buf()
        with Scope(arena) as st:
            cT = sb(st, "cT", [128, 16], F32)
            wa = [sb(st, f"wa{i}", [128, 16, 512], BF16) for i in range(2)]
            Bwa = [Buf(), Buf()]
            S.dma(SP, lambda e: e.dma_start(out=cT[:], in_=cT_d), writes=[Bc])
            S.dma(SP, lambda e: e.dma_start(out=badd[:], in_=b_adaT), writes=[Bc])
            S.op(ACT, lambda e: e.activation(out=cb[:], in_=cT[:], func=AF.Silu), reads=[Bc], writes=[Bc])
            wv_ada = w_ada.rearrange("(c p) n -> p c n", p=128)
            for blk in range(8):
                w_t = wa[blk % 2]
                for q4 in range(4):
                    S.dma(POOL, lambda e, w_t=w_t, q4=q4, blk=blk: e.dma_start(
                        out=w_t[:, q4 * 4:(q4 + 1) * 4, :], in_=wv_ada[:, q4 * 4:(q4 + 1) * 4, blk * 512:(blk + 1) * 512]),
                        writes=[Bwa[blk % 2]])
                for cc in range(4):
                    m = blk * 4 + cc
                    for kc in range(KC):
                        S.op(PE, lambda e, w_t=w_t, cc=cc, kc=kc, m=m: e.matmul(
                            PB[0][:, m:m + 1], lhsT=w_t[:, kc, cc * 128:(cc + 1) * 128], rhs=cb[:, kc:kc + 1],
                            start=(kc == 0), stop=(kc == KC - 1)),
                            reads=[Bwa[blk % 2], Bc], writes=[PBb[0]])
            S.op(DVE, lambda e: e.tensor_tensor(out=modT[:, 0:32], in0=PB[0][:, 0:32], in1=badd[:, 0:32], op=ALU.add),
                 reads=[PBb[0], Bc], writes=[B_mod])
            S.op(DVE, lambda e: e.scalar_tensor_tensor(out=s1[:], in0=modT[:, 16:32], scalar=1.0, in1=smallv[:, 0:16],
                                                       op0=ALU.add, op1=ALU.mult), reads=[B_mod, B_const], writes=[B_mod])
            S.barrier()
            if limit == 1:
                raise _Stop()

        def norm_transpose(st_bufs, src_tiles, nt, sc_t, bias_ap_fn, dst_fn, dst_bufs, src_bufs):
            xn, Bxn, ssq, rs, tmp, Bst = st_bufs
            for t in range(nt):
                S.op(ACT, lambda e, t=t: e.activation(out=xn[:, t, :], in_=src_tiles[t], func=AF.Square,
                                                      accum_out=ssq[:, t:t + 1]),
                     reads=[src_bufs[t]], writes=[Bxn[t], Bst[t]])
                rstd_from_ssq(rs[:, t:t + 1], ssq[:, t:t + 1], tmp[:, t:t + 1], [Bst[t]], [Bst[t]])
                S.op(ACT, lambda e, t=t: e.activation(out=xn[:, t, :], in_=src_tiles[t], func=AF.Copy,
                                                      scale=rs[:, t:t + 1]),
                     reads=[src_bufs[t], Bst[t]], writes=[Bxn[t]])
            for c in range(KC):
                pt = PT[c % 2]
                for t in range(nt):
                    S.op(PE, lambda e, t=t, c=c, pt=pt: e.transpose(out=pt[:, t * 128:(t + 1) * 128],
                                                                   in_=xn[:, t, c * 128:(c + 1) * 128], identity=identb[:]),
                         reads=[Bxn[t], B_const], writes=[PTb[c % 2]])
                if c % 2 == 0:
                    S.op(ACT, lambda e, c=c, pt=pt: e.activation(out=dst_fn(c), in_=pt[:, 0:nt * 128], func=AF.Identity,
                                                                 scale=sc_t[:, c:c + 1], bias=bias_ap_fn(c)),
                         reads=[PTb[c % 2], B_mod], writes=dst_bufs)
                else:
                    S.op(DVE, lambda e, c=c, pt=pt: e.tensor_scalar(out=dst_fn(c), in0=pt[:, 0:nt * 128],
                                                                    scalar1=sc_t[:, c:c + 1], scalar2=bias_ap_fn(c),
                                                                    op0=ALU.mult, op1=ALU.add),
                         reads=[PTb[c % 2], B_mod], writes=dst_bufs)

        with Scope(arena) as st:
            Wkv = sb(st, "Wkv", [128, 16, 2048], BF16)
            Wki = sb(st, "Wki", [128, 16, 128], BF16)
            Bw = Buf()
            for kc4 in range(8):
                S.dma(POOL, lambda e, kc4=kc4: e.dma_start(out=Wkv[:, kc4 * 2:kc4 * 2 + 2, :],
                                                           in_=wv_in[:, kc4 * 2:kc4 * 2 + 2, O_K:O_K + 2048]), writes=[Bw])
            for hlf in range(2):
                S.dma(POOL, lambda e, hlf=hlf: e.dma_start(out=Wki[:, :, hlf * 64:(hlf + 1) * 64],
                                                           in_=wv_in[:, :, O_KI:O_KI + 64]), writes=[Bw])
            xt = [sb(st, f"xt{i}", [128, D], F32) for i in range(2)]
            Bxt = [Buf(), Buf()]
            xn = sb(st, "xn", [128, 4, D], BF16)
            Bxn = [Buf() for _ in range(4)]
            ssq = sb(st, "ssq", [128, 4], F32)
            rs = sb(st, "rs", [128, 4], F32)
            tmp = sb(st, "tmpv", [128, 4], F32)
            Bst = [Buf() for _ in range(4)]
            hT_tmp = sb(st, "hT_tmp", [128, 16, 512], BF16)
            B_htmp = Buf()
            kT_g = sb(st, "kT_g", [128, 8, 512], BF16)
            B_kTg = Buf()
            v_g = sb(st, "v_g", [128, 4, 8, 160], BF16)
            B_vg = Buf()
            S.op(POOL, lambda e: e.memset(v_g[:], 0.0), writes=[B_vg])
            for t_ in range(4):
                S.op(POOL, lambda e, t_=t_: e.memset(v_g[:, t_, :, 64:65], 1.0), writes=[B_vg])
            B_kTd = Buf("kTd")
            B_vd = Buf("vd")
            def norm_group(g):
                for t in range(4):
                    xb = xt[t % 2]
                    r0 = g * 512 + t * 128
                    S.dma(SP, lambda e, xb=xb, r0=r0: e.dma_start(out=xb[:], in_=xctx[r0:r0 + 128, :]),
                          writes=[Bxt[t % 2]])
                    S.op(ACT, lambda e, t=t, xb=xb: e.activation(out=xn[:, t, :], in_=xb[:], func=AF.Square,
                                                                 accum_out=ssq[:, t:t + 1]),
                         reads=[Bxt[t % 2]], writes=[Bxn[t], Bst[t]])
                    rstd_from_ssq(rs[:, t:t + 1], ssq[:, t:t + 1], tmp[:, t:t + 1], [Bst[t]], [Bst[t]])
                    S.op(ACT, lambda e, t=t, xb=xb: e.activation(out=xn[:, t, :], in_=xb[:], func=AF.Copy,
                                                                 scale=rs[:, t:t + 1]),
                         reads=[Bxt[t % 2], Bst[t]], writes=[Bxn[t]])
            norm_group(0)
            for g in range(8):
                own = g >= 6
                if own:
                    hsl = lambda c, g=g: hT_own[:, c, (g - 6) * 512:(g - 5) * 512]
                    hb = B_hown[g - 6]
                else:
                    hsl = lambda c: hT_tmp[:, c, :]
                    hb = B_htmp
                for c in range(KC):
                    pt = PT[c % 2]
                    for t in range(4):
                        S.op(PE, lambda e, t=t, c=c, pt=pt: e.transpose(out=pt[:, t * 128:(t + 1) * 128],
                                                                       in_=xn[:, t, c * 128:(c + 1) * 128],
                                                                       identity=identb[:]),
                             reads=[Bxn[t], B_const], writes=[PTb[c % 2]])
                    if c % 2 == 0:
                        S.op(ACT, lambda e, c=c, pt=pt, hsl=hsl: e.activation(out=hsl(c), in_=pt[:, 0:512], func=AF.Identity,
                                                                             scale=s1[:, c:c + 1], bias=modT[:, c:c + 1]),
                             reads=[PTb[c % 2], B_mod], writes=[hb])
                    else:
                        S.op(DVE, lambda e, c=c, pt=pt, hsl=hsl: e.tensor_scalar(out=hsl(c), in0=pt[:, 0:512],
                                                                                scalar1=s1[:, c:c + 1], scalar2=modT[:, c:c + 1],
                                                                                op0=ALU.mult, op1=ALU.add),
                             reads=[PTb[c % 2], B_mod], writes=[hb])
                if g + 1 < 8:
                    norm_group(g + 1)
                if g == 5:
                    S.op(DVE, lambda e: e.tensor_copy(out=hT_halo[:], in_=hT_tmp[:, :, 480:512]),
                         reads=[B_htmp], writes=[B_halo])
                for cc in range(8):
                    pb = cc % 2
                    for kc in range(KC):
                        S.op(PE, lambda e, cc=cc, kc=kc, pb=pb, hsl=hsl: e.matmul(
                            PB[pb][:, :], lhsT=Wkv[:, kc, cc * 128:(cc + 1) * 128], rhs=hsl(kc),
                            start=(kc == 0), stop=(kc == KC - 1)), reads=[Bw, hb], writes=[PBb[pb]])
                    if cc % 2 == 0:
                        S.op(ACT, lambda e, cc=cc, pb=pb: e.activation(out=kT_g[:, cc, :], in_=PB[pb][:, :], func=AF.Copy),
                             reads=[PBb[pb]], writes=[B_kTg])
                    else:
                        S.op(DVE, lambda e, cc=cc, pb=pb: e.tensor_copy(out=kT_g[:, cc, :], in_=PB[pb][:, :]),
                             reads=[PBb[pb]], writes=[B_kTg])
                S.dma(SP, lambda e, g=g: e.dma_start(out=kT_d[:, :, g * 512:(g + 1) * 512].rearrange("c p n -> p c n"),
                                                     in_=kT_g[:]), reads=[B_kTg], writes=[B_kTd])
                for kc in range(KC):
                    S.op(PE, lambda e, kc=kc, hsl=hsl: e.matmul(PB[2][:, :], lhsT=Wki[:, kc, :], rhs=hsl(kc),
                                                                start=(kc == 0), stop=(kc == KC - 1)),
                         reads=[Bw, hb], writes=[PBb[2]])
                S.op(ACT, lambda e, g=g: e.activation(out=kiT[:, g * 512:(g + 1) * 512], in_=PB[2][:, :], func=AF.Copy),
                     reads=[PBb[2]], writes=[B_kiT])
                for t in range(4):
                    for nb in range(2):
                        pb = 3 + (t * 2 + nb) % 2
                        for kc in range(KC):
                            S.op(PE, lambda e, t=t, nb=nb, kc=kc, pb=pb, hsl=hsl: e.matmul(
                                PB[pb][:, :], lhsT=hsl(kc)[:, t * 128:(t + 1) * 128],
                                rhs=Wkv[:, kc, 1024 + nb * 512:1024 + (nb + 1) * 512],
                                start=(kc == 0), stop=(kc == KC - 1)), reads=[Bw, hb], writes=[PBb[pb]])
                        srcv = PB[pb][:, :].rearrange("p (r e d) -> p r e d", e=2, d=64)
                        dst_e = v_g[:, t, nb * 4:(nb + 1) * 4, 0:64]
                        dst_o = v_g[:, t, nb * 4:(nb + 1) * 4, 96:160]
                        if pb == 3:
                            S.op(ACT, lambda e, dst_e=dst_e, srcv=srcv: e.activation(out=dst_e, in_=srcv[:, :, 0, :], func=AF.Copy),
                                 reads=[PBb[pb]], writes=[B_vg])
                            S.op(ACT, lambda e, dst_o=dst_o, srcv=srcv: e.activation(out=dst_o, in_=srcv[:, :, 1, :], func=AF.Copy),
                                 reads=[PBb[pb]], writes=[B_vg])
                        else:
                            S.op(DVE, lambda e, dst_e=dst_e, srcv=srcv: e.tensor_copy(out=dst_e, in_=srcv[:, :, 0, :]),
                                 reads=[PBb[pb]], writes=[B_vg])
                            S.op(DVE, lambda e, dst_o=dst_o, srcv=srcv: e.tensor_copy(out=dst_o, in_=srcv[:, :, 1, :]),
                                 reads=[PBb[pb]], writes=[B_vg])
                S.dma(SP, lambda e, g=g: e.dma_start(
                    out=v_d[g * 512:(g + 1) * 512, :].rearrange("(t p) n -> p t n", p=128),
                    in_=v_g[:].rearrange("p t h d -> p t (h d)")), reads=[B_vg], writes=[B_vd])
            tap("hT_own", hT_own[:], [128, 16, NOWN], BF16, B_hown)
            tap("kiT", kiT[:], [128, NK], BF16, [B_kiT])
            S.barrier()
            if limit == 2:
                raise _Stop()

        def proj_fm(st, wsrc3, col0, nchunks, segs, consume, Bw2, wblk, seg_reads):
            nblk = (nchunks + 3) // 4
            for blk in range(nblk):
                w_t = wblk[blk % 2]
                ncol = min(4, nchunks - blk * 4) * 128
                for q4 in range(2):
                    S.dma(POOL, lambda e, w_t=w_t, q4=q4, blk=blk, ncol=ncol: e.dma_start(
                        out=w_t[:, q4 * 8:(q4 + 1) * 8, 0:ncol],
                        in_=wsrc3[:, q4 * 8:(q4 + 1) * 8, col0 + blk * 512:col0 + blk * 512 + ncol]),
                        writes=[Bw2[blk % 2]])
                for cc in range(ncol // 128):
                    ch = blk * 4 + cc
                    for si, (rhs_fn, wd) in enumerate(segs):
                        pbi = (ch * len(segs) + si) % 2
                        for kc in range(KC):
                            S.op(PE, lambda e, w_t=w_t, cc=cc, kc=kc, pbi=pbi, rhs_fn=rhs_fn, wd=wd: e.matmul(
                                PB[pbi][:, 0:wd], lhsT=w_t[:, kc, cc * 128:(cc + 1) * 128], rhs=rhs_fn(kc),
                                start=(kc == 0), stop=(kc == KC - 1)),
                                reads=[Bw2[blk % 2]] + seg_reads, writes=[PBb[pbi]])
                        consume(ch, si, PB[pbi], PBb[pbi])

        own_segs = [(lambda kc: hT_own[:, kc, 0:512], 512), (lambda kc: hT_own[:, kc, 512:1024], 512)]

        att_stack = Scope(arena)
        attT = sb(att_stack, "attT", [128, 8, NOWN], BF16)
        B_att = Buf("attT")
        with Scope(arena) as st:
            qT = sb(st, "qT", [128, 8, NOWN], BF16)
            qiT = sb(st, "qiT", [128, 8, NOWN], BF16)
            B_q = Buf()
            B_qi = Buf()
            wsc = sb(st, "wsc", [128, 8, 16], F32)
            wabs = sb(st, "wabs", [128, 8, 16], F32)
            wsgn = sb(st, "wsgn", [128, 8, 16], F32)
            B_wi = Buf()
            st2 = Scope(arena)
            wblk = [sb(st2, f"wblk{i}", [128, 16, 512], BF16) for i in range(2)]
            Bw2 = [Buf(), Buf()]
            Wwi = sb(st2, "Wwi", [128, 16, 16], BF16)
            B_wwi = Buf()

            def cons_q(dst, Bd):
                def f(ch, si, pb, pbb):
                    eng = ACT if (ch + si) % 2 == 0 else DVE
                    if eng is ACT:
                        S.op(ACT, lambda e: e.activation(out=dst[:, ch, si * 512:(si + 1) * 512], in_=pb[:, :], func=AF.Copy),
                             reads=[pbb], writes=[Bd])
                    else:
                        S.op(DVE, lambda e: e.tensor_copy(out=dst[:, ch, si * 512:(si + 1) * 512], in_=pb[:, :]),
                             reads=[pbb], writes=[Bd])
                return f
            proj_fm(st, wv_in, O_Q, 8, own_segs, cons_q(qT, B_q), Bw2, wblk, B_hown)
            proj_fm(st, wv_in, O_QI, 8, own_segs, cons_q(qiT, B_qi), Bw2, wblk, B_hown)
            S.dma(POOL, lambda e: e.dma_start(out=Wwi[:], in_=wv_in[:, :, O_WI:O_WI + 16]), writes=[B_wwi])
            for t in range(8):
                for kc in range(KC):
                    S.op(PE, lambda e, t=t, kc=kc: e.matmul(PB[2][:, 0:16], lhsT=hT_own[:, kc, t * 128:(t + 1) * 128],
                                                            rhs=Wwi[:, kc, :], start=(kc == 0), stop=(kc == KC - 1)),
                         reads=[B_wwi] + B_hown, writes=[PBb[2]])
                S.op(ACT, lambda e, t=t: e.activation(out=wsc[:, t, :], in_=PB[2][:, 0:16], func=AF.Copy, scale=IDX_SCALE),
                     reads=[PBb[2]], writes=[B_wi])
            S.op(ACT, lambda e: e.activation(out=wabs[:], in_=wsc[:], func=AF.Abs), reads=[B_wi], writes=[B_wi])
            S.op(ACT, lambda e: e.activation(out=wsgn[:], in_=wsc[:], func=AF.Sign), reads=[B_wi], writes=[B_wi])
            tap("qT", qT[:], [128, 8, NOWN], BF16, [B_q])
            tap("wsc", wsc[:], [128, 8, 16], F32, [B_wi])
            B_hownd = Buf("hownd")
            S.dma(SP, lambda e: e.dma_start(out=hown_d, in_=hT_own[:].rearrange("p c n -> p (c n)")), reads=B_hown, writes=[B_hownd])
            S.barrier()
            if limit == 3:
                raise _Stop()
            st2.close()
            sc_hown.close()
            PTf = PT[1][:, :].bitcast(F32)
            NWAS = 4
            was = [sb(st, f"was{i}", [128, 16, 128], BF16) for i in range(NWAS)]
            Bwas = [Buf() for _ in range(NWAS)]
            mod_state = {"dma": 32, "mm": 32, "tick": 0}

            def mod_dma():
                m = mod_state["dma"]
                if m >= 96:
                    return
                mod_state["dma"] += 1
                w_t, Bwt = was[m % NWAS], Bwas[m % NWAS]
                S.dma(POOL, lambda e: e.dma_start(out=w_t[:], in_=wv_ada[:, :, m * 128:(m + 1) * 128]), writes=[Bwt])

            def mod_mm():
                m = mod_state["mm"]
                if m >= 96:
                    return
                mod_state["mm"] += 1
                w_t, Bwt = was[m % NWAS], Bwas[m % NWAS]
                for kc in range(KC):
                    S.op(PE, lambda e, kc=kc: e.matmul(PTf[:, m:m + 1], lhsT=w_t[:, kc, :], rhs=cb[:, kc:kc + 1],
                                                       start=(kc == 0), stop=(kc == KC - 1)),
                         reads=[Bwt, Bc], writes=[PTb[1]])
                mod_dma()

            def mod_tick(every=24):
                mod_state["tick"] += 1
                if mod_state["tick"] % every == 0:
                    mod_mm()
            for _ in range(NWAS - 1):
                mod_dma()

            scb2 = [sb(st, f"scb{i}", [128, NK], F32) for i in range(2)]
            B_sc2 = [Buf(), Buf()]
            mk = sb(st, "mk", [128, NK], BF16)
            B_mk = Buf()
            NQ = SBQ * 128
            maskT = sb(st, "maskT", [128, 32, NQ], BF16)
            B_maskT = Buf()
            dg2 = [sb(st, f"dg{i}", [128, 16, 128], BF16) for i in range(2)]
            B_dg2 = [Buf(), Buf()]
            NRR = 6
            rr = [sb(st, f"rr{i}", [128, 512], BF16) for i in range(NRR)]
            B_rr = [Buf() for _ in range(NRR)]
            kTc = sb(st, "kTc", [128, NK], BF16)
            B_kTc = [Buf(), Buf()]
            vc = sb(st, "vc", [128, 32, 160], BF16)
            B_vc = [Buf(), Buf()]
            NET = 4
            et = [sb(st, f"et{i}", [128, 2, NQ], BF16) for i in range(NET)]
            B_et = [Buf() for _ in range(NET)]
            ptl = [sb(st, f"ptl{i}", [128, 2, NQ], BF16) for i in range(NET)]
            B_ptl = [Buf() for _ in range(NET)]
            qpad = sb(st, "qpad", [128, 8, 2, NQ], BF16)
            B_qpad = Buf()
            S.op(POOL, lambda e: e.memset(qpad[:], 0.0), writes=[B_qpad])
            sv = sb(st, "sv", [128, 64], F32)
            B_sv = Buf()
            wtab = sb(st, "wtab", [128, NITER + 1], F32)
            rd = sb(st, "rd", [128, NQ], F32)
            B_rd = Buf()
            rbc = sb(st, "rbc", [128, NQ], F32)
            B_rbc = Buf()
            thr_dbg = sb(st, "thr_dbg", [128, 8], F32)
            B_thr = Buf()
            qk_slot = [PB[2][:, 0:2 * NQ], PB[3][:, 0:2 * NQ], PB[4][:, 0:2 * NQ],
                       PT[0][:, :].bitcast(F32)[:, 0:2 * NQ]]
            B_qk = [PBb[2], PBb[3], PBb[4], PTb[0]]
            NQK = len(qk_slot)

            def run_pipeline(nsteps, stages, lags):
                for tick in range(nsteps + max(lags)):
                    for f, lg in zip(stages, lags):
                        i = tick - lg
                        if 0 <= i < nsteps:
                            f(i)

            def indexer(qb):
                scb, B_sc = scb2[qb % 2], B_sc2[qb % 2]
                dg, B_dg = dg2[qb % 2], B_dg2[qb % 2]
                nkt = 24 + qb + 1
                N = nkt * 128
                q0 = qb * 128
                for h in range(16):
                    S.op(DVE if BIS == "dgdve" else POOL, lambda e, h=h: e.tensor_scalar(out=dg[:, h, :], in0=identb[:],
                                                              scalar1=wsgn[:, qb, h:h + 1], scalar2=None, op0=ALU.mult),
                         reads=[B_const, B_wi], writes=[B_dg])
                ngrp = (N + 511) // 512
                steps = [(n, h) for n in range(ngrp) for h in range(16)]

                def stA(i):
                    n, h = steps[i]
                    wdt = min(512, N - n * 512)
                    hb = (h % 2) * 64
                    pbi = i % 4
                    S.op(PE, lambda e: e.matmul(PB[pbi][:, 0:wdt], lhsT=qiT[hb:hb + 64, h // 2, q0:q0 + 128],
                                                rhs=kiT[hb:hb + 64, n * 512:n * 512 + wdt], start=True, stop=True),
                         reads=[B_qi, B_kiT], writes=[PBb[pbi]])

                def stB(i):
                    n, h = steps[i]
                    wdt = min(512, N - n * 512)
                    pbi = i % 4
                    r_t, B_r = rr[i % NRR], B_rr[i % NRR]
                    S.op(ACT, lambda e: e.activation(out=r_t[:, 0:wdt], in_=PB[pbi][:, 0:wdt], func=AF.Relu,
                                                     scale=wabs[:, qb, h:h + 1]),
                         reads=[PBb[pbi], B_wi], writes=[B_r])

                def stC(i):
                    mod_tick()
                    n, h = steps[i]
                    wdt = min(512, N - n * 512)
                    r_t, B_r = rr[i % NRR], B_rr[i % NRR]
                    psc, pscb = PB[4 + n % 2], PBb[4 + n % 2]
                    S.op(PE, lambda e: e.matmul(psc[:, 0:wdt], lhsT=dg[:, h, :], rhs=r_t[:, 0:wdt], start=(h == 0), stop=False),
                         reads=[B_dg, B_r], writes=[pscb])
                    if h == 15:
                        last = (n == ngrp - 1)
                        sgm = min(n // 2, 3)
                        S.op(PE, lambda e: e.matmul(psc[:, 0:wdt], lhsT=kneg_b[0:1, sgm * 128:(sgm + 1) * 128],
                                                    rhs=ones512[0:1, 0:wdt], start=False, stop=(not last)),
                             reads=[B_const], writes=[pscb])
                        if last:
                            S.op(PE, lambda e: e.matmul(psc[:, wdt - 128:wdt], lhsT=diag_b[0:1, 0:128], rhs=diag_b[0:1, 128:256],
                                                        start=False, stop=True), reads=[B_const], writes=[pscb])
                        S.op(ACT, lambda e: e.activation(out=scb[:, n * 512:n * 512 + wdt], in_=psc[:, 0:wdt], func=AF.Copy),
                             reads=[pscb], writes=[B_sc])
                run_pipeline(len(steps), [stA, stB, stC], [0, 0, 3])

            def search(qb):
                scb, B_sc = scb2[qb % 2], B_sc2[qb % 2]
                nkt = 24 + qb + 1
                N = nkt * 128
                S.op(DVE, lambda e: e.tensor_reduce(out=sv[:, 0:1], in_=scb[:, 0:N], axis=AX.X, op=ALU.max),
                     reads=[B_sc], writes=[B_sv])
                S.op(DVE, lambda e: e.tensor_reduce(out=sv[:, 8:11], in_=scb[:, 0:NCTX].rearrange("p (s n) -> p s n", s=3),
                                                    axis=AX.X, op=ALU.min), reads=[B_sc], writes=[B_sv])
                if qb > 0:
                    S.op(DVE, lambda e: e.tensor_reduce(out=sv[:, 11:12], in_=scb[:, NCTX:NCTX + qb * 128],
                                                        axis=AX.X, op=ALU.min), reads=[B_sc], writes=[B_sv])
                else:
                    S.op(DVE, lambda e: e.memset(sv[:, 11:12], 3.0e38), writes=[B_sv])
                S.op(DVE, lambda e: e.tensor_reduce(out=sv[:, 12:14],
                                                    in_=scb[:, N - 128:N].rearrange("p (s n) -> p s n", s=2),
                                                    axis=AX.X, op=ALU.min), reads=[B_sc], writes=[B_sv])
                S.op(DVE, lambda e: e.tensor_tensor(out=sv[:, 16:22], in0=sv[:, 8:14], in1=pen[:], op=ALU.add),
                     reads=[B_sv, B_const], writes=[B_sv])
                S.op(DVE, lambda e: e.tensor_reduce(out=sv[:, 1:2], in_=sv[:, 16:22], axis=AX.X, op=ALU.min),
                     reads=[B_sv], writes=[B_sv])
                S.op(DVE, lambda e: e.tensor_tensor(out=sv[:, 2:3], in0=sv[:, 0:1], in1=sv[:, 1:2], op=ALU.subtract),
                     reads=[B_sv], writes=[B_sv])
                S.op(DVE, lambda e: e.tensor_scalar(out=sv[:, 2:3], in0=sv[:, 2:3], scalar1=1.002, scalar2=1e-6,
                                                    op0=ALU.mult, op1=ALU.add), reads=[B_sv], writes=[B_sv])
                S.op(DVE, lambda e: e.tensor_scalar(out=wtab[:], in0=pow2[:], scalar1=sv[:, 2:3], scalar2=None,
                                                    op0=ALU.mult), reads=[B_sv, B_const], writes=[B_sv])
                S.op(DVE, lambda e: e.tensor_tensor(out=sv[:, 1:2], in0=sv[:, 0:1], in1=sv[:, 2:3], op=ALU.subtract),
                     reads=[B_sv], writes=[B_sv])
                S.op(DVE, lambda e: e.tensor_tensor(out=sv[:, 3:4], in0=sv[:, 1:2], in1=wtab[:, 0:1], op=ALU.add),
                     reads=[B_sv], writes=[B_sv])
                for k in range(NITER):
                    S.op(DVE, lambda e: e.tensor_scalar(out=mk[:, 0:N], in0=scb[:, 0:N], scalar1=sv[:, 3:4],
                                                        scalar2=0.0, op0=ALU.is_ge, op1=ALU.add, accum_out=sv[:, 4:5]),
                         reads=[B_sc, B_sv], writes=[B_mk, B_sv])
                    S.op(DVE, lambda e: e.tensor_scalar(out=sv[:, 5:6], in0=sv[:, 4:5], scalar1=255.5, scalar2=0.5,
                                                        op0=ALU.is_ge, op1=ALU.subtract), reads=[B_sv], writes=[B_sv])
                    S.op(DVE, lambda e, k=k: e.scalar_tensor_tensor(out=sv[:, 3:4], in0=sv[:, 5:6], scalar=wtab[:, k:k + 1],
                                                                    in1=sv[:, 3:4], op0=ALU.mult, op1=ALU.add),
                         reads=[B_sv], writes=[B_sv])
                S.op(DVE, lambda e: e.tensor_tensor(out=sv[:, 6:7], in0=sv[:, 3:4], in1=wtab[:, NITER:NITER + 1],
                                                    op=ALU.subtract), reads=[B_sv], writes=[B_sv])
                S.op(DVE, lambda e: e.tensor_copy(out=thr_dbg[:, qb:qb + 1], in_=sv[:, 6:7]),
                     reads=[B_sv], writes=[B_thr])
                S.op(DVE, lambda e: e.tensor_scalar(out=mk[:, 0:N], in0=scb[:, 0:N], scalar1=sv[:, 6:7],
                                                    scalar2=None, op0=ALU.is_ge), reads=[B_sc, B_sv], writes=[B_mk])
                if qb == 7:
                    tap("scb", scb[:], [128, NK], F32, [B_sc])

            def mask_transposes(qb, i):
                nkt = 24 + qb + 1
                for kt0 in range(0, nkt, 8):
                    nk8 = min(8, nkt - kt0)
                    pt = PT[0]
                    ptb = PTb[0]
                    for a_ in range(nk8):
                        S.op(PE, lambda e, a_=a_: e.transpose(
                            out=pt[:, a_ * 128:(a_ + 1) * 128], in_=mk[:, (kt0 + a_) * 128:(kt0 + a_ + 1) * 128],
                            identity=identb[:]), reads=[B_mk, B_const], writes=[ptb])
                    S.op(ACT, lambda e: e.activation(
                        out=maskT[:, kt0:kt0 + nk8, i * 128:(i + 1) * 128],
                        in_=pt[:, 0:nk8 * 128].rearrange("p (a q) -> p a q", q=128), func=AF.Copy),
                        reads=[ptb], writes=[B_maskT])

            def attention(s):
                nkt_s = 24 + (s + 1) * SBQ
                qs0 = s * NQ
                S.op(POOL, lambda e: e.tensor_copy(out=qpad[0:64, :, 0, :], in_=qT[0:64, :, qs0:qs0 + NQ]),
                     reads=[B_q], writes=[B_qpad])
                S.op(POOL, lambda e: e.tensor_copy(out=qpad[64:128, :, 1, :], in_=qT[64:128, :, qs0:qs0 + NQ]),
                     reads=[B_q], writes=[B_qpad])
                steps = [(hc, kt) for hc in range(8) for kt in range(nkt_s)]
                kv_view = v_d.rearrange("(kt p) n -> p kt n", p=128)

                def load_kv(hc, half):
                    k0, k1 = (0, 16) if half == 0 else (16, 32)
                    S.dma(SP, lambda e: e.dma_start(out=kTc[:, k0 * 128:k1 * 128], in_=kT_d[hc][:, k0 * 128:k1 * 128]),
                          reads=[B_kTd], writes=[B_kTc[half]])
                    S.dma(SP, lambda e: e.dma_start(out=vc[:, k0:k1, :], in_=kv_view[:, k0:k1, hc * 160:(hc + 1) * 160]),
                          reads=[B_vd], writes=[B_vc[half]])

                def stA(i):
                    hc, kt = steps[i]
                    sl = i % NQK
                    S.op(PE, lambda e: e.matmul(qk_slot[sl], lhsT=kTc[:, kt * 128:(kt + 1) * 128],
                                                rhs=qpad[:, hc, :, :], start=True, stop=True),
                         reads=[B_kTc[kt // 16], B_qpad], writes=[B_qk[sl]])

                def stB(i):
                    sl = i % NQK
                    S.op(ACT, lambda e: e.activation(out=et[i % NET][:, :, :], in_=qk_slot[sl].rearrange("p (a q) -> p a q", a=2),
                                                     func=AF.Exp, scale=0.125),
                         reads=[B_qk[sl]], writes=[B_et[i % NET]])

                def stC(i):
                    hc, kt = steps[i]
                    S.op(DVE, lambda e: e.tensor_tensor(out=ptl[i % NET][:, :, :], in0=et[i % NET][:, :, :],
                                                        in1=maskT[:, kt:kt + 1, :].broadcast_to((128, 2, NQ)), op=ALU.mult),
                         reads=[B_et[i % NET], B_maskT], writes=[B_ptl[i % NET]])

                def stD(i):
                    mod_tick()
                    hc, kt = steps[i]
                    for hh in range(2):
                        S.op(PE, lambda e, hh=hh: e.matmul(PB[hh][0:(65 if hh == 0 else 128), 0:NQ],
                                                           lhsT=(vc[:, kt, 0:65] if hh == 0 else vc[:, kt, 32:160]),
                                                           rhs=ptl[i % NET][:, hh, :], start=(kt == 0), stop=(kt == nkt_s - 1)),
                             reads=[B_vc[kt // 16], B_ptl[i % NET]], writes=[PBb[hh]])
                    if kt == 15 and hc + 1 < 8:
                        load_kv(hc + 1, 0)
                    if kt == nkt_s - 1:
                        if hc + 1 < 8:
                            load_kv(hc + 1, 1)
                        normalize(hc, 0)
                        normalize(hc, 1)

                def normalize(hc, hh):
                    hb = hh * 64
                    po, pob = PB[hh], PBb[hh]
                    dp = 64 if hh == 0 else 32
                    S.op(DVE, lambda e: e.reciprocal(out=rd[dp:dp + 1, :], in_=po[dp:dp + 1, 0:NQ]),
                         reads=[pob], writes=[B_rd])
                    S.op(PE, lambda e: e.matmul(PB[5][:, 0:NQ], lhsT=onesf[dp:dp + 1, 0:128], rhs=rd[dp:dp + 1, :],
                                                start=True, stop=True), reads=[B_rd, B_const], writes=[PBb[5]])
                    S.op(ACT, lambda e: e.activation(out=rbc[:, :], in_=PB[5][:, 0:NQ], func=AF.Copy),
                         reads=[PBb[5]], writes=[B_rbc])
                    S.op(DVE, lambda e: e.tensor_tensor(out=attT[hb:hb + 64, hc, qs0:qs0 + NQ], in0=po[hb:hb + 64, 0:NQ],
                                                        in1=rbc[hb:hb + 64, :], op=ALU.mult),
                         reads=[pob, B_rbc], writes=[B_att])
                load_kv(0, 0)
                load_kv(0, 1)
                run_pipeline(len(steps), [stA, stB, stC, stD], [0, 0, 0, 3])

            nsb = 8 // SBQ
            indexer(0)
            for s in range(nsb):
                S.op(POOL, lambda e: e.memset(maskT[:], 0.0), writes=[B_maskT])
                for i in range(SBQ):
                    qb = s * SBQ + i
                    search(qb)
                    if qb + 1 < 8:
                        indexer(qb + 1)
                    mask_transposes(qb, i)
                attention(s)
            while mod_state["mm"] < 96:
                mod_mm()
            S.op(DVE, lambda e: e.tensor_tensor(out=modT[:, 32:96], in0=PTf[:, 32:96], in1=badd[:, 32:96], op=ALU.add),
                 reads=[PTb[1], Bc, B_mod], writes=[B_mod])
            S.op(DVE, lambda e: e.scalar_tensor_tensor(out=s2[:], in0=modT[:, 64:80], scalar=1.0, in1=smallv[:, 32:48],
                                                       op0=ALU.add, op1=ALU.mult), reads=[B_mod, B_const], writes=[B_mod])
            S.op(DVE, lambda e: e.tensor_tensor(out=smallv[:, 64:80], in0=modT[:, 32:48], in1=smallv[:, 16:32], op=ALU.mult),
                 reads=[B_mod, B_const], writes=[B_mod])
            S.op(DVE, lambda e: e.tensor_tensor(out=smallv[:, 80:96], in0=modT[:, 80:96], in1=smallv[:, 48:64], op=ALU.mult),
                 reads=[B_mod, B_const], writes=[B_mod])
            B_gsc = Buf("gsc")
            for i in range(2):
                S.dma(SP, lambda e, i=i: e.dma_start(out=gsc_d[i].rearrange("(c p) -> p c", p=128),
                                                     in_=smallv[:, 64 + 16 * i:80 + 16 * i], allow_slow_non_contiguous=True),
                      reads=[B_mod], writes=[B_gsc])
            tap("modT", modT[:], [128, 96], F32, [B_mod])
            tap("thr", thr_dbg[:], [128, 8], F32, [B_thr])
            tap("attT", attT[:], [128, 8, NOWN], BF16, [B_att])
            S.barrier()
            if limit == 4:
                raise _Stop()

        uact = sb(att_stack, "uact", [128, 8, NOWN], BF16)
        B_uact = Buf("uact")
        with Scope(arena) as st:
            sc_hown = Scope(arena)
            hT_own = sb(sc_hown, "hT_own2", [128, 16, NOWN], BF16)
            S.dma(SP, lambda e: e.dma_start(out=hT_own[:].rearrange("p c n -> p (c n)"), in_=hown_d), reads=[B_hownd], writes=B_hown)
            uT = sb(st, "uT", [128, 8, 32 + NOWN], BF16)
            B_uT = Buf()
            st2 = Scope(arena)
            sg = sb(st2, "sg", [128, 8, 32 + NOWN], BF16)
            B_sg = Buf()
            wblk = [sb(st2, f"wblkb{i}", [128, 16, 512], BF16) for i in range(2)]
            Bw2 = [Buf(), Buf()]
            glu_segs = [(lambda kc: hT_halo[:, kc, :], 32)] + own_segs
            seg_off = [0, 32, 32 + 512]

            def cons_sg(ch, si, pb, pbb):
                wd = glu_segs[si][1]
                S.op(ACT, lambda e: e.activation(out=sg[:, ch, seg_off[si]:seg_off[si] + wd], in_=pb[:, 0:wd], func=AF.Sigmoid),
                     reads=[pbb], writes=[B_sg])

            def cons_u(ch, si, pb, pbb):
                wd = glu_segs[si][1]
                S.op(DVE, lambda e: e.tensor_tensor(out=uT[:, ch, seg_off[si]:seg_off[si] + wd], in0=pb[:, 0:wd],
                                                    in1=sg[:, ch, seg_off[si]:seg_off[si] + wd], op=ALU.mult),
                     reads=[pbb, B_sg], writes=[B_uT])
            proj_fm(st, wv_in, O_GLU_G, 8, glu_segs, cons_sg, Bw2, wblk, B_hown + [B_halo])
            proj_fm(st, wv_in, O_GLU_A, 8, glu_segs, cons_u, Bw2, wblk, B_hown + [B_halo])
            S.op(DVE, lambda e: e.tensor_scalar(out=uT[:, :, 0:32], in0=uT[:, :, 0:32], scalar1=halov[:, 0:1], scalar2=None,
                                                op0=ALU.mult), reads=[B_uT, B_const], writes=[B_uT])
            tap("uT", uT[:], [128, 8, 32 + NOWN], BF16, [B_uT])
            S.barrier()
            if limit == 5:
                raise _Stop()
            st2.close()
            yb = sb(st, "yb", [128, 8, NOWN], F32)
            ysq = sb(st, "ysq", [128, 8, NOWN], F32)
            B_y = Buf()
            dgc = [sb(st, f"dgc{i}", [128, 31, 128], BF16) for i in range(2)]
            B_dgc = [Buf(), Buf()]
            mu = sb(st, "mu", [128, 512], F32)
            var = sb(st, "var", [128, 512], F32)
            rstd = sb(st, "rstd", [128, 512], F32)
            B_ln = Buf()
            zt = sb(st, "zt", [128, 512], F32)
            B_z = Buf()
            for ch in range(8):
                dg_t, Bdg = dgc[ch % 2], B_dgc[ch % 2]
                for tp in range(31):
                    engd = DVE if tp % 2 == 0 else POOL
                    S.op(engd, lambda e, tp=tp: e.tensor_scalar(out=dg_t[:, tp, :], in0=identb[:],
                                                                scalar1=wdw[:, ch * 31 + tp:ch * 31 + tp + 1], scalar2=None,
                                                                op0=ALU.mult), reads=[B_const], writes=[Bdg])
                for g in range(2):
                    off = g * 512 + 2
                    pbi = 2 + (ch * 2 + g) % 2
                    for tp in range(31):
                        S.op(PE, lambda e, tp=tp: e.matmul(PB[pbi][:, :], lhsT=dg_t[:, tp, :], rhs=uT[:, ch, off + tp:off + tp + 512],
                                                           start=(tp == 0), stop=(tp == 30)),
                             reads=[Bdg, B_uT], writes=[PBb[pbi]])
                    S.op(ACT, lambda e: e.activation(out=yb[:, ch, g * 512:(g + 1) * 512], in_=PB[pbi][:, :], func=AF.Identity,
                                                     bias=convv[:, ch:ch + 1], scale=1.0),
                         reads=[PBb[pbi], B_const], writes=[B_y])
                    S.op(ACT, lambda e: e.activation(out=ysq[:, ch, g * 512:(g + 1) * 512], in_=PB[pbi][:, :], func=AF.Square,
                                                     bias=convv[:, ch:ch + 1], scale=1.0),
                         reads=[PBb[pbi], B_const], writes=[B_y])
            for g in range(2):
                gs = slice(g * 512, (g + 1) * 512)
                for ch in range(8):
                    S.op(PE, lambda e, ch=ch: e.matmul(PB[0][:, :], lhsT=onesf[:], rhs=yb[:, ch, gs], start=(ch == 0),
                                                       stop=(ch == 7)), reads=[B_y, B_const], writes=[PBb[0]])
                for ch in range(8):
                    S.op(PE, lambda e, ch=ch: e.matmul(PB[1][:, :], lhsT=onesf[:], rhs=ysq[:, ch, gs], start=(ch == 0),
                                                       stop=(ch == 7)), reads=[B_y, B_const], writes=[PBb[1]])
                S.op(ACT, lambda e: e.activation(out=mu[:], in_=PB[0][:, :], func=AF.Copy, scale=1.0 / 1024),
                     reads=[PBb[0]], writes=[B_ln])
                S.op(DVE, lambda e: e.tensor_tensor(out=var[:], in0=mu[:], in1=mu[:], op=ALU.mult), reads=[B_ln], writes=[B_ln])
                S.op(DVE, lambda e: e.scalar_tensor_tensor(out=var[:], in0=PB[1][:, :], scalar=1.0 / 1024, in1=var[:],
                                                           op0=ALU.mult, op1=ALU.subtract), reads=[PBb[1], B_ln], writes=[B_ln])
                S.op(ACT, lambda e: e.activation(out=rstd[:], in_=var[:], func=AF.Sqrt, bias=epsc[:, 0:1], scale=1.0),
                     reads=[B_ln, B_const], writes=[B_ln])
                S.op(DVE, lambda e: e.reciprocal(out=rstd[:], in_=rstd[:]), reads=[B_ln], writes=[B_ln])
                for ch in range(8):
                    S.op(DVE, lambda e, ch=ch: e.tensor_tensor(out=zt[:], in0=yb[:, ch, gs], in1=mu[:], op=ALU.subtract),
                         reads=[B_y, B_ln], writes=[B_z])
                    S.op(DVE, lambda e: e.tensor_tensor(out=zt[:], in0=zt[:], in1=rstd[:], op=ALU.mult),
                         reads=[B_ln], writes=[B_z])
                    S.op(ACT, lambda e, ch=ch: e.activation(out=uact[:, ch, gs], in_=zt[:], func=AF.Silu,
                                                            scale=convv[:, 8 + ch:9 + ch], bias=convv[:, 16 + ch:17 + ch]),
                         reads=[B_z, B_const], writes=[B_uact])
            tap("uact", uact[:], [128, 8, NOWN], BF16, [B_uact])
            S.barrier()
            if limit == 6:
                raise _Stop()

        wv_co = w_co.rearrange("(c p) n -> p c n", p=128)
        wv_ao = w_ao.rearrange("(c p) n -> p c n", p=128)
        wv_o = w_o.rearrange("(c p) n -> p c n", p=128)
        wv_g = w_gate.rearrange("(c p) n -> p c n", p=128)
        wv_u = w_up.rearrange("(c p) n -> p c n", p=128)
        wv_d = w_down.rearrange("(c p) n -> p c n", p=128)
        sc_kiT.close()
        sc_halo.close()
        wslot = [0, 0, 0]

        def wload(slots, Bs, which, dst_view_fn, src_ap, nsplit=2):
            i = wslot[which] % 2
            wslot[which] += 1
            w_t = slots[i]
            n0 = src_ap.shape[1]
            step = (n0 + nsplit - 1) // nsplit
            for a in range(0, n0, step):
                b = min(n0, a + step)
                S.dma(POOL, lambda e, a=a, b=b, w_t=w_t: e.dma_start(out=dst_view_fn(w_t)[:, a:b, :], in_=src_ap[:, a:b, :]),
                      writes=[Bs[i]])
            return w_t, Bs[i]

        sc_mg = Scope(arena)
        mergedT = sb(sc_mg, "mergedT", [128, 16, NOWN], BF16)
        B_mg = Buf()
        with Scope(arena) as st:
            Wa = [sb(st, f"Wa{i}", [128, 16, 512], BF16) for i in range(2)]
            B_Wa = [Buf(), Buf()]
            Wb = [sb(st, f"Wb{i}", [128, 16, 512], BF16) for i in range(2)]
            B_Wb = [Buf(), Buf()]
            tA = sb(st, "tA", [128, 512], F32)
            tB = sb(st, "tB", [128, 512], BF16)
            tC = sb(st, "tC", [128, 512], BF16)
            B_t = [Buf(), Buf(), Buf()]
            for nb in range(4):
                cs = slice(nb * 512, (nb + 1) * 512)
                Wgc, Bgc = wload(Wa, B_Wa, 0, lambda w: w[:, :, :], wv_in[:, :, O_GC + nb * 512:O_GC + (nb + 1) * 512])
                Wga, Bga = wload(Wa, B_Wa, 0, lambda w: w[:, :, :], wv_in[:, :, O_GA + nb * 512:O_GA + (nb + 1) * 512])
                Wco, Bco = wload(Wb, B_Wb, 1, lambda w: w[:, 0:8, :], wv_co[:, :, cs])
                Wao, Bao = wload(Wb, B_Wb, 1, lambda w: w[:, 0:8, :], wv_ao[:, :, cs])
                for g in range(2):
                    gs = slice(g * 512, (g + 1) * 512)
                    for cc in range(4):
                        f = nb * 4 + cc
                        fs = slice(cc * 128, (cc + 1) * 128)
                        for kc in range(KC):
                            S.op(PE, lambda e, kc=kc, fs=fs, Wgc=Wgc, gs=gs: e.matmul(PB[0][:, :], lhsT=Wgc[:, kc, fs], rhs=hT_own[:, kc, gs],
                                                                                   start=(kc == 0), stop=(kc == KC - 1)),
                                 reads=[Bgc] + B_hown, writes=[PBb[0]])
                        for kc in range(KC):
                            S.op(PE, lambda e, kc=kc, fs=fs, Wga=Wga, gs=gs: e.matmul(PB[1][:, :], lhsT=Wga[:, kc, fs], rhs=hT_own[:, kc, gs],
                                                                                   start=(kc == 0), stop=(kc == KC - 1)),
                                 reads=[Bga] + B_hown, writes=[PBb[1]])
                        for kc in range(8):
                            S.op(PE, lambda e, kc=kc, fs=fs, Wco=Wco, gs=gs: e.matmul(PB[2][:, :], lhsT=Wco[:, kc, fs], rhs=uact[:, kc, gs],
                                                                                   start=(kc == 0), stop=(kc == 7)),
                                 reads=[Bco, B_uact], writes=[PBb[2]])
                        for kc in range(8):
                            S.op(PE, lambda e, kc=kc, fs=fs, Wao=Wao, gs=gs: e.matmul(PB[3][:, :], lhsT=Wao[:, kc, fs], rhs=attT[:, kc, gs],
                                                                                   start=(kc == 0), stop=(kc == 7)),
                                 reads=[Bao, B_att], writes=[PBb[3]])
                        S.op(ACT, lambda e: e.activation(out=tB[:], in_=PB[0][:, :], func=AF.Sigmoid), reads=[PBb[0]], writes=[B_t[1]])
                        S.op(ACT, lambda e: e.activation(out=tC[:], in_=PB[1][:, :], func=AF.Sigmoid), reads=[PBb[1]], writes=[B_t[2]])
                        S.op(DVE, lambda e: e.tensor_tensor(out=tA[:], in0=PB[2][:, :], in1=tB[:], op=ALU.mult),
                             reads=[PBb[2], B_t[1]], writes=[B_t[0]])
                        S.op(DVE, lambda e: e.tensor_tensor(out=tB[:], in0=PB[3][:, :], in1=tC[:], op=ALU.mult),
                             reads=[PBb[3], B_t[2]], writes=[B_t[1]])
                        S.op(DVE, lambda e, f=f, gs=gs: e.tensor_tensor(out=mergedT[:, f, gs], in0=tA[:], in1=tB[:], op=ALU.add),
                             reads=[B_t[0], B_t[1]], writes=[B_mg])
            tap("mergedT", mergedT[:], [128, 16, NOWN], BF16, [B_mg])
            S.barrier()
            if limit == 7:
                raise _Stop()
        att_stack.close()
        sc_hown.close()

        sc_h2 = Scope(arena)
        h2T = sb(sc_h2, "h2T", [128, 16, NOWN], BF16)
        B_h2 = Buf()
        B_out = Buf("out")
        with Scope(arena) as st:
            mx = sb(st, "mx", [128, 8, D], F32)
            B_mx = [Buf() for _ in range(8)]
            ssp = sb(st, "ssp", [128, 8, 4], F32)
            B_ss = [Buf() for _ in range(8)]
            tC = sb(st, "tC2", [128, 512], BF16)
            B_tc = Buf()
            with Scope(arena) as stw:
                Wa = [sb(stw, f"Wo{i}", [128, 16, 512], BF16) for i in range(2)]
                B_Wa = [Buf(), Buf()]
                for nb in range(4):
                    Wo, Bo = wload(Wa, B_Wa, 0, lambda w: w[:, :, :], wv_o[:, :, nb * 512:(nb + 1) * 512])
                    for t in range(8):
                        pbi = 2 + (t % 4)
                        tok = slice(t * 128, (t + 1) * 128)
                        for kc in range(KC):
                            S.op(PE, lambda e, kc=kc: e.matmul(PB[pbi][:, :], lhsT=mergedT[:, kc, tok], rhs=Wo[:, kc, :],
                                                               start=(kc == 0), stop=(kc == KC - 1)),
                                 reads=[Bo, B_mg], writes=[PBb[pbi]])
                        S.op(ACT, lambda e: e.activation(out=mx[:, t, nb * 512:(nb + 1) * 512], in_=PB[pbi][:, :], func=AF.Copy),
                             reads=[PBb[pbi]], writes=[B_mx[t]])
                        S.op(ACT, lambda e: e.activation(out=tC[:], in_=PB[pbi][:, :], func=AF.Square,
                                                         accum_out=ssp[:, t, nb:nb + 1]),
                             reads=[PBb[pbi]], writes=[B_tc, B_ss[t]])
                S.barrier()
                if limit == 8:
                    raise _Stop()
            with Scope(arena) as stf:
                G1 = sb(stf, "G1", [128, D], F32)
                B_G = Buf()
                S.dma(SP, lambda e: e.dma_start(out=G1[:], in_=gsc_d[0].partition_broadcast(128)), reads=[B_gsc], writes=[B_G])
                ss1_ = sb(stf, "ss1", [128, 8, 16], F32)
                xt = [sb(stf, f"xtb{i}", [128, D], F32) for i in range(2)]
                B_xt = [Buf(), Buf()]
                x1t = [sb(stf, f"x1t{i}", [128, D], F32) for i in range(2)]
                B_x1 = [Buf(), Buf()]
                xn2 = [sb(stf, f"xn2{i}", [128, D], BF16) for i in range(2)]
                B_xn2 = [Buf(), Buf()]
                for t in range(8):
                    ss1 = ss1_[:, t, :]
                    xb = xt[t % 2]
                    x1 = x1t[t % 2]
                    xn = xn2[t % 2]
                    Bxn = B_xn2[t % 2]
                    r0 = NCTX + t * 128
                    o0 = t * 128
                    S.dma(SP, lambda e: e.dma_start(out=xb[:], in_=xctx[r0:r0 + 128, :]), writes=[B_xt[t % 2]])
                    S.op(DVE, lambda e: e.tensor_reduce(out=ss1[:, 0:1], in_=ssp[:, t, :], axis=AX.X, op=ALU.add),
                         reads=[B_ss[t]], writes=[B_ss[t]])
                    rstd_from_ssq(ss1[:, 2:3], ss1[:, 0:1], ss1[:, 1:2], [B_ss[t]], [B_ss[t]])
                    S.op(DVE, lambda e: e.scalar_tensor_tensor(out=mx[:, t, :], in0=mx[:, t, :], scalar=ss1[:, 2:3], in1=G1[:],
                                                               op0=ALU.mult, op1=ALU.mult),
                         reads=[B_ss[t], B_G], writes=[B_mx[t]])
                    S.op(DVE, lambda e: e.tensor_tensor(out=x1[:], in0=mx[:, t, :], in1=xb[:], op=ALU.add),
                         reads=[B_mx[t], B_xt[t % 2]], writes=[B_x1[t % 2]])
                    S.dma(SP, lambda e: e.dma_start(out=out[o0:o0 + 128, :], in_=x1[:]), reads=[B_x1[t % 2]],
                          writes=[B_out])
                    S.op(ACT, lambda e: e.activation(out=xn[:], in_=x1[:], func=AF.Square, accum_out=ss1[:, 4:5]),
                         reads=[B_x1[t % 2]], writes=[Bxn, B_ss[t]])
                    rstd_from_ssq(ss1[:, 6:7], ss1[:, 4:5], ss1[:, 5:6], [B_ss[t]], [B_ss[t]])
                    S.op(ACT, lambda e: e.activation(out=xn[:], in_=x1[:], func=AF.Copy, scale=ss1[:, 6:7]),
                         reads=[B_x1[t % 2], B_ss[t]], writes=[Bxn])
                    for c4 in range(4):
                        pt = PT[c4 % 2]
                        for cc in range(4):
                            c = c4 * 4 + cc
                            S.op(PE, lambda e, c=c, cc=cc: e.transpose(out=pt[:, cc * 128:(cc + 1) * 128], in_=xn[:, c * 128:(c + 1) * 128],
                                                                     identity=identb[:]), reads=[Bxn, B_const], writes=[PTb[c4 % 2]])
                        for cc in range(4):
                            c = c4 * 4 + cc
                            if c4 % 2 == 0:
                                S.op(ACT, lambda e, c=c, cc=cc: e.activation(out=h2T[:, c, o0:o0 + 128], in_=pt[:, cc * 128:(cc + 1) * 128],
                                                                            func=AF.Identity, scale=s2[:, c:c + 1],
                                                                            bias=modT[:, 48 + c:49 + c]),
                                     reads=[PTb[c4 % 2], B_mod], writes=[B_h2])
                            else:
                                S.op(DVE, lambda e, c=c, cc=cc: e.tensor_scalar(out=h2T[:, c, o0:o0 + 128], in0=pt[:, cc * 128:(cc + 1) * 128],
                                                                               scalar1=s2[:, c:c + 1], scalar2=modT[:, 48 + c:49 + c],
                                                                               op0=ALU.mult, op1=ALU.add),
                                     reads=[PTb[c4 % 2], B_mod], writes=[B_h2])
                tap("h2T", h2T[:], [128, 16, NOWN], BF16, [B_h2])
                S.barrier()
                if limit == 9:
                    raise _Stop()
        sc_mg.close()

        with Scope(arena) as st:
            mx = sb(st, "mx3", [128, 8, D], F32)
            B_mx = [Buf() for _ in range(8)]
            with Scope(arena) as stw:
                Wa = [sb(stw, f"Wg{i}", [128, 16, 256], BF16) for i in range(2)]
                B_Wa = [Buf(), Buf()]
                Wb = [sb(stw, f"Wu{i}", [128, 16, 256], BF16) for i in range(2)]
                B_Wb = [Buf(), Buf()]
                Wd_t = [sb(stw, f"Wd{i}", [128, 11, 512], BF16) for i in range(2)]
                B_Wd = [Buf(), Buf()]
                tBs = [sb(stw, f"tB3{i}", [128, 512], BF16) for i in range(2)]
                B_tb = [Buf(), Buf()]
                aT = sb(stw, "aT", [128, 11, NOWN], BF16)
                B_aT = Buf()
                stepi = 0
                for Q in range(4):
                    for j2 in range(6):
                        nch = 2 if j2 < 5 else 1
                        c0 = (Q * 11 + j2 * 2) * 128
                        Wg, Bg = wload(Wa, B_Wa, 0, lambda w: w[:, :, 0:nch * 128], wv_g[:, :, c0:c0 + nch * 128])
                        Wu, Bu = wload(Wb, B_Wb, 1, lambda w: w[:, :, 0:nch * 128], wv_u[:, :, c0:c0 + nch * 128])
                        for cc in range(nch):
                            j = j2 * 2 + cc
                            fs = slice(cc * 128, (cc + 1) * 128)
                            for half in range(2):
                                gs = slice(half * 512, (half + 1) * 512)
                                pg, pu = (0, 1) if stepi % 2 == 0 else (4, 5)
                                tB, Btb = tBs[stepi % 2], B_tb[stepi % 2]
                                stepi += 1
                                for kc in range(KC):
                                    S.op(PE, lambda e, kc=kc: e.matmul(PB[pg][:, :], lhsT=Wg[:, kc, fs], rhs=h2T[:, kc, gs],
                                                                       start=(kc == 0), stop=(kc == KC - 1)),
                                         reads=[Bg, B_h2], writes=[PBb[pg]])
                                for kc in range(KC):
                                    S.op(PE, lambda e, kc=kc: e.matmul(PB[pu][:, :], lhsT=Wu[:, kc, fs], rhs=h2T[:, kc, gs],
                                                                       start=(kc == 0), stop=(kc == KC - 1)),
                                         reads=[Bu, B_h2], writes=[PBb[pu]])
                                S.op(ACT, lambda e: e.activation(out=tB[:], in_=PB[pg][:, :], func=AF.Silu), reads=[PBb[pg]], writes=[Btb])
                                S.op(DVE, lambda e: e.tensor_tensor(out=aT[:, j, gs], in0=PB[pu][:, :], in1=tB[:], op=ALU.mult),
                                     reads=[PBb[pu], Btb], writes=[B_aT])
                    for nb in range(4):
                        Wd, Bd = wload(Wd_t, B_Wd, 2, lambda w: w[:, :, :],
                                       wv_d[:, Q * 11:(Q + 1) * 11, nb * 512:(nb + 1) * 512])
                        for t in range(8):
                            pbi = 2 + (t % 2)
                            for j in range(11):
                                S.op(PE, lambda e, j=j: e.matmul(PB[pbi][:, :], lhsT=aT[:, j, t * 128:(t + 1) * 128], rhs=Wd[:, j, :],
                                                                 start=(j == 0), stop=(j == 10)), reads=[Bd, B_aT], writes=[PBb[pbi]])
                            dst = mx[:, t, nb * 512:(nb + 1) * 512]
                            if Q == 0:
                                S.op(ACT, lambda e: e.activation(out=dst, in_=PB[pbi][:, :], func=AF.Copy),
                                     reads=[PBb[pbi]], writes=[B_mx[t]])
                            else:
                                S.op(DVE, lambda e: e.tensor_tensor(out=dst, in0=PB[pbi][:, :], in1=dst, op=ALU.add),
                                     reads=[PBb[pbi]], writes=[B_mx[t]])
                S.barrier()
                if limit == 10:
                    raise _Stop()
            with Scope(arena) as stf:
                G2 = sb(stf, "G2", [128, D], F32)
                B_G = Buf()
                S.dma(SP, lambda e: e.dma_start(out=G2[:], in_=gsc_d[1].partition_broadcast(128)), reads=[B_gsc], writes=[B_G])
                ss1_ = sb(stf, "ss3", [128, 8, 16], F32)
                B_ss = [Buf() for _ in range(8)]
                x1t = [sb(stf, f"x1t3{i}", [128, D], F32) for i in range(2)]
                B_x1 = [Buf(), Buf()]
                xn2 = sb(stf, "xn3", [128, D], BF16)
                B_xn2 = Buf()
                for t in range(8):
                    ss1 = ss1_[:, t, :]
                    x1 = x1t[t % 2]
                    o0 = t * 128
                    S.dma(SP, lambda e: e.dma_start(out=x1[:], in_=out[o0:o0 + 128, :]), reads=[B_out],
                          writes=[B_x1[t % 2]])
                    S.op(ACT, lambda e: e.activation(out=xn2[:], in_=mx[:, t, :], func=AF.Square, accum_out=ss1[:, 8:9]),
                         reads=[B_mx[t]], writes=[B_xn2, B_ss[t]])
                    rstd_from_ssq(ss1[:, 10:11], ss1[:, 8:9], ss1[:, 9:10], [B_ss[t]], [B_ss[t]])
                    S.op(DVE, lambda e: e.scalar_tensor_tensor(out=mx[:, t, :], in0=mx[:, t, :], scalar=ss1[:, 10:11], in1=G2[:],
                                                               op0=ALU.mult, op1=ALU.mult),
                         reads=[B_ss[t], B_G], writes=[B_mx[t]])
                    S.op(DVE, lambda e: e.tensor_tensor(out=x1[:], in0=mx[:, t, :], in1=x1[:], op=ALU.add),
                         reads=[B_mx[t]], writes=[B_x1[t % 2]])
                    S.dma(SP, lambda e: e.dma_start(out=out[o0:o0 + 128, :], in_=x1[:]), reads=[B_x1[t % 2]],
                          writes=[B_out])
                S.barrier()
        sc_h2.close()
    except _Stop:
        S.barrier()
    stack.close()
    build.last_sched = S
    return nc, dbg_outs


def _fm(v, nch):
    return np.ascontiguousarray(np.asarray(v, np.float32).reshape(nch, 128).T)


def make_in_maps(x, c, w_ada, b_ada, g_pre_mix, w_in, w_dw, b_dw, g_conv_ln, b_conv_ln, w_conv_out, w_attn_out,
                 w_o, g_post_mix, g_pre_ffn, w_gate, w_up, w_down, g_post_ffn):
    x = np.asarray(x, np.float32)
    shared = {
        "w_ada": np.ascontiguousarray(np.asarray(w_ada, np.float32)[0]),
        "b_adaT": _fm(np.asarray(b_ada)[0], 96),
        "g_pre_mixT": _fm(np.asarray(g_pre_mix)[0], 16),
        "w_in": np.ascontiguousarray(np.asarray(w_in, np.float32)[0]),
        "w_dwT": np.ascontiguousarray(
            np.asarray(w_dw, np.float32)[0].T.reshape(8, 128, 31).transpose(1, 0, 2).reshape(128, 8 * 31)),
        "b_dwT": _fm(np.asarray(b_dw)[0], 8),
        "g_lnT": _fm(np.asarray(g_conv_ln)[0], 8),
        "b_lnT": _fm(np.asarray(b_conv_ln)[0], 8),
        "w_conv_out": np.ascontiguousarray(np.asarray(w_conv_out, np.float32)[0]),
        "w_attn_out": np.ascontiguousarray(np.asarray(w_attn_out, np.float32)[0]),
        "w_o": np.ascontiguousarray(np.asarray(w_o, np.float32)[0]),
        "g_post_mixT": _fm(np.asarray(g_post_mix)[0], 16),
        "g_pre_ffnT": _fm(np.asarray(g_pre_ffn)[0], 16),
        "w_gate": np.ascontiguousarray(np.asarray(w_gate, np.float32)[0]),
        "w_up": np.ascontiguousarray(np.asarray(w_up, np.float32)[0]),
        "w_down": np.ascontiguousarray(np.asarray(w_down, np.float32)[0]),
        "g_post_ffnT": _fm(np.asarray(g_post_ffn)[0], 16),
        "ident": np.eye(128, dtype=np.float32),
        "pow2": np.ascontiguousarray(np.tile((2.0 ** -(np.arange(NITER + 1) + 1.0)).astype(np.float32)[None, :], (128, 1))),
    }
    diagc = np.zeros((1, 256), np.float32)
    diagc[0, 0:64] = 1.0
    diagc[0, 128 + 64:256] = -BIG
    shared["diagc"] = diagc
    maps = []
    for core in range(8):
        b, j = core // 4, core % 4
        xc = np.zeros((NK, D), np.float32)
        nval = j * 1024
        xc[NCTX - nval:NCTX] = x[b, 0:nval]
        xc[NCTX:] = x[b, j * 1024:(j + 1) * 1024]
        kneg = np.zeros((1, 512), np.float32)
        for sgm in range(3):
            if sgm < 3 - j:
                kneg[0, sgm * 128:(sgm + 1) * 128] = -BIG
        pen = np.zeros((128, 6), np.float32)
        for sgm in range(3):
            if sgm < 3 - j:
                pen[:, sgm] = 2 * BIG
        pen[0:64, 5] = 2 * BIG
        m = dict(shared)
        m["xctx"] = xc
        m["cT"] = _fm(np.asarray(c, np.float32)[b], 16)
        m["kneg"] = kneg
        m["pen"] = pen
        m["halo_valid"] = np.full((128, 1), 1.0 if j > 0 else 0.0, np.float32)
        maps.append(m)
    return maps


_NC_CACHE = {}


def kernel(**inputs):
    if "nc" not in _NC_CACHE:
        _NC_CACHE["nc"] = build()[0]
    nc = _NC_CACHE["nc"]
    maps = make_in_maps(**inputs)
    res = run_bass_kernel_spmd(nc, maps, core_ids=list(range(8)))
    outp = np.zeros((2, 4096, D), np.float32)
    for core in range(8):
        b, j = core // 4, core % 4
        outp[b, j * 1024:(j + 1) * 1024] = res.results[core]["out"]
    return outp
```
